# Optimizing a Trainium2 kernel written in Bass

```python
import jax, jax.numpy as jnp
from jax import lax
import numpy as np

D_MODEL = 4096
BATCH = 1
SEQ = 8192
DEPTH = 1

MEM_LEN = 256
MIX_WIDTH = D_MODEL
POOL_WIDTH = MIX_WIDTH // 2
POOL_WINDOWS = (2, 4, 8, 16)
POOL_GROUP = POOL_WIDTH // len(POOL_WINDOWS)
HGRN_WIDTH = MIX_WIDTH - POOL_WIDTH
HGRN_HEAD_K = 128
HGRN_HEADS = HGRN_WIDTH // HGRN_HEAD_K
HGRN_HEAD_V = HGRN_WIDTH // HGRN_HEADS
HGRN_KEY = HGRN_HEADS * HGRN_HEAD_K
CHUNK = 64
XATTN_HEADS = 4
XATTN_HEAD_DIM = D_MODEL // XATTN_HEADS
N_GROUPS = 4
EXPERTS_PER_GROUP = 8
N_EXPERTS = N_GROUPS * EXPERTS_PER_GROUP
TOP_K = 2
D_EXPERT = D_MODEL // 4
MOE_BLOCK = 128
EPS = 1e-6
IN_SPLITS = [POOL_WIDTH,
             POOL_WIDTH + HGRN_KEY,
             POOL_WIDTH + 2 * HGRN_KEY,
             POOL_WIDTH + 3 * HGRN_KEY,
             POOL_WIDTH + 3 * HGRN_KEY + HGRN_WIDTH]
IN_COLS = POOL_WIDTH + 3 * HGRN_KEY + 2 * HGRN_WIDTH

kernel_name = "hybrid_pool_hgrn2_memxattn_hmoe_encoder"


def rms_norm(x, gain):
    xf = x.astype(jnp.float32)
    y = xf * lax.rsqrt(jnp.mean(xf * xf, axis=-1, keepdims=True) + EPS)
    return (y * gain.astype(jnp.float32)).astype(x.dtype)


def multiscale_pool(u, w_pool, scale):
    S = u.shape[1]
    uf = u.astype(jnp.float32)
    cs = jnp.concatenate([jnp.zeros_like(uf[:, :1]), jnp.cumsum(uf, axis=1)], axis=1)
    t = jnp.arange(S)
    outs = []
    for gi, w in enumerate(POOL_WINDOWS):
        lo = jnp.clip(t - w // 2, 0, S - 1)
        hi = jnp.clip(t + w // 2 - 1, 0, S - 1)
        sl = slice(gi * POOL_GROUP, (gi + 1) * POOL_GROUP)
        csg = cs[..., sl]
        win_sum = csg[:, hi + 1] - csg[:, lo]
        count = (hi - lo + 1).astype(jnp.float32)[None, :, None]
        mixed = (win_sum / count - uf[..., sl]).astype(u.dtype)
        outs.append(jnp.einsum('bsc,cd->bsd', mixed, w_pool[gi]))
    return jnp.concatenate(outs, axis=-1) * scale


def hgrn2_scan(q, v, f_logit, lb):
    B, S, H, K = q.shape
    V = v.shape[-1]
    f = lb + (1.0 - lb) * jax.nn.sigmoid(f_logit)
    logf = jnp.log(f)
    k = 1.0 - f
    N = S // CHUNK
    qc = q.reshape(B, N, CHUNK, H, K)
    kc = k.reshape(B, N, CHUNK, H, K)
    vc = v.reshape(B, N, CHUNK, H, V)
    b = jnp.cumsum(logf.reshape(B, N, CHUNK, H, K), axis=2)
    b_last = b[:, :, -1]
    qd = qc * jnp.exp(b)
    kd = kc * jnp.exp(-b)
    mask = jnp.tril(jnp.ones((CHUNK, CHUNK), dtype=bool))
    A = jnp.where(mask, jnp.einsum('bnthk,bnshk->bnhts', qd, kd), 0.0)
    o_intra = jnp.einsum('bnhts,bnshv->bnthv', A, vc)
    k_end = kc * jnp.exp(b_last[:, :, None] - b)
    dS = jnp.einsum('bnshk,bnshv->bnhkv', k_end, vc)
    decay = jnp.exp(b_last)

    def step(state, inp):
        dec, ds = inp
        return dec[..., None] * state + ds, state

    s0 = jnp.zeros((B, H, K, V), jnp.float32)
    _, s_in = lax.scan(step, s0, (jnp.moveaxis(decay, 1, 0), jnp.moveaxis(dS, 1, 0)))
    s_in = jnp.moveaxis(s_in, 0, 1)
    o_inter = jnp.einsum('bnthk,bnhkv->bnthv', qd, s_in)
    return (o_intra + o_inter).reshape(B, S, H, V)


def hgrn2_mixer(q, f_fwd, f_bwd, i_in, g, lb_fwd, lb_bwd, norm_gain):
    B, S, _ = q.shape
    dt = q.dtype
    qh = jax.nn.silu(q.astype(jnp.float32).reshape(B, S, HGRN_HEADS, HGRN_HEAD_K))
    ih = i_in.astype(jnp.float32).reshape(B, S, HGRN_HEADS, HGRN_HEAD_V)
    ff = f_fwd.astype(jnp.float32).reshape(B, S, HGRN_HEADS, HGRN_HEAD_K)
    fb = f_bwd.astype(jnp.float32).reshape(B, S, HGRN_HEADS, HGRN_HEAD_K)
    o_f = hgrn2_scan(qh, ih, ff, lb_fwd.reshape(HGRN_HEADS, HGRN_HEAD_K))
    o_b = jnp.flip(hgrn2_scan(jnp.flip(qh, 1), jnp.flip(ih, 1), jnp.flip(fb, 1),
                              lb_bwd.reshape(HGRN_HEADS, HGRN_HEAD_K)), 1)
    o = o_f + o_b
    o = o * lax.rsqrt(jnp.mean(o * o, axis=-1, keepdims=True) + EPS)
    o = o * norm_gain.astype(jnp.float32).reshape(HGRN_HEADS, HGRN_HEAD_V)
    o = o.reshape(B, S, HGRN_WIDTH) * jax.nn.silu(g.astype(jnp.float32))
    return o.astype(dt)


def layer_lower_bound(lb_param, layer):
    return jnp.cumsum(jax.nn.softmax(lb_param.astype(jnp.float32), axis=0), axis=0)[layer]


def memory_cross_attention(h, mem_n, wq, wk, wv, wo):
    B, S, D = h.shape
    M = mem_n.shape[1]
    q = jnp.einsum('bsd,de->bse', h, wq).reshape(B, S, XATTN_HEADS, XATTN_HEAD_DIM)
    k = jnp.einsum('bmd,de->bme', mem_n, wk).reshape(B, M, XATTN_HEADS, XATTN_HEAD_DIM)
    v = jnp.einsum('bmd,de->bme', mem_n, wv).reshape(B, M, XATTN_HEADS, XATTN_HEAD_DIM)
    s = jnp.einsum('bshd,bmhd->bhsm', q, k).astype(jnp.float32) * (XATTN_HEAD_DIM ** -0.5)
    p = jax.nn.softmax(s, axis=-1).astype(v.dtype)
    o = jnp.einsum('bhsm,bmhd->bshd', p, v).reshape(B, S, D)
    return jnp.einsum('bsd,de->bse', o, wo)


def hierarchical_moe(h, layer, w_router_group, w_router_expert, w1, w3, w2):
    B, S, D = h.shape
    T = B * S
    xt = h.reshape(T, D)
    g_logits = jnp.einsum('td,dg->tg', xt, w_router_group[layer]).astype(jnp.float32)
    g_probs = jax.nn.softmax(g_logits, axis=-1)
    g_idx = jnp.argmax(g_logits, axis=-1).astype(jnp.int32)
    g_w = jnp.take_along_axis(g_probs, g_idx[:, None], axis=-1)
    e_logits = jnp.einsum('td,gde->tge', xt, w_router_expert[layer]).astype(jnp.float32)
    e_logits = jnp.take_along_axis(e_logits, g_idx[:, None, None], axis=1)[:, 0]
    top_v, top_i = lax.top_k(e_logits, TOP_K)
    e_w = jax.nn.softmax(top_v, axis=-1) * g_w
    eid = g_idx[:, None] * EXPERTS_PER_GROUP + top_i.astype(jnp.int32)

    A = T * TOP_K
    flat_e = eid.reshape(A)
    flat_t = jnp.repeat(jnp.arange(T, dtype=jnp.int32), TOP_K)
    flat_w = e_w.reshape(A)
    order = jnp.argsort(flat_e)
    se = flat_e[order]
    counts = jnp.bincount(flat_e, length=N_EXPERTS)
    padded = ((counts + MOE_BLOCK - 1) // MOE_BLOCK) * MOE_BLOCK
    start_sorted = jnp.cumsum(counts) - counts
    pad_end = jnp.cumsum(padded)
    start_pad = pad_end - padded
    dest = start_pad[se] + jnp.arange(A, dtype=jnp.int32) - start_sorted[se]
    P = A + N_EXPERTS * MOE_BLOCK
    NB = P // MOE_BLOCK
    buf_t = jnp.zeros((P,), jnp.int32).at[dest].set(flat_t[order])
    buf_w = jnp.zeros((P,), jnp.float32).at[dest].set(flat_w[order])
    block_e = jnp.minimum(jnp.searchsorted(pad_end, jnp.arange(NB) * MOE_BLOCK, side='right'),
                          N_EXPERTS - 1).astype(jnp.int32)

    def expert_block(args):
        tok, wt, e = args
        xb = xt[tok]
        a = xb @ w1[layer, e]
        c = xb @ w3[layer, e]
        y = (jax.nn.silu(a) * c) @ w2[layer, e]
        return y * wt[:, None].astype(y.dtype)

    y = lax.map(expert_block, (buf_t.reshape(NB, MOE_BLOCK), buf_w.reshape(NB, MOE_BLOCK), block_e))
    out = jnp.zeros((T, D), h.dtype).at[buf_t].add(y.reshape(P, D).astype(h.dtype))
    return out.reshape(B, S, D)


def setup_inputs(seed: int = 0) -> dict:
    key = jax.random.key(seed)
    ks = jax.random.split(key, 24)
    f32 = jnp.float32
    nrm = lambda k, shape, fan_in: jax.random.normal(k, shape, f32) * (fan_in ** -0.5)
    gain = lambda k, shape: 1.0 + 0.02 * jax.random.normal(k, shape, f32)
    return {
        "x": jax.random.normal(ks[0], (BATCH, SEQ, D_MODEL), f32),
        "mem": jax.random.normal(ks[1], (BATCH, MEM_LEN, D_MODEL), f32),
        "norm_mix": gain(ks[2], (DEPTH, D_MODEL)),
        "w_in": nrm(ks[3], (DEPTH, D_MODEL, IN_COLS), D_MODEL),
        "pool_w": nrm(ks[4], (DEPTH, len(POOL_WINDOWS), POOL_GROUP, POOL_GROUP), POOL_GROUP),
        "pool_scale": gain(ks[5], (DEPTH, POOL_WIDTH)),
        "lb_fwd": 0.1 * jax.random.normal(ks[6], (DEPTH + 1, HGRN_KEY), f32),
        "lb_bwd": 0.1 * jax.random.normal(ks[7], (DEPTH + 1, HGRN_KEY), f32),
        "hgrn_norm": gain(ks[8], (DEPTH, HGRN_WIDTH)),
        "w_out": nrm(ks[9], (DEPTH, MIX_WIDTH, D_MODEL), MIX_WIDTH),
        "norm_xattn": gain(ks[10], (DEPTH, D_MODEL)),
        "norm_mem": gain(ks[11], (DEPTH, D_MODEL)),
        "w_q": nrm(ks[12], (DEPTH, D_MODEL, D_MODEL), D_MODEL),
        "w_k": nrm(ks[13], (DEPTH, D_MODEL, D_MODEL), D_MODEL),
        "w_v": nrm(ks[14], (DEPTH, D_MODEL, D_MODEL), D_MODEL),
        "w_o": nrm(ks[15], (DEPTH, D_MODEL, D_MODEL), D_MODEL),
        "norm_moe": gain(ks[16], (DEPTH, D_MODEL)),
        "w_router_group": nrm(ks[17], (DEPTH, D_MODEL, N_GROUPS), D_MODEL),
        "w_router_expert": nrm(ks[18], (DEPTH, N_GROUPS, D_MODEL, EXPERTS_PER_GROUP), D_MODEL),
        "w1": nrm(ks[19], (DEPTH, N_EXPERTS, D_MODEL, D_EXPERT), D_MODEL),
        "w3": nrm(ks[20], (DEPTH, N_EXPERTS, D_MODEL, D_EXPERT), D_MODEL),
        "w2": nrm(ks[21], (DEPTH, N_EXPERTS, D_EXPERT, D_MODEL), D_EXPERT),
        "norm_final": gain(ks[22], (D_MODEL,)),
    }


def reference(x, mem, norm_mix, w_in, pool_w, pool_scale, lb_fwd, lb_bwd, hgrn_norm, w_out,
              norm_xattn, norm_mem, w_q, w_k, w_v, w_o, norm_moe, w_router_group,
              w_router_expert, w1, w3, w2, norm_final):
    for l in range(DEPTH):
        h = rms_norm(x, norm_mix[l])
        proj = jnp.einsum('bsd,dc->bsc', h, w_in[l])
        u_pool, q, f_f, f_b, i_in, g = jnp.split(proj, IN_SPLITS, axis=-1)
        y_pool = multiscale_pool(u_pool, pool_w[l], pool_scale[l])
        y_hgrn = hgrn2_mixer(q, f_f, f_b, i_in, g,
                             layer_lower_bound(lb_fwd, l), layer_lower_bound(lb_bwd, l),
                             hgrn_norm[l])
        mixed = jnp.concatenate([y_pool, y_hgrn.astype(y_pool.dtype)], axis=-1)
        x = x + jnp.einsum('bsc,cd->bsd', mixed, w_out[l])
        mem_n = rms_norm(mem, norm_mem[l])
        x = x + memory_cross_attention(rms_norm(x, norm_xattn[l]), mem_n,
                                       w_q[l], w_k[l], w_v[l], w_o[l])
        x = x + hierarchical_moe(rms_norm(x, norm_moe[l]), l, w_router_group,
                                 w_router_expert, w1, w3, w2)
    return rms_norm(x, norm_final)
```

```python
import contextlib
import numpy as np
import concourse.bass as bass
import concourse.mybir as mybir
from concourse.bass_utils import run_bass_kernel_spmd

F32 = mybir.dt.float32
F32R = mybir.dt.float32r
I32 = mybir.dt.int32
AF = mybir.ActivationFunctionType
ALU = mybir.AluOpType
AX = mybir.AxisListType
EPS = 1e-6
TB = 512
NSLOT = 8192


class Stop(Exception):
    pass


class Buf:
    __slots__ = ("w", "r")

    def __init__(self):
        self.w = None
        self.r = []


class Ctx:
    def __init__(self, nc, es):
        self.nc = nc
        self.eng = dict(pe=nc.tensor, act=nc.scalar, dve=nc.vector, pool=nc.gpsimd, sp=nc.sync)
        self.sem = {e: es.enter_context(nc.semaphore("s_" + e)) for e in self.eng}
        self.cnt = {e: 0 for e in self.eng}
        self.seen = {e: {} for e in self.eng}
        self.dsem = {q: [es.enter_context(nc.semaphore("d_%s%d" % (q, i))) for i in range(8)] for q in ("sp", "pool")}
        self.dcnt = {q: [0] * 8 for q in ("sp", "pool")}
        self.dnext = {"sp": 0, "pool": 0}
        self.alltoks = []
        self.dead = False

    def wait(self, e, toks):
        best = {}
        for t in toks:
            if t is None:
                continue
            sem, val, key = t
            if e == "pe" and key == "s_pe":
                continue
            if self.seen[e].get(key, 0) >= val:
                continue
            if key not in best or best[key][1] < val:
                best[key] = t
        for key, (sem, val, _) in best.items():
            self.eng[e].wait_ge(sem, val)
            self.seen[e][key] = val

    def _deps(self, reads, writes):
        toks = []
        for b in reads:
            toks.append(b.w)
        for b in writes:
            toks.append(b.w)
            toks.extend(b.r)
        return toks

    def _commit(self, tok, reads, writes):
        for b in reads:
            b.r.append(tok)
        for b in writes:
            b.w = tok
            b.r = []

    def op(self, e, fn, reads=(), writes=()):
        if self.dead:
            return None
        self.wait(e, self._deps(reads, writes))
        inst = fn(self.eng[e])
        self.cnt[e] += 1
        inst.then_inc(self.sem[e], 1)
        tok = (self.sem[e], self.cnt[e], "s_" + e)
        self._commit(tok, reads, writes)
        return tok

    def pe(self, fns, reads=(), writes=()):
        if self.dead:
            return None
        self.wait("pe", self._deps(reads, writes))
        inst = None
        for fn in fns:
            inst = fn(self.nc.tensor)
        self.cnt["pe"] += 1
        inst.then_inc(self.sem["pe"], 1)
        tok = (self.sem["pe"], self.cnt["pe"], "s_pe")
        self._commit(tok, reads, writes)
        return tok

    def dma(self, q, fn, reads=(), writes=()):
        if self.dead:
            return None
        i = self.dnext[q]
        self.dnext[q] = (i + 1) % 8
        sem = self.dsem[q][i]
        key = "d_%s%d" % (q, i)
        toks = self._deps(reads, writes)
        if self.dcnt[q][i] > 0:
            toks.append((sem, self.dcnt[q][i], key))
        self.wait(q, toks)
        inst = fn(self.eng[q])
        self.dcnt[q][i] += 16
        inst.then_inc(sem, 16)
        tok = (sem, self.dcnt[q][i], key)
        self._commit(tok, reads, writes)
        self.alltoks.append(tok)
        return tok

    def barrier(self):
        if self.dead:
            return
        toks = [(self.sem[e], self.cnt[e], "s_" + e) for e in self.eng if self.cnt[e] > 0]
        for q in ("sp", "pool"):
            for i in range(8):
                if self.dcnt[q][i] > 0:
                    toks.append((self.dsem[q][i], self.dcnt[q][i], "d_%s%d" % (q, i)))
        for e in self.eng:
            self.wait(e, [t for t in toks if not (e == "pe" and t[2] == "s_pe")])


def dims(cfg):
    D = cfg["D"]
    d = dict(D=D, NB=cfg["NB"], NC=cfg["NC"], KC=D // 128, TOK=cfg["NB"] * TB, PW=D // 2, PG=D // 8,
             HK=D // 2, NH=D // 256, XH=4, XD=D // 4, XC=D // 512, MEM=256, NE=32, DE=D // 4, DC=D // 512)
    d["NVB"] = d["NC"] * d["NB"]
    return d


def build(cfg):
    g = dims(cfg)
    D, NB, NC, KC, TOK, PW, PG, HK, NH = g["D"], g["NB"], g["NC"], g["KC"], g["TOK"], g["PW"], g["PG"], g["HK"], g["NH"]
    XH, XD, XC, MEM, NE, DE, DC, NVB = g["XH"], g["XD"], g["XC"], g["MEM"], g["NE"], g["DE"], g["DC"], g["NVB"]
    nc = bass.Bass("TRN2", target_bir_lowering=False)
    nc.dge_precook = False

    def din(name, shape, dt=F32):
        return nc.dram_tensor(name, shape, dt, kind="ExternalInput").ap()

    def dsc(name, shape, dt=F32, **kw):
        return nc.dram_tensor(name, shape, dt, **kw).ap()

    xh = din("xh", [NVB, 528, D])
    mem = din("mem", [MEM, D])
    gains = {n: din(n, [1, D]) for n in ("g_mix", "g_xat", "g_mem", "g_moe", "g_fin")}
    w_in = din("w_in", [D, 3 * D], F32R)
    pool_w = din("pool_w", [4, PG, PG], F32R)
    psc_d = din("psc", [128, PW // 128])
    lbp_d = din("lbp", [128, 2, 2, NH])
    hgn_d = din("hgn", [128, NH])
    w_out = din("w_out", [D, D], F32R)
    wq = din("wq", [D, D], F32R)
    wk = din("wk", [D, D], F32R)
    wv = din("wv", [D, D], F32R)
    wo = din("wo", [D, D], F32R)
    wr_d = din("wr", [D, 36])
    w1 = din("w1", [NE, D, DE], F32R)
    w3 = din("w3", [NE, D, DE], F32R)
    w2 = din("w2", [NE, DE, D], F32R)
    ident_d = din("ident", [128, 128])
    maskf_d = din("maskf", [128, 512])
    maskb_d = din("maskb", [128, 512])
    rmask_d = din("rmask", [128, 512])
    ones_d = din("ones", [128, 128], F32R)
    sl_d = din("sl", [128, 128])
    invcnt_d = din("invcnt", [NB, 4 * 512])
    cmask_d = din("cmask", [NB, 2 * NVB])
    ebase_d = din("ebase", [1, 32])
    out_d = nc.dram_tensor("out", [TOK, D], F32, kind="ExternalOutput").ap()

    mixT = dsc("mixT", [D, TOK], F32R)
    olocT = dsc("olocT", [HK, TOK])
    gsT = dsc("gsT", [HK, TOK])
    qdgT = dsc("qdgT", [2, HK, TOK], F32R)
    CCR = NVB * 2 * NH * 128
    Gd = dsc("Gd", [CCR, 129])
    x1_d = dsc("x1s", [TOK, D])
    qT_d = dsc("qTs", [D, TOK], F32R)
    kT_d = dsc("kTs", [D, MEM], F32R)
    v_d = dsc("vs", [MEM, D], F32R)
    oT_d = dsc("oTs", [D, TOK], F32R)
    x2_d = dsc("x2s", [TOK, D])
    Xs_d = dsc("Xs", [NSLOT, D])
    Y_d = dsc("Ys", [NSLOT, D])

    with contextlib.ExitStack() as es:
        cx = Ctx(nc, es)
        cc_sem = es.enter_context(nc.semaphore("cc_sem"))

        uid = [0]

        def sb(st, name, shape, dt=F32):
            uid[0] += 1
            return st.enter_context(nc.sbuf_tensor("%s_u%d" % (name, uid[0]), shape, dt))

        psb = [es.enter_context(nc.psum_tensor("ps%d" % i, [128, 512], F32)) for i in range(8)]
        psB = [Buf() for _ in range(8)]
        pstate = [0]

        def pbank():
            i = pstate[0]
            pstate[0] = (i + 1) % 6
            return psb[i], psB[i]

        ident = sb(es, "ident", [128, 128])
        ones = sb(es, "ones", [128, 128], F32R)
        small = sb(es, "small", [128, 64])
        idx_all = sb(es, "idx_all", [128, 2 * NB * 4], I32)
        cw_all = sb(es, "cw_all", [128, 2 * NB * 4])
        Mall = sb(es, "Mall", [128, NB * 4, 32])
        cB = Buf()
        cx.dma("sp", lambda q: q.dma_start(out=ident[:], in_=ident_d), writes=[cB])
        cx.dma("sp", lambda q: q.dma_start(out=ones[:], in_=ones_d), writes=[cB])
        idxB, cwB, MallB = Buf(), Buf(), Buf()

        def bl(x):
            return list(x) if isinstance(x, (list, tuple)) else [x]

        def load_w(dst, src_ap, B, extra_writes=()):
            return cx.dma("sp", lambda q: q.dma_start(out=dst, in_=src_ap.rearrange("(kc p) c -> p kc c", p=128)),
                          writes=bl(B) + list(extra_writes))

        def rstd_from_ss(ss, n, B, scale):
            cx.op("dve", lambda e: e.tensor_scalar(ss[0:n], ss[0:n], scale, EPS, op0=ALU.mult, op1=ALU.add), reads=[B], writes=[B])
            cx.op("act", lambda e: e.activation(out=ss[0:n], in_=ss[0:n], func=AF.Sqrt), reads=[B], writes=[B])
            cx.op("dve", lambda e: e.reciprocal(ss[0:n], ss[0:n]), reads=[B], writes=[B])

        def norm_transpose(st_name, row_src, tiles, gain_d, hT, hTB, out_dt_rows=None, keep_rows=None):
            with contextlib.ExitStack() as st:
                gbc = sb(st, st_name + "gbc", [128, D])
                xt = [sb(st, st_name + "xt%d" % i, [128, D]) for i in range(2)]
                xs = [sb(st, st_name + "xs%d" % i, [128, D]) for i in range(2)]
                junk = sb(st, st_name + "junk", [128, D])
                ss = sb(st, st_name + "ss", [128, 8])
                gB, jB = Buf(), Buf()
                xtB = [Buf(), Buf()]
                xsB = [Buf(), Buf()]
                ssB = [Buf() for _ in range(8)]
                cx.dma("sp", lambda q: q.dma_start(out=gbc[:], in_=gain_d[0].partition_broadcast(128)), writes=[gB])
                for i, (r0, n, c0) in enumerate(tiles):
                    a = i % 2
                    s1 = ss[:, (i % 8):(i % 8) + 1]
                    sB = ssB[i % 8]
                    cx.dma("sp", lambda q: q.dma_start(out=xt[a][0:n], in_=row_src(r0, n)), writes=[xtB[a]])
                    cx.op("act", lambda e: e.activation(out=junk[0:n], in_=xt[a][0:n], func=AF.Square, accum_out=s1[0:n]),
                          reads=[xtB[a]], writes=[jB, sB])
                    rstd_from_ss(s1, n, sB, 1.0 / D)
                    cx.op("dve", lambda e: e.scalar_tensor_tensor(out=xs[a][0:n], in0=xt[a][0:n], scalar=s1[0:n], in1=gbc[0:n],
                                                                  op0=ALU.mult, op1=ALU.mult),
                          reads=[xtB[a], sB, gB], writes=[xsB[a]])
                    if keep_rows is not None:
                        keep_rows(i, xs[a], xsB[a], n)
                    if hT is None:
                        continue
                    for k0 in range(0, KC, 4):
                        pt, pB = pbank()
                        kk = min(4, KC - k0)
                        cx.pe([(lambda t, c=c: t.transpose(out=pt[:, c * 128:c * 128 + n], in_=xs[a][0:n, (k0 + c) * 128:(k0 + c + 1) * 128],
                                                           identity=ident[0:n, 0:n])) for c in range(kk)],
                              reads=[xsB[a], cB], writes=[pB])
                        src = pt[:, 0:kk * 128].rearrange("p (c n) -> p c n", c=kk)[:, :, 0:n]
                        dst = hT[:, k0:k0 + kk, c0:c0 + n]
                        if (k0 // 4) % 2 == 0:
                            cx.op("act", lambda e: e.activation(out=dst, in_=src, func=AF.Copy), reads=[pB], writes=[hTB])
                        else:
                            cx.op("dve", lambda e: e.tensor_copy(dst, src), reads=[pB], writes=[hTB])
                cx.barrier()

        def gemm_fm(AT, ATB, ntok, W_ap, ncols, epilogue, wbufs, wB, tag=[0]):
            kc_n = AT.shape[1]
            WB = wbufs[0].shape[2]
            for c0 in range(0, ncols, WB):
                a = tag[0] % len(wbufs)
                tag[0] += 1
                wcur = min(WB, ncols - c0)
                load_w(wbufs[a][:, 0:kc_n, 0:wcur], W_ap[:, c0:c0 + wcur], wB[a])
                for c1 in range(0, wcur, 128):
                    for t0 in range(0, ntok, 512):
                        tn = min(512, ntok - t0)
                        pt, pB = pbank()
                        cx.pe([(lambda t, k=k: t.matmul(pt[:, 0:tn], wbufs[a][:, k, c1:c1 + 128], AT[:, k, t0:t0 + tn],
                                                        start=(k == 0), stop=(k == kc_n - 1))) for k in range(kc_n)],
                              reads=bl(wB[a]) + [ATB], writes=[pB])
                        epilogue((c0 + c1) // 128, pt, pB, t0, tn)

        def gemm_tm(AT, ATB, ntok, W_ap, ncols, epilogue, wbufs, wB, tag=[0]):
            kc_n = AT.shape[1]
            WB = wbufs[0].shape[2]
            for c0 in range(0, ncols, WB):
                a = tag[0] % len(wbufs)
                tag[0] += 1
                wcur = min(WB, ncols - c0)
                load_w(wbufs[a][:, 0:kc_n, 0:wcur], W_ap[:, c0:c0 + wcur], wB[a])
                for t0 in range(0, ntok, 128):
                    pt, pB = pbank()
                    cx.pe([(lambda t, k=k: t.matmul(pt[:, 0:wcur], AT[:, k, t0:t0 + 128], wbufs[a][:, k, 0:wcur],
                                                    start=(k == 0), stop=(k == kc_n - 1))) for k in range(kc_n)],
                          reads=bl(wB[a]) + [ATB], writes=[pB])
                    epilogue(c0, wcur, t0 // 128, pt, pB)

        def chk(n):
            if cfg.get("stop", 99) == n:
                cx.barrier()
                cx.dead = True

        def _body():
            for b in range(NVB):
                own = b < NB
                tsl = slice(b * TB, (b + 1) * TB) if own else None
                with contextlib.ExitStack() as st:
                    hT = sb(st, "hT", [128, KC, 528], F32R)
                    hTB = Buf()
                    tiles = [(r, 128, r) for r in range(0, 512, 128)] + [(512, 16, 512)]
                    norm_transpose("n1", lambda r0, n: xh[b, r0:r0 + n, :], tiles, gains["g_mix"], hT, hTB)
                    chk(1)
                    with contextlib.ExitStack() as st2:
                        wbig = sb(st2, "wA", [128, KC, 512], F32R)
                        wslot = [wbig[:, :, j * 128:(j + 1) * 128] for j in range(4)]
                        wsB = [Buf() for _ in range(4)]
                        wbufs = [wbig[:, :, 0:256], wbig[:, :, 256:512]]
                        wB = [[wsB[0], wsB[1]], [wsB[2], wsB[3]]]
                        wtagA = [0]
                        wtagS = [0]
                        with contextlib.ExitStack() as st3:
                          if own:
                            inv = sb(st3, "inv", [128, 4 * 512])
                            psc = sb(st3, "psc", [128, PW // 128])
                            pw = sb(st3, "pw", [128, PG // 128, PG], F32R)
                            uT = [sb(st3, "uT%d" % i, [128, 528]) for i in range(2)]
                            pa = [sb(st3, "pa%d" % i, [128, 528]) for i in range(2)]
                            pb_ = [sb(st3, "pb%d" % i, [128, 528]) for i in range(2)]
                            miT = sb(st3, "miT", [128, PG // 128, 512], F32R)
                            yp = [sb(st3, "yp%d" % i, [128, 512], F32R) for i in range(2)]
                            invB, pscB, pwB, miB = Buf(), Buf(), Buf(), Buf()
                            uB, paB, pbB, ypB = [Buf(), Buf()], [Buf(), Buf()], [Buf(), Buf()], [Buf(), Buf()]
                            cx.dma("sp", lambda q: q.dma_start(out=inv[:], in_=invcnt_d[b].partition_broadcast(128)), writes=[invB])
                            cx.dma("sp", lambda q: q.dma_start(out=psc[:], in_=psc_d), writes=[pscB])
                            cnt = [0]
                            for gi, w in enumerate((2, 4, 8, 16)):
                                cx.dma("sp", lambda q: q.dma_start(out=pw[:], in_=pool_w[gi].rearrange("(kc p) c -> p kc c", p=128)),
                                       writes=[pwB])

                                def ep_pool(cb, pt, pB, t0, tn, gi=gi, w=w):
                                    a = (cnt[0] // 2) % 2
                                    cnt[0] += 1
                                    if tn == 512:
                                        cx.op("act", lambda e: e.activation(out=uT[a][:, 0:512], in_=pt[:, 0:512], func=AF.Copy),
                                              reads=[pB], writes=[uB[a]])
                                        return
                                    cx.op("act", lambda e: e.activation(out=uT[a][:, 512:528], in_=pt[:, 0:16], func=AF.Copy),
                                          reads=[pB], writes=[uB[a]])
                                    cur, curB, m = uT[a], uB[a], 1
                                    alt = [(pa[a], paB[a]), (pb_[a], pbB[a])]
                                    k = 0
                                    while m < w // 2:
                                        nx, nxB = alt[k % 2]
                                        k += 1
                                        cx.op("pool", lambda e, nx=nx, cur=cur, m=m: e.tensor_tensor(out=nx[:, 2 * m - 1:528], in0=cur[:, 2 * m - 1:528],
                                                                                                     in1=cur[:, m - 1:528 - m], op=ALU.add),
                                              reads=[curB], writes=[nxB])
                                        cur, curB, m = nx, nxB, 2 * m
                                    h = w // 2
                                    nx, nxB = alt[k % 2]
                                    cx.op("dve", lambda e: e.tensor_tensor(out=nx[:, 8:520], in0=cur[:, 7:519], in1=cur[:, 7 + h:519 + h], op=ALU.add),
                                          reads=[curB], writes=[nxB])
                                    cx.op("dve", lambda e: e.tensor_tensor(out=nx[:, 8:520], in0=nx[:, 8:520], in1=inv[:, gi * 512:(gi + 1) * 512], op=ALU.mult),
                                          reads=[nxB, invB], writes=[nxB])
                                    cbl = cb
                                    cx.op("dve", lambda e: e.tensor_tensor(out=miT[:, cbl, :], in0=nx[:, 8:520], in1=uT[a][:, 8:520], op=ALU.subtract),
                                          reads=[nxB, uB[a]], writes=[miB])

                                gemm_fm(hT, hTB, 528, w_in[:, gi * PG:(gi + 1) * PG], PG, ep_pool, wbufs, wB, wtagA)
                                for db in range(PG // 128):
                                    pt, pB = pbank()
                                    ncb = PG // 128
                                    cx.pe([(lambda t, c=c: t.matmul(pt[:, :], pw[:, c, db * 128:(db + 1) * 128], miT[:, c, :],
                                                                    start=(c == 0), stop=(c == ncb - 1))) for c in range(ncb)],
                                          reads=[pwB, miB], writes=[pB])
                                    a = db % 2
                                    col = gi * (PG // 128) + db
                                    cx.op("dve", lambda e: e.tensor_scalar(yp[a][:], pt[:, :], psc[:, col:col + 1], None, op0=ALU.mult),
                                          reads=[pB, pscB], writes=[ypB[a]])
                                    r0 = gi * PG + db * 128
                                    cx.dma("pool", lambda q: q.dma_start(out=mixT[r0:r0 + 128, tsl], in_=yp[a][:]), reads=[ypB[a]])
                            cx.barrier()
                        chk(2)
                        with contextlib.ExitStack() as st3:
                            maskf = sb(st3, "maskf", [128, 512])
                            maskb = sb(st3, "maskb", [128, 512])
                            rmask = sb(st3, "rmask", [128, 512])
                            lbp = sb(st3, "lbp", [128, 2, 2, NH])
                            lb = sb(st3, "lb", [128, 2, NH])
                            oml = sb(st3, "oml", [128, 2, NH])
                            hgn = sb(st3, "hgn", [128, NH])
                            ones8 = sb(st3, "ones8", [128, 8])
                            kB = Buf()
                            for t_, d_ in ((maskf, maskf_d), (maskb, maskb_d), (rmask, rmask_d), (lbp, lbp_d), (hgn, hgn_d)):
                                cx.dma("sp", lambda q, t_=t_, d_=d_: q.dma_start(out=t_[:], in_=d_), writes=[kB])
                            cx.op("dve", lambda e: e.memset(ones8[:], 1.0), writes=[kB])
                            cx.op("dve", lambda e: e.tensor_tensor(out=lb[:], in0=lbp[:, :, 0, :], in1=lbp[:, :, 1, :], op=ALU.subtract), reads=[kB], writes=[kB])
                            cx.op("act", lambda e: e.activation(out=lb[:], in_=lb[:], func=AF.Sigmoid), reads=[kB], writes=[kB])
                            cx.op("dve", lambda e: e.tensor_scalar(oml[:], lb[:], -1.0, 1.0, op0=ALU.mult, op1=ALU.add), reads=[kB], writes=[kB])
                            NS = 2
                            names = ["qs", "t1", "t2", "t3", "t4", "t5", "t6"]
                            T = [{n: sb(st3, "%s_%d" % (n, s), [128, 512]) for n in names} for s in range(NS)]
                            TR = [{n: sb(st3, "%s_%d" % (n, s), [128, 512], F32R) for n in ("qd", "kd", "qg")} for s in range(NS)]
                            kend = [sb(st3, "kend%d" % s, [128, 2, 4, 128], F32R) for s in range(NS)]
                            Sst = [sb(st3, "Sst%d" % s, [128, 9, 128], F32R) for s in range(NS)]
                            vt = [sb(st3, "vt%d" % s, [128, 4, 256], F32R) for s in range(2)]
                            sm = [sb(st3, "sm%d" % s, [128, 40]) for s in range(NS)]
                            TBf = [{n: Buf() for n in names + ["qd", "kd", "qg", "kend", "S", "sm"]} for s in range(NS)]
                            for s_ in range(NS):
                                for al, tg in (("gs", "t6"), ("ke", "t2"), ("ol", "t2")):
                                    T[s_][al] = T[s_][tg]
                                    TBf[s_][al] = TBf[s_][tg]
                                TR[s_]["AT"] = TR[s_]["qg"]
                                TBf[s_]["AT"] = TBf[s_]["qg"]
                            vtB = [Buf(), Buf()]
                            psum_hold = {}

                            def ep_hold(key):
                                def ep(cb, pt, pB, t0, tn):
                                    psum_hold[(key, cb)] = (pt, pB)
                                return ep

                            for hp in range(NH // 2):
                                va = hp % 2

                                def ep_v(c0, wcur, ti, pt, pB, va=va):
                                    cx.op("act", lambda e: e.activation(out=vt[va][:, ti, :], in_=pt[:, 0:256], func=AF.Copy), reads=[pB], writes=[vtB[va]])
                                gemm_tm(hT[:, :, 8:520], hTB, 512, w_in[:, PW + 3 * HK + hp * 256:PW + 3 * HK + (hp + 1) * 256], 256, ep_v, wbufs, wB, wtagA)
                                for hh in range(2):
                                    h = hp * 2 + hh
                                    s = h % NS
                                    tt, tr, bf = T[s], TR[s], TBf[s]
                                    hs = slice(h * 128, (h + 1) * 128)
                                    psum_hold.clear()

                                    def proj(region):
                                        c = PW + region * HK + h * 128
                                        res = {}

                                        def ep(cb, pt, pB, t0, tn):
                                            res["p"] = (pt, pB)
                                        a = wtagS[0] % 4
                                        wtagS[0] += 1
                                        load_w(wslot[a], w_in[:, c:c + 128], wsB[a])
                                        pt, pB = pbank()
                                        cx.pe([(lambda t, k=k: t.matmul(pt[:, :], wslot[a][:, k, :], hT[:, k, 8:520], start=(k == 0), stop=(k == KC - 1)))
                                               for k in range(KC)], reads=[wsB[a], hTB], writes=[pB])
                                        return pt, pB
                                    _sv = cx.dead; cx.dead = _sv or not own
                                    pt, pB = proj(0)
                                    cx.op("act", lambda e: e.activation(out=tt["qs"][:], in_=pt[:, :], func=AF.Silu), reads=[pB], writes=[bf["qs"]])
                                    pt, pB = proj(4)
                                    cx.op("act", lambda e: e.activation(out=tt["gs"][:], in_=pt[:, :], func=AF.Silu), reads=[pB], writes=[bf["gs"]])
                                    cx.op("dve", lambda e: e.tensor_scalar(tt["gs"][:], tt["gs"][:], hgn[:, h:h + 1], None, op0=ALU.mult),
                                          reads=[bf["gs"], kB], writes=[bf["gs"]])
                                    cx.dma("pool", lambda q: q.dma_start(out=gsT[hs, tsl], in_=tt["gs"][:]), reads=[bf["gs"]])
                                    cx.dead = _sv
                                    ot, oB = psb[6 + h % 2], psB[6 + h % 2]
                                    first_o = [True]
                                    for di in range(2):
                                        fwd = di == 0
                                        pt, pB = proj(1 + di)
                                        t1, t2, t3, t4, t5, t6, ke = (tt[n] for n in ("t1", "t2", "t3", "t4", "t5", "t6", "ke"))
                                        smt = sm[s]
                                        tot = t3[:, 63::64]
                                        inc8, ex8, dec, dt1 = smt[:, 0:8], smt[:, 8:16], smt[:, 16:24], smt[:, 24:25]
                                        cx.op("act", lambda e: e.activation(out=t1[:], in_=pt[:, :], func=AF.Sigmoid), reads=[pB], writes=[bf["t1"]])
                                        cx.op("dve", lambda e: e.tensor_scalar(t1[:], t1[:], oml[:, di, h:h + 1], lb[:, di, h:h + 1], op0=ALU.mult, op1=ALU.add),
                                              reads=[bf["t1"], kB], writes=[bf["t1"]])
                                        cx.op("act", lambda e: e.activation(out=t2[:], in_=t1[:], func=AF.Ln), reads=[bf["t1"]], writes=[bf["t2"]])
                                        cx.op("pool", lambda e: e.tensor_scalar(t1[:], t1[:], -1.0, 1.0, op0=ALU.mult, op1=ALU.add),
                                              reads=[bf["t1"]], writes=[bf["t1"]])
                                        cx.op("dve", lambda e: e.tensor_tensor_scan(out=t3[:], data0=rmask[:], data1=t2[:], initial=0.0, op0=ALU.mult, op1=ALU.add),
                                              reads=[bf["t2"], kB], writes=[bf["t3"]])
                                        if fwd:
                                            bb, bbB = t3, bf["t3"]
                                        else:
                                            cx.op("pool", lambda e: e.tensor_tensor(out=t4[:], in0=t2[:], in1=t3[:], op=ALU.subtract),
                                                  reads=[bf["t2"], bf["t3"]], writes=[bf["t4"]])
                                            cx.op("dve", lambda e: e.tensor_tensor(out=t4[:].rearrange("p (c n) -> p c n", c=8),
                                                                                   in0=t4[:].rearrange("p (c n) -> p c n", c=8),
                                                                                   in1=tot.unsqueeze(2).to_broadcast([128, 8, 64]), op=ALU.add),
                                                  reads=[bf["t4"], bf["t3"]], writes=[bf["t4"]])
                                            bb, bbB = t4, bf["t4"]
                                        cx.op("act", lambda e: e.activation(out=dec, in_=tot, func=AF.Exp), reads=[bf["t3"]], writes=[bf["sm"]])
                                        _sv = cx.dead; cx.dead = _sv or not own
                                        cx.op("act", lambda e: e.activation(out=t5[:], in_=bb[:], func=AF.Exp), reads=[bbB], writes=[bf["t5"]])
                                        cx.dead = _sv
                                        cx.op("act", lambda e: e.activation(out=t6[:], in_=bb[:], func=AF.Exp, scale=-1.0), reads=[bbB], writes=[bf["t6"]])
                                        _sv = cx.dead; cx.dead = _sv or not own
                                        cx.op("dve", lambda e: e.tensor_tensor(out=tr["qd"][:], in0=tt["qs"][:], in1=t5[:], op=ALU.mult),
                                              reads=[bf["qs"], bf["t5"]], writes=[bf["qd"]])
                                        cx.dead = _sv
                                        cx.op("dve", lambda e: e.tensor_tensor(out=tr["kd"][:], in0=t1[:], in1=t6[:], op=ALU.mult),
                                              reads=[bf["t1"], bf["t6"]], writes=[bf["kd"]])
                                        cx.op("dve", lambda e: e.tensor_tensor(out=ke[:].rearrange("p (c n) -> p c n", c=8),
                                                                               in0=tr["kd"][:].bitcast(F32).rearrange("p (c n) -> p c n", c=8),
                                                                               in1=dec.unsqueeze(2).to_broadcast([128, 8, 64]), op=ALU.mult),
                                              reads=[bf["kd"], bf["sm"]], writes=[bf["ke"]])
                                        cx.op("dve", lambda e: e.tensor_tensor_scan(out=inc8, data0=ones8[:], data1=tot, initial=0.0, op0=ALU.mult, op1=ALU.add),
                                              reads=[bf["t3"], kB, bf["sm"]], writes=[bf["sm"]])
                                        cx.op("act", lambda e: e.activation(out=dt1, in_=inc8[:, 7:8], func=AF.Exp), reads=[bf["sm"]], writes=[bf["sm"]])
                                        _sv = cx.dead; cx.dead = _sv or not own
                                        if fwd:
                                            cx.op("dve", lambda e: e.tensor_tensor(out=ex8, in0=inc8, in1=tot, op=ALU.subtract), reads=[bf["sm"], bf["t3"]], writes=[bf["sm"]])
                                        else:
                                            cx.op("dve", lambda e: e.tensor_scalar(ex8, inc8, -1.0, inc8[:, 7:8], op0=ALU.mult, op1=ALU.add), reads=[bf["sm"]], writes=[bf["sm"]])
                                        cx.op("act", lambda e: e.activation(out=ex8, in_=ex8, func=AF.Exp), reads=[bf["sm"]], writes=[bf["sm"]])
                                        cx.op("dve", lambda e: e.tensor_tensor(out=tr["qg"][:].rearrange("p (c n) -> p c n", c=8),
                                                                                in0=tr["qd"][:].bitcast(F32).rearrange("p (c n) -> p c n", c=8),
                                                                                in1=ex8.unsqueeze(2).to_broadcast([128, 8, 64]), op=ALU.mult),
                                              reads=[bf["qd"], bf["sm"]], writes=[bf["qg"]])
                                        cx.dma("pool", lambda q: q.dma_start(out=qdgT[di, hs, tsl], in_=tr["qg"][:]), reads=[bf["qg"]])
                                        cx.dead = _sv
                                        chk(21)
                                        kp, kpB = pbank()
                                        cx.pe([(lambda t, p=p: t.transpose(out=kp[:, p * 128:(p + 1) * 128], in_=ke[:, p * 128:(p + 1) * 128], identity=ident[:]))
                                               for p in range(4)], reads=[bf["ke"], cB], writes=[kpB])
                                        cx.op("dve", lambda e: e.tensor_scalar(kend[s][:, 0].rearrange("p a b -> p (a b)"), kp[:, :], maskf[:, 63:64], None, op0=ALU.mult),
                                              reads=[kpB, kB], writes=[bf["kend"]])
                                        cx.op("dve", lambda e: e.tensor_scalar(kend[s][:, 1].rearrange("p a b -> p (a b)"), kp[:, :], maskb[:, 64:65], None, op0=ALU.mult),
                                              reads=[kpB, kB, bf["kend"]], writes=[bf["kend"]])
                                        chk(22)
                                        _sv = cx.dead; cx.dead = _sv or not own
                                        ap_, apB = pbank()
                                        cx.pe([(lambda t, p=p: t.matmul(ap_[:, p * 128:(p + 1) * 128], tr["kd"][:, p * 128:(p + 1) * 128],
                                                                        tr["qd"][:, p * 128:(p + 1) * 128], start=True, stop=True)) for p in range(4)],
                                              reads=[bf["kd"], bf["qd"]], writes=[apB])
                                        mk = maskf if fwd else maskb
                                        cx.op("dve", lambda e: e.tensor_tensor(out=tr["AT"][:], in0=ap_[:, :], in1=mk[:], op=ALU.mult), reads=[apB, kB], writes=[bf["AT"]])
                                        chk(23)
                                        vv = vt[va]
                                        vcs = slice(hh * 128, (hh + 1) * 128)
                                        fns = []
                                        for p in range(4):
                                            fns.append(lambda t, p=p, st_=first_o[0] and p == 0: t.matmul(ot[:, p * 128:(p + 1) * 128], vv[:, p, vcs],
                                                                                                          tr["AT"][:, p * 128:(p + 1) * 128], start=st_, stop=False,
                                                                                                          skip_group_check=True))
                                        first_o[0] = False
                                        cx.pe(fns, reads=[vtB[va], bf["AT"]], writes=[oB])
                                        cx.dead = _sv
                                        chk(24)
                                        dps = []
                                        for half in range(2):
                                            dp, dpB = pbank()
                                            dps.append((dp, dpB))
                                            fns = []
                                            for j in range(4):
                                                n = half * 4 + j
                                                p, off = n // 2, (n % 2) * 64
                                                fns.append(lambda t, j=j, p=p, off=off: t.matmul(dp[:, j * 128:(j + 1) * 128], kend[s][:, off // 64, p, :],
                                                                                                 vv[:, p, vcs], start=True, stop=True))
                                            cx.pe(fns, reads=[bf["kend"], vtB[va]], writes=[dpB])
                                        chk(25)
                                        S = Sst[s]
                                        order = list(range(8)) if fwd else list(range(7, -1, -1))
                                        cx.op("pool", lambda e: e.memset(S[:, 0, :].bitcast(F32), 0.0), writes=[bf["S"]])
                                        for step, n in enumerate(order):
                                            dp, dpB = dps[n // 4]
                                            cx.op("dve", lambda e, step=step, n=n, dp=dp: e.scalar_tensor_tensor(
                                                out=S[:, step + 1, :], in0=S[:, step, :].bitcast(F32), scalar=dec[:, n:n + 1],
                                                in1=dp[:, (n % 4) * 128:(n % 4 + 1) * 128], op0=ALU.mult, op1=ALU.add),
                                                reads=[bf["S"], bf["sm"], dpB], writes=[bf["S"]])
                                            if step < 7 and own:
                                                n2 = order[step + 1]
                                                cx.pe([lambda t, step=step, n2=n2: t.matmul(ot[:, n2 * 64:(n2 + 1) * 64], S[:, step + 1, :],
                                                                                            tr["qd"][:, n2 * 64:(n2 + 1) * 64], start=False, stop=False,
                                                                                            skip_group_check=True)],
                                                      reads=[bf["S"], bf["qd"]], writes=[oB])
                                        chk(26)
                                        row0 = ((b * 2 + di) * NH + h) * 128
                                        cx.dma("pool", lambda q: q.dma_start(out=Gd[row0:row0 + 128, 0:128], in_=S[:, 8, :].bitcast(F32)), reads=[bf["S"]])
                                        cx.dma("pool", lambda q: q.dma_start(out=Gd[row0:row0 + 128, 128:129], in_=dt1, allow_slow_non_contiguous=True), reads=[bf["sm"]])
                                    _sv = cx.dead; cx.dead = _sv or not own
                                    cx.op("act", lambda e: e.activation(out=tt["ol"][:], in_=ot[:, :], func=AF.Copy), reads=[oB], writes=[bf["ol"]])
                                    cx.dma("pool", lambda q: q.dma_start(out=olocT[hs, tsl], in_=tt["ol"][:]), reads=[bf["ol"]])
                                    cx.dead = _sv
                            cx.barrier()
                    cx.barrier()

            chk(3)
            cx.barrier()
            chk(4)
            with contextlib.ExitStack() as st:
                memT = sb(st, "memT", [128, KC, MEM], F32R)
                mB = Buf()
                norm_transpose("nm", lambda r0, n: mem[r0:r0 + n, :], [(0, 128, 0), (128, 128, 128)], gains["g_mem"], memT, mB)
                wbufs = [sb(st, "wK%d" % i, [128, KC, 256], F32R) for i in range(2)]
                wB = [Buf(), Buf()]
                kv = [sb(st, "kv%d" % i, [128, 256], F32R) for i in range(2)]
                kvB = [Buf(), Buf()]
                ci = [0]

                def ep_k(cb, pt, pB, t0, tn):
                    a = ci[0] % 2
                    ci[0] += 1
                    cx.op("act", lambda e: e.activation(out=kv[a][:], in_=pt[:, 0:256], func=AF.Copy), reads=[pB], writes=[kvB[a]])
                    cx.dma("pool", lambda q: q.dma_start(out=kT_d[cb * 128:(cb + 1) * 128, :], in_=kv[a][:]), reads=[kvB[a]])
                gemm_fm(memT, mB, MEM, wk, D, ep_k, wbufs, wB)

                def ep_vm(c0, wcur, ti, pt, pB):
                    a = ci[0] % 2
                    ci[0] += 1
                    cx.op("dve", lambda e: e.tensor_copy(kv[a][:], pt[:, 0:256]), reads=[pB], writes=[kvB[a]])
                    cx.dma("pool", lambda q: q.dma_start(out=v_d[ti * 128:(ti + 1) * 128, c0:c0 + 256], in_=kv[a][:]), reads=[kvB[a]])
                gemm_tm(memT, mB, MEM, wv, D, ep_vm, wbufs, wB)
                cx.barrier()

            chk(5)
            with contextlib.ExitStack() as st:
                cmk = sb(st, "cmk", [128, NB, 2 * NVB])
                G = [sb(st, "G%d" % i, [128, NVB * 2, 129]) for i in range(1)]
                acf = sb(st, "acf", [128, NVB])
                msk = sb(st, "msk", [128, 128])
                Sin = [sb(st, "Sin%d" % i, [128, 128]) for i in range(2)]
                SinR = [sb(st, "SinR%d" % i, [128, 128], F32R) for i in range(2)]
                qg = [sb(st, "qgl%d" % i, [128, 512], F32R) for i in range(2)]
                ol = sb(st, "oll", [128, 512])
                gsl = sb(st, "gsl", [128, 512])
                sq = sb(st, "sq", [128, 512], F32R)
                rs = sb(st, "rs", [128, 512])
                yo = sb(st, "yo", [128, 512], F32R)
                cmB, GB, acB, mskB, olB, gslB, sqB, rsB, yoB = (Buf() for _ in range(9))
                SinB, SinRB, qgB = [Buf(), Buf()], [Buf(), Buf()], [Buf(), Buf()]
                cx.dma("sp", lambda q: q.dma_start(out=cmk[:].rearrange("p a b -> p (a b)"),
                                                   in_=cmask_d.rearrange("a b -> (a b)").partition_broadcast(128)), writes=[cmB])
                gdv = Gd.rearrange("(x h k) c -> k x h c", h=NH, k=128)
                for h in range(NH):
                    cx.dma("sp", lambda q: q.dma_start(out=G[0][:], in_=gdv[:, :, h, :]), writes=[GB])
                    Gv = G[0][:].rearrange("p (v d) c -> p v d c", d=2)
                    for b in range(NB):
                        tsl = slice(b * TB, (b + 1) * TB)
                        hs = slice(h * 128, (h + 1) * 128)
                        for di in range(2):
                            mrow = cmk[:, b, di * NVB:(di + 1) * NVB]
                            cx.op("dve", lambda e: e.tensor_scalar(acf[:], Gv[:, :, di, 128], -1.0, None, op0=ALU.add), reads=[GB], writes=[acB])
                            cx.op("dve", lambda e: e.tensor_tensor(out=acf[:], in0=acf[:], in1=mrow, op=ALU.mult), reads=[acB, cmB], writes=[acB])
                            cx.op("dve", lambda e: e.tensor_scalar(acf[:], acf[:], 1.0, None, op0=ALU.add), reads=[acB], writes=[acB])
                            cx.op("pool", lambda e: e.memset(Sin[di][:], 0.0), writes=[SinB[di]])
                            order = (list(range(NB, NVB)) + list(range(NB))) if di == 0 else (list(range(NVB - 1, NB - 1, -1)) + list(range(NB - 1, -1, -1)))
                            for v in order:
                                cx.op("pool", lambda e, v=v: e.tensor_scalar(msk[:], Gv[:, v, di, 0:128], mrow[:, v:v + 1], None, op0=ALU.mult),
                                      reads=[GB, cmB], writes=[mskB])
                                cx.op("dve", lambda e, v=v: e.scalar_tensor_tensor(out=Sin[di][:], in0=Sin[di][:], scalar=acf[:, v:v + 1], in1=msk[:],
                                                                                   op0=ALU.mult, op1=ALU.add),
                                      reads=[SinB[di], acB, mskB], writes=[SinB[di]])
                            cx.op("act", lambda e: e.activation(out=SinR[di][:], in_=Sin[di][:], func=AF.Copy), reads=[SinB[di]], writes=[SinRB[di]])
                            cx.dma("sp", lambda q: q.dma_start(out=qg[di][:], in_=qdgT[di, hs, tsl]), writes=[qgB[di]])
                        cx.dma("sp", lambda q: q.dma_start(out=ol[:], in_=olocT[hs, tsl]), writes=[olB])
                        cx.dma("sp", lambda q: q.dma_start(out=gsl[:], in_=gsT[hs, tsl]), writes=[gslB])
                        pt, pB = pbank()
                        cx.pe([(lambda t, di=di: t.matmul(pt[:, :], SinR[di][:], qg[di][:], start=(di == 0), stop=(di == 1))) for di in range(2)],
                              reads=SinRB + qgB, writes=[pB])
                        cx.op("dve", lambda e: e.tensor_tensor(out=ol[:], in0=pt[:, :], in1=ol[:], op=ALU.add), reads=[pB, olB], writes=[olB])
                        cx.op("act", lambda e: e.activation(out=sq[:], in_=ol[:], func=AF.Square), reads=[olB], writes=[sqB])
                        p2, p2B = pbank()
                        cx.pe([lambda t: t.matmul(p2[:, :], ones[:], sq[:], start=True, stop=True)], reads=[cB, sqB], writes=[p2B])
                        cx.op("dve", lambda e: e.tensor_scalar(rs[:], p2[:, :], 1.0 / 128, EPS, op0=ALU.mult, op1=ALU.add), reads=[p2B], writes=[rsB])
                        cx.op("act", lambda e: e.activation(out=rs[:], in_=rs[:], func=AF.Sqrt), reads=[rsB], writes=[rsB])
                        cx.op("dve", lambda e: e.reciprocal(rs[:], rs[:]), reads=[rsB], writes=[rsB])
                        cx.op("dve", lambda e: e.tensor_tensor(out=rs[:], in0=rs[:], in1=ol[:], op=ALU.mult), reads=[rsB, olB], writes=[rsB])
                        cx.op("dve", lambda e: e.tensor_tensor(out=yo[:], in0=rs[:], in1=gsl[:], op=ALU.mult), reads=[rsB, gslB], writes=[yoB])
                        cx.dma("pool", lambda q: q.dma_start(out=mixT[PW + h * 128:PW + (h + 1) * 128, tsl], in_=yo[:]), reads=[yoB])
                cx.barrier()

            chk(6)
            def proj_residual(name, AT_d, W_d, res_src, dst_d, b):
                tsl = slice(b * TB, (b + 1) * TB)
                with contextlib.ExitStack() as st:
                    AT = sb(st, name + "AT", [128, KC, 512], F32R)
                    wbufs = [sb(st, name + "w%d" % i, [128, KC, 256], F32R) for i in range(2)]
                    xr = [sb(st, name + "xr%d" % i, [128, 4, 256]) for i in range(2)]
                    ATB, wB, xrB = Buf(), [Buf(), Buf()], [Buf(), Buf()]
                    cx.dma("sp", lambda q: q.dma_start(out=AT[:], in_=AT_d[:, tsl].rearrange("(kc p) t -> p kc t", p=128)), writes=[ATB])
                    st_ = {}

                    def ep(c0, wcur, ti, pt, pB):
                        a = (c0 // 256) % 2
                        if ti == 0:
                            cx.dma("sp", lambda q: q.dma_start(out=xr[a][:], in_=res_src(b)[:, c0:c0 + 256].rearrange("(t p) c -> p t c", p=128)),
                                   writes=[xrB[a]])
                        cx.op("dve", lambda e: e.tensor_tensor(out=xr[a][:, ti, :], in0=pt[:, 0:256], in1=xr[a][:, ti, :], op=ALU.add),
                              reads=[pB, xrB[a]], writes=[xrB[a]])
                        if ti == 3:
                            cx.dma("pool", lambda q: q.dma_start(out=dst_d[tsl, c0:c0 + 256].rearrange("(t p) c -> p t c", p=128), in_=xr[a][:]),
                                   reads=[xrB[a]])
                    gemm_tm(AT, ATB, 512, W_d, D, ep, wbufs, wB)
                    cx.barrier()

            for b in range(NB):
                tsl = slice(b * TB, (b + 1) * TB)
                proj_residual("s5", mixT, w_out, lambda b: xh[b, 8:520, :], x1_d, b)
                chk(7)
                with contextlib.ExitStack() as st:
                    h1T = sb(st, "h1T", [128, KC, 512], F32R)
                    h1B = Buf()
                    norm_transpose("n2", lambda r0, n: x1_d[b * TB + r0:b * TB + r0 + n, :], [(r, 128, r) for r in range(0, 512, 128)],
                                   gains["g_xat"], h1T, h1B)
                    wbufs = [sb(st, "wq%d" % i, [128, KC, 256], F32R) for i in range(2)]
                    wB = [Buf(), Buf()]
                    qo = [sb(st, "qo%d" % i, [128, 512], F32R) for i in range(2)]
                    qoB = [Buf(), Buf()]
                    ci = [0]

                    def ep_q(cb, pt, pB, t0, tn):
                        a = ci[0] % 2
                        ci[0] += 1
                        if a == 0:
                            cx.op("act", lambda e: e.activation(out=qo[a][:], in_=pt[:, :], func=AF.Copy), reads=[pB], writes=[qoB[a]])
                        else:
                            cx.op("dve", lambda e: e.tensor_copy(qo[a][:], pt[:, :]), reads=[pB], writes=[qoB[a]])
                        cx.dma("pool", lambda q: q.dma_start(out=qT_d[cb * 128:(cb + 1) * 128, tsl], in_=qo[a][:]), reads=[qoB[a]])
                    gemm_fm(h1T, h1B, 512, wq, D, ep_q, wbufs, wB)
                    cx.barrier()
                chk(8)
                with contextlib.ExitStack() as st:
                    qh = sb(st, "qh", [128, XC, 512], F32R)
                    kh = sb(st, "kh", [128, XC, MEM], F32R)
                    vh = sb(st, "vh", [128, 2, XD], F32R)
                    pe_ = [sb(st, "pe%d" % i, [128, MEM]) for i in range(2)]
                    pT = sb(st, "pT", [128, 2, 512], F32R)
                    oh = [sb(st, "oh%d" % i, [128, 512], F32R) for i in range(2)]
                    smx = sb(st, "smx", [128, 16])
                    qhB, khB, vhB, pTB = Buf(), Buf(), Buf(), Buf()
                    peB, ohB = [Buf(), Buf()], [Buf(), Buf()]
                    smB = [Buf() for _ in range(4)]
                    sc = float(XD) ** -0.5
                    for hx in range(XH):
                        es_ = slice(hx * XD, (hx + 1) * XD)
                        cx.dma("sp", lambda q: q.dma_start(out=qh[:], in_=qT_d[es_, tsl].rearrange("(kc p) t -> p kc t", p=128)), writes=[qhB])
                        cx.dma("sp", lambda q: q.dma_start(out=kh[:], in_=kT_d[es_, :].rearrange("(kc p) t -> p kc t", p=128)), writes=[khB])
                        cx.dma("sp", lambda q: q.dma_start(out=vh[:], in_=v_d[:, es_].rearrange("(mt p) c -> p mt c", p=128)), writes=[vhB])
                        for ti in range(4):
                            a = ti % 2
                            pt, pB = pbank()
                            cx.pe([(lambda t, k=k: t.matmul(pt[:, 0:MEM], qh[:, k, ti * 128:(ti + 1) * 128], kh[:, k, :], start=(k == 0), stop=(k == XC - 1)))
                                   for k in range(XC)], reads=[qhB, khB], writes=[pB])
                            mx, nmx, ssum = smx[:, ti * 4:ti * 4 + 1], smx[:, ti * 4 + 1:ti * 4 + 2], smx[:, ti * 4 + 2:ti * 4 + 3]
                            cx.op("dve", lambda e: e.tensor_reduce(out=mx, in_=pt[:, 0:MEM], axis=AX.X, op=ALU.max), reads=[pB], writes=[smB[ti]])
                            cx.op("dve", lambda e: e.tensor_scalar(nmx, mx, -sc, None, op0=ALU.mult), reads=[smB[ti]], writes=[smB[ti]])
                            cx.op("act", lambda e: e.activation(out=pe_[a][:], in_=pt[:, 0:MEM], func=AF.Exp, bias=nmx, scale=sc, accum_out=ssum),
                                  reads=[pB, smB[ti]], writes=[peB[a], smB[ti]])
                            cx.op("dve", lambda e: e.reciprocal(ssum, ssum), reads=[smB[ti]], writes=[smB[ti]])
                            cx.op("dve", lambda e: e.tensor_scalar(pe_[a][:], pe_[a][:], ssum, None, op0=ALU.mult), reads=[peB[a], smB[ti]], writes=[peB[a]])
                            p2, p2B = pbank()
                            cx.pe([(lambda t, m=m: t.transpose(out=p2[:, m * 128:(m + 1) * 128], in_=pe_[a][:, m * 128:(m + 1) * 128], identity=ident[:]))
                                   for m in range(2)], reads=[peB[a], cB], writes=[p2B])
                            cx.op("act", lambda e: e.activation(out=pT[:, :, ti * 128:(ti + 1) * 128], in_=p2[:, 0:256].rearrange("p (m t) -> p m t", m=2),
                                                                func=AF.Copy), reads=[p2B], writes=[pTB])
                        for eb in range(XC):
                            a = eb % 2
                            pt, pB = pbank()
                            cx.pe([(lambda t, m=m: t.matmul(pt[:, :], vh[:, m, eb * 128:(eb + 1) * 128], pT[:, m, :], start=(m == 0), stop=(m == 1)))
                                   for m in range(2)], reads=[vhB, pTB], writes=[pB])
                            cx.op("dve" if a else "act", (lambda e: e.tensor_copy(oh[a][:], pt[:, :])) if a else
                                  (lambda e: e.activation(out=oh[a][:], in_=pt[:, :], func=AF.Copy)), reads=[pB], writes=[ohB[a]])
                            r0 = hx * XD + eb * 128
                            cx.dma("pool", lambda q: q.dma_start(out=oT_d[r0:r0 + 128, tsl], in_=oh[a][:]), reads=[ohB[a]])
                    cx.barrier()
                chk(9)
                proj_residual("s6", oT_d, wo, lambda b: x1_d[b * TB:(b + 1) * TB, :], x2_d, b)
                chk(10)
                with contextlib.ExitStack() as st:
                    h3T = sb(st, "h3T", [128, KC, 512])
                    h3B = Buf()
                    wr = sb(st, "wr", [128, KC, 36])
                    sl = sb(st, "sl", [128, 128])
                    ebase = sb(st, "ebase", [128, 32])
                    rB = Buf()
                    cx.dma("sp", lambda q: q.dma_start(out=wr[:], in_=wr_d.rearrange("(kc p) c -> p kc c", p=128)), writes=[rB])
                    cx.dma("sp", lambda q: q.dma_start(out=sl[:], in_=sl_d), writes=[rB])
                    cx.dma("sp", lambda q: q.dma_start(out=ebase[:], in_=ebase_d[0].partition_broadcast(128)), writes=[rB])
                    RT = [{n_: sb(st, "%s%d" % (n_, s_), shp) for n_, shp in (("lg", [128, 36]), ("oh4", [128, 4]), ("t32", [128, 32]), ("sel", [128, 8]),
                                                                              ("m8", [128, 8]), ("o1", [128, 8]), ("o2", [128, 8]), ("M1", [128, 32]),
                                                                              ("M2", [128, 32]), ("pos", [128, 32]), ("sc", [128, 16]))} for s_ in range(2)]
                    idf = [sb(st, "idf%d" % s_, [128, 2]) for s_ in range(2)]
                    RB = [Buf(), Buf()]

                    for ti in range(4):
                        gt = b * 4 + ti
                        norm_transpose("n3", lambda r0, n: x2_d[b * TB + r0:b * TB + r0 + n, :], [(ti * 128, 128, ti * 128)], gains["g_moe"], h3T, h3B)
                        R = RT[ti % 2]
                        rb = RB[ti % 2]
                        pt, pB = pbank()
                        cx.pe([(lambda t, k=k: t.matmul(pt[:, 0:36], h3T[:, k, ti * 128:(ti + 1) * 128], wr[:, k, :], start=(k == 0), stop=(k == KC - 1)))
                               for k in range(KC)], reads=[h3B, rB], writes=[pB])
                        lg, oh4, t32, sel, m8, o1, o2, M1, M2, pos, scr = (R[k] for k in ("lg", "oh4", "t32", "sel", "m8", "o1", "o2", "M1", "M2", "pos", "sc"))
                        V = lambda f: cx.op("dve", f, reads=[rb, rB, MallB], writes=[rb])
                        cx.op("dve", lambda e: e.tensor_copy(lg[:], pt[:, 0:36]), reads=[pB], writes=[rb])
                        gmx, gsum, gw, d21, w1c, w2c = (scr[:, j:j + 1] for j in range(6))
                        V(lambda e: e.tensor_reduce(out=gmx, in_=lg[:, 0:4], axis=AX.X, op=ALU.max))
                        V(lambda e: e.tensor_scalar(oh4[:], lg[:, 0:4], gmx, None, op0=ALU.is_equal))
                        V(lambda e: e.tensor_scalar(scr[:, 6:7], gmx, -1.0, None, op0=ALU.mult))
                        cx.op("act", lambda e: e.activation(out=scr[:, 8:12], in_=lg[:, 0:4], func=AF.Exp, bias=scr[:, 6:7], scale=1.0, accum_out=gsum),
                              reads=[rb], writes=[rb])
                        V(lambda e: e.reciprocal(gw, gsum))
                        V(lambda e: e.tensor_tensor(out=t32[:].rearrange("p (g j) -> p g j", g=4), in0=lg[:, 4:36].rearrange("p (g j) -> p g j", g=4),
                                                    in1=oh4[:].unsqueeze(2).to_broadcast([128, 4, 8]), op=ALU.mult))
                        V(lambda e: e.tensor_reduce(out=sel[:], in_=t32[:].rearrange("p (g j) -> p j g", g=4), axis=AX.X, op=ALU.add))
                        V(lambda e: e.max(out=m8[:], in_=sel[:]))
                        V(lambda e: e.tensor_scalar(o1[:], sel[:], m8[:, 0:1], None, op0=ALU.is_equal))
                        V(lambda e: e.tensor_scalar(o2[:], sel[:], m8[:, 1:2], None, op0=ALU.is_equal))
                        V(lambda e: e.tensor_tensor(out=d21, in0=m8[:, 1:2], in1=m8[:, 0:1], op=ALU.subtract))
                        cx.op("act", lambda e: e.activation(out=d21, in_=d21, func=AF.Exp), reads=[rb], writes=[rb])
                        V(lambda e: e.tensor_scalar(d21, d21, 1.0, None, op0=ALU.add))
                        V(lambda e: e.reciprocal(w1c, d21))
                        V(lambda e: e.tensor_scalar(w2c, w1c, -1.0, 1.0, op0=ALU.mult, op1=ALU.add))
                        cx.op("dve", lambda e: e.tensor_scalar(cw_all[:, gt * 2:gt * 2 + 1], w1c, gw, None, op0=ALU.mult), reads=[rb], writes=[cwB])
                        cx.op("dve", lambda e: e.tensor_scalar(cw_all[:, gt * 2 + 1:gt * 2 + 2], w2c, gw, None, op0=ALU.mult), reads=[rb], writes=[cwB])
                        for Mx, ox in ((M1, o1), (M2, o2)):
                            V(lambda e, Mx=Mx, ox=ox: e.tensor_tensor(out=Mx[:].rearrange("p (g j) -> p g j", g=4),
                                                                      in0=oh4[:].unsqueeze(2).to_broadcast([128, 4, 8]),
                                                                      in1=ox[:].unsqueeze(1).to_broadcast([128, 4, 8]), op=ALU.mult))
                        cx.op("dve", lambda e: e.tensor_tensor(out=Mall[:, gt, :], in0=M1[:], in1=M2[:], op=ALU.add), reads=[rb], writes=[MallB])
                        pp, ppB = pbank()
                        fns = [(lambda t, j=j: t.matmul(pp[:, 0:32], ones[:].bitcast(F32), Mall[:, j, :], start=(j == 0), stop=False)) for j in range(gt)]
                        fns.append(lambda t: t.matmul(pp[:, 0:32], sl[:], Mall[:, gt, :], start=(gt == 0), stop=True))
                        cx.pe(fns, reads=[MallB, cB, rB], writes=[ppB])
                        cx.op("dve", lambda e: e.tensor_tensor(out=pos[:], in0=pp[:, 0:32], in1=ebase[:], op=ALU.add), reads=[ppB, rB], writes=[rb])
                        for j, Mx in enumerate((M1, M2)):
                            V(lambda e, Mx=Mx: e.tensor_tensor(out=t32[:], in0=pos[:], in1=Mx[:], op=ALU.mult))
                            V(lambda e, j=j: e.tensor_reduce(out=idf[ti % 2][:, j:j + 1], in_=t32[:], axis=AX.X, op=ALU.add))
                        cx.op("dve", lambda e: e.tensor_copy(idx_all[:, gt * 2:gt * 2 + 2], idf[ti % 2][:]), reads=[rb], writes=[idxB])
                    def scat(i, xs_, xsB_, n):
                        gt = b * 4 + i
                        for j in range(2):
                            cx.dma("pool", lambda q, j=j: q.indirect_dma_start(out=Xs_d[:, :], out_offset=bass.IndirectOffsetOnAxis(ap=idx_all[:, gt * 2 + j:gt * 2 + j + 1], axis=0),
                                                                              in_=xs_[:, :], in_offset=None), reads=[xsB_, idxB])
                    norm_transpose("n4", lambda r0, n: x2_d[b * TB + r0:b * TB + r0 + n, :], [(r, 128, r) for r in range(0, 512, 128)], gains["g_moe"],
                                   None, None, keep_rows=scat)
                    cx.barrier()

            chk(11)
            with contextlib.ExitStack() as st:
                Xe = [sb(st, "Xe%d" % i, [128, D]) for i in range(2)]
                XeT = sb(st, "XeT", [128, KC, 128], F32R)
                wbufs = [sb(st, "wE%d" % i, [128, KC, 256], F32R) for i in range(3)]
                hm = sb(st, "hm", [128, DE])
                a_s = sb(st, "a_s", [128, DE])
                hmT = sb(st, "hmT", [128, DC, 128], F32R)
                ye = [sb(st, "ye%d" % i, [128, D]) for i in range(2)]
                XeB, XeTB, hmB, asB, hmTB = [Buf(), Buf()], Buf(), Buf(), Buf(), Buf()
                wB = [Buf(), Buf(), Buf()]
                yeB = [Buf(), Buf()]
                wtag = [0]
                for e_ in range(NE):
                    a = e_ % 2
                    cx.dma("sp", lambda q: q.dma_start(out=Xe[a][:], in_=Xs_d[e_ * 128:(e_ + 1) * 128, :]), writes=[XeB[a]])
                    for k0 in range(0, KC, 4):
                        pt, pB = pbank()
                        cx.pe([(lambda t, c=c: t.transpose(out=pt[:, c * 128:(c + 1) * 128], in_=Xe[a][:, (k0 + c) * 128:(k0 + c + 1) * 128], identity=ident[:]))
                               for c in range(4)], reads=[XeB[a], cB], writes=[pB])
                        src = pt[:, :].rearrange("p (c n) -> p c n", c=4)
                        if (k0 // 4) % 2:
                            cx.op("dve", lambda e: e.tensor_copy(XeT[:, k0:k0 + 4, :], src), reads=[pB], writes=[XeTB])
                        else:
                            cx.op("act", lambda e: e.activation(out=XeT[:, k0:k0 + 4, :], in_=src, func=AF.Copy), reads=[pB], writes=[XeTB])

                    def ep_a(c0, wcur, ti, pt, pB):
                        cx.op("act", lambda e: e.activation(out=a_s[:, c0:c0 + wcur], in_=pt[:, 0:wcur], func=AF.Silu), reads=[pB], writes=[asB])

                    def ep_c(c0, wcur, ti, pt, pB):
                        cx.op("dve", lambda e: e.tensor_tensor(out=hm[:, c0:c0 + wcur], in0=pt[:, 0:wcur], in1=a_s[:, c0:c0 + wcur], op=ALU.mult),
                              reads=[pB, asB], writes=[hmB])
                    gemm_tm(XeT, XeTB, 128, w1[e_], DE, ep_a, wbufs, wB, wtag)
                    gemm_tm(XeT, XeTB, 128, w3[e_], DE, ep_c, wbufs, wB, wtag)
                    for k0 in range(0, DC, 4):
                        kk = min(4, DC - k0)
                        pt, pB = pbank()
                        cx.pe([(lambda t, c=c: t.transpose(out=pt[:, c * 128:(c + 1) * 128], in_=hm[:, (k0 + c) * 128:(k0 + c + 1) * 128], identity=ident[:]))
                               for c in range(kk)], reads=[hmB, cB], writes=[pB])
                        cx.op("act", lambda e: e.activation(out=hmT[:, k0:k0 + kk, :], in_=pt[:, 0:kk * 128].rearrange("p (c n) -> p c n", c=kk), func=AF.Copy),
                              reads=[pB], writes=[hmTB])
                    WB2 = (KC * 256) // DC
                    WB2 = min(WB2, D)
                    for c0 in range(0, D, WB2):
                        wa = wtag[0] % 3
                        wtag[0] += 1
                        wv_ = wbufs[wa][:].rearrange("p k c -> p (k c)")[:, 0:DC * WB2].rearrange("p (k c) -> p k c", k=DC)
                        load_w(wv_, w2[e_][:, c0:c0 + WB2], wB[wa])
                        for c1 in range(0, WB2, 512):
                            cw = min(512, WB2 - c1)
                            pt, pB = pbank()
                            cx.pe([(lambda t, k=k: t.matmul(pt[:, 0:cw], hmT[:, k, :], wv_[:, k, c1:c1 + cw], start=(k == 0), stop=(k == DC - 1))) for k in range(DC)],
                                  reads=[hmTB, wB[wa]], writes=[pB])
                            dst = ye[a][:, c0 + c1:c0 + c1 + cw]
                            if (c1 // 512) % 2:
                                cx.op("dve", lambda e: e.tensor_copy(dst, pt[:, 0:cw]), reads=[pB], writes=[yeB[a]])
                            else:
                                cx.op("act", lambda e: e.activation(out=dst, in_=pt[:, 0:cw], func=AF.Copy), reads=[pB], writes=[yeB[a]])
                    cx.dma("pool", lambda q: q.dma_start(out=Y_d[e_ * 128:(e_ + 1) * 128, :], in_=ye[a][:]), reads=[yeB[a]])
                cx.barrier()

            chk(12)
            with contextlib.ExitStack() as st:
                gbc = sb(st, "fgbc", [128, D])
                y1 = [sb(st, "y1_%d" % i, [128, D]) for i in range(2)]
                y2 = [sb(st, "y2_%d" % i, [128, D]) for i in range(2)]
                xx = [sb(st, "xx_%d" % i, [128, D]) for i in range(2)]
                junk = sb(st, "fjunk", [128, D])
                ss = sb(st, "fss", [128, 8])
                gB, jB = Buf(), Buf()
                y1B, y2B, xxB = [Buf(), Buf()], [Buf(), Buf()], [Buf(), Buf()]
                ssB = [Buf() for _ in range(8)]
                cx.dma("sp", lambda q: q.dma_start(out=gbc[:], in_=gains["g_fin"][0].partition_broadcast(128)), writes=[gB])
                for gt in range(NB * 4):
                    a = gt % 2
                    cx.dma("sp", lambda q: q.dma_start(out=xx[a][:], in_=x2_d[gt * 128:(gt + 1) * 128, :]), writes=[xxB[a]])
                    for j, (yy, yyB) in enumerate(((y1[a], y1B[a]), (y2[a], y2B[a]))):
                        cx.dma("pool", lambda q, yy=yy, j=j: q.indirect_dma_start(out=yy[:, :], out_offset=None, in_=Y_d[:, :],
                                                                                 in_offset=bass.IndirectOffsetOnAxis(ap=idx_all[:, gt * 2 + j:gt * 2 + j + 1], axis=0)),
                               reads=[idxB], writes=[yyB])
                        cx.op("dve", lambda e, yy=yy, j=j: e.scalar_tensor_tensor(out=xx[a][:], in0=yy[:], scalar=cw_all[:, gt * 2 + j:gt * 2 + j + 1], in1=xx[a][:],
                                                                                 op0=ALU.mult, op1=ALU.add), reads=[yyB, cwB, xxB[a]], writes=[xxB[a]])
                    s1 = ss[:, gt % 8:gt % 8 + 1]
                    sB = ssB[gt % 8]
                    cx.op("act", lambda e: e.activation(out=junk[:], in_=xx[a][:], func=AF.Square, accum_out=s1), reads=[xxB[a]], writes=[jB, sB])
                    rstd_from_ss(s1, 128, sB, 1.0 / D)
                    cx.op("dve", lambda e: e.scalar_tensor_tensor(out=xx[a][:], in0=xx[a][:], scalar=s1, in1=gbc[:], op0=ALU.mult, op1=ALU.mult),
                          reads=[xxB[a], sB, gB], writes=[xxB[a]])
                    cx.dma("sp", lambda q: q.dma_start(out=out_d[gt * 128:(gt + 1) * 128, :], in_=xx[a][:]), reads=[xxB[a]])
                cx.barrier()
        _body()
        cx.dead = False
        cx.barrier()
    return nc


def host_inputs(cfg, inp):
    g = dims(cfg)
    D, NB, NC, TOK, PW, PG, HK, NH, NVB = g["D"], g["NB"], g["NC"], g["TOK"], g["PW"], g["PG"], g["HK"], g["NH"], g["NVB"]
    f = np.float32
    x = np.asarray(inp["x"], f)[0]
    S = x.shape[0]
    xp = np.zeros((S + 16, D), f)
    xp[8:8 + S] = x
    common = dict(
        mem=np.ascontiguousarray(np.asarray(inp["mem"], f)[0]),
        g_mix=np.asarray(inp["norm_mix"], f).reshape(1, D), g_xat=np.asarray(inp["norm_xattn"], f).reshape(1, D),
        g_mem=np.asarray(inp["norm_mem"], f).reshape(1, D), g_moe=np.asarray(inp["norm_moe"], f).reshape(1, D),
        g_fin=np.asarray(inp["norm_final"], f).reshape(1, D),
        w_in=np.asarray(inp["w_in"], f)[0], pool_w=np.asarray(inp["pool_w"], f)[0],
        psc=np.ascontiguousarray(np.asarray(inp["pool_scale"], f)[0].reshape(PW // 128, 128).T),
        lbp=np.ascontiguousarray(np.stack([np.asarray(inp["lb_fwd"], f), np.asarray(inp["lb_bwd"], f)], 0).reshape(2, 2, NH, 128).transpose(3, 0, 1, 2)),
        hgn=np.ascontiguousarray(np.asarray(inp["hgrn_norm"], f)[0].reshape(NH, 128).T),
        w_out=np.asarray(inp["w_out"], f)[0], wq=np.asarray(inp["w_q"], f)[0], wk=np.asarray(inp["w_k"], f)[0],
        wv=np.asarray(inp["w_v"], f)[0], wo=np.asarray(inp["w_o"], f)[0],
        wr=np.ascontiguousarray(np.concatenate([np.asarray(inp["w_router_group"], f)[0],
                                                np.asarray(inp["w_router_expert"], f)[0].transpose(1, 0, 2).reshape(D, 32)], 1)),
        w1=np.asarray(inp["w1"], f)[0], w3=np.asarray(inp["w3"], f)[0], w2=np.asarray(inp["w2"], f)[0],
        ident=np.eye(128, dtype=f), ones=np.ones((128, 128), f),
        sl=np.triu(np.ones((128, 128), f), 1),
        ebase=(np.arange(32, dtype=f) * 128).reshape(1, 32),
    )
    i = np.arange(128)
    same = (i[:, None] // 64) == (i[None, :] // 64)
    common["maskf"] = np.tile((same & (i[:, None] <= i[None, :])).astype(f), (1, 4))
    common["maskb"] = np.tile((same & (i[:, None] >= i[None, :])).astype(f), (1, 4))
    common["rmask"] = np.tile(((np.arange(512) % 64) != 0).astype(f)[None, :], (128, 1))
    t = np.arange(S)
    inv = np.zeros((4, S), f)
    for gi, w in enumerate((2, 4, 8, 16)):
        lo = np.clip(t - w // 2, 0, S - 1)
        hi = np.clip(t + w // 2 - 1, 0, S - 1)
        inv[gi] = 1.0 / (hi - lo + 1)
    maps = []
    for c in range(NC):
        m = dict(common)
        own = [c * NB + b for b in range(NB)]
        slots = own + [v for v in range(NVB) if v not in own]
        m["xh"] = np.stack([xp[v * TB: v * TB + 528] for v in slots], 0)
        m["invcnt"] = np.stack([inv[:, v * TB:(v + 1) * TB].reshape(-1) for v in own], 0)
        cm = np.zeros((NB, 2, NVB), f)
        for b in range(NB):
            for j, v in enumerate(slots):
                cm[b, 0, j] = 1.0 if v < own[b] else 0.0
                cm[b, 1, j] = 1.0 if v > own[b] else 0.0
        m["cmask"] = cm.reshape(NB, 2 * NVB)
        maps.append(m)
    return maps


FULL = dict(D=4096, NB=2, NC=8)
_NC_CACHE = {}


def run(cfg, inp):
    key = tuple(sorted(cfg.items()))
    if key not in _NC_CACHE:
        _NC_CACHE[key] = build(cfg)
    nc = _NC_CACHE[key]
    maps = host_inputs(cfg, inp)
    res = run_bass_kernel_spmd(nc, maps, core_ids=list(range(cfg["NC"])))
    out = np.concatenate([r["out"] for r in res.results], axis=0)
    return out[None].astype(np.float32)


def kernel(**inputs):
    return run(FULL, inputs)
```

```python
import contextlib
import numpy as np
import concourse.bass as bass
import concourse.mybir as mybir
from concourse.bass_utils import run_bass_kernel_spmd

F32 = mybir.dt.float32
F32R = mybir.dt.float32r
I32 = mybir.dt.int32
AF = mybir.ActivationFunctionType
ALU = mybir.AluOpType
AX = mybir.AxisListType
EPS = 1e-6
TB = 512
NSLOT = 8192


class Stop(Exception):
    pass


class Buf:
    __slots__ = ("w", "r")

    def __init__(self):
        self.w = None
        self.r = []


class Ctx:
    def __init__(self, nc, es):
        self.nc = nc
        self.eng = dict(pe=nc.tensor, act=nc.scalar, dve=nc.vector, pool=nc.gpsimd, sp=nc.sync)
        self.sem = {e: es.enter_context(nc.semaphore("s_" + e)) for e in self.eng}
        self.cnt = {e: 0 for e in self.eng}
        self.seen = {e: {} for e in self.eng}
        self.dsem = {q: [es.enter_context(nc.semaphore("d_%s%d" % (q, i))) for i in range(8)] for q in ("sp", "pool")}
        self.dcnt = {q: [0] * 8 for q in ("sp", "pool")}
        self.dnext = {"sp": 0, "pool": 0}
        self.alltoks = []
        self.dead = False

    def wait(self, e, toks):
        best = {}
        for t in toks:
            if t is None:
                continue
            sem, val, key = t
            if e == "pe" and key == "s_pe":
                continue
            if self.seen[e].get(key, 0) >= val:
                continue
            if key not in best or best[key][1] < val:
                best[key] = t
        for key, (sem, val, _) in best.items():
            self.eng[e].wait_ge(sem, val)
            self.seen[e][key] = val

    def _deps(self, reads, writes):
        toks = []
        for b in reads:
            toks.append(b.w)
        for b in writes:
            toks.append(b.w)
            toks.extend(b.r)
        return toks

    def _commit(self, tok, reads, writes):
        for b in reads:
            b.r.append(tok)
        for b in writes:
            b.w = tok
            b.r = []

    def op(self, e, fn, reads=(), writes=()):
        if self.dead:
            return None
        self.wait(e, self._deps(reads, writes))
        inst = fn(self.eng[e])
        self.cnt[e] += 1
        inst.then_inc(self.sem[e], 1)
        tok = (self.sem[e], self.cnt[e], "s_" + e)
        self._commit(tok, reads, writes)
        return tok

    def pe(self, fns, reads=(), writes=()):
        if self.dead:
            return None
        self.wait("pe", self._deps(reads, writes))
        inst = None
        for fn in fns:
            inst = fn(self.nc.tensor)
        self.cnt["pe"] += 1
        inst.then_inc(self.sem["pe"], 1)
        tok = (self.sem["pe"], self.cnt["pe"], "s_pe")
        self._commit(tok, reads, writes)
        return tok

    def dma(self, q, fn, reads=(), writes=()):
        if self.dead:
            return None
        i = self.dnext[q]
        self.dnext[q] = (i + 1) % 8
        sem = self.dsem[q][i]
        key = "d_%s%d" % (q, i)
        toks = self._deps(reads, writes)
        if self.dcnt[q][i] > 0:
            toks.append((sem, self.dcnt[q][i], key))
        self.wait(q, toks)
        inst = fn(self.eng[q])
        self.dcnt[q][i] += 16
        inst.then_inc(sem, 16)
        tok = (sem, self.dcnt[q][i], key)
        self._commit(tok, reads, writes)
        self.alltoks.append(tok)
        return tok

    def barrier(self):
        if self.dead:
            return
        toks = [(self.sem[e], self.cnt[e], "s_" + e) for e in self.eng if self.cnt[e] > 0]
        for q in ("sp", "pool"):
            for i in range(8):
                if self.dcnt[q][i] > 0:
                    toks.append((self.dsem[q][i], self.dcnt[q][i], "d_%s%d" % (q, i)))
        for e in self.eng:
            self.wait(e, [t for t in toks if not (e == "pe" and t[2] == "s_pe")])


def dims(cfg):
    D = cfg["D"]
    d = dict(D=D, NB=cfg["NB"], NC=cfg["NC"], KC=D // 128, TOK=cfg["NB"] * TB, PW=D // 2, PG=D // 8,
             HK=D // 2, NH=D // 256, XH=4, XD=D // 4, XC=D // 512, MEM=256, NE=32, DE=D // 4, DC=D // 512)
    d["NVB"] = d["NC"] * d["NB"]
    return d


def build(cfg):
    g = dims(cfg)
    D, NB, NC, KC, TOK, PW, PG, HK, NH = g["D"], g["NB"], g["NC"], g["KC"], g["TOK"], g["PW"], g["PG"], g["HK"], g["NH"]
    XH, XD, XC, MEM, NE, DE, DC, NVB = g["XH"], g["XD"], g["XC"], g["MEM"], g["NE"], g["DE"], g["DC"], g["NVB"]
    nc = bass.Bass("TRN2", target_bir_lowering=False)
    nc.dge_precook = False

    def din(name, shape, dt=F32):
        return nc.dram_tensor(name, shape, dt, kind="ExternalInput").ap()

    def dsc(name, shape, dt=F32, **kw):
        return nc.dram_tensor(name, shape, dt, **kw).ap()

    xh = din("xh", [NVB, 528, D])
    mem = din("mem", [MEM, D])
    gains = {n: din(n, [1, D]) for n in ("g_mix", "g_xat", "g_mem", "g_moe", "g_fin")}
    w_in = din("w_in", [D, 3 * D], F32R)
    pool_w = din("pool_w", [4, PG, PG], F32R)
    psc_d = din("psc", [128, PW // 128])
    lbp_d = din("lbp", [128, 2, 2, NH])
    hgn_d = din("hgn", [128, NH])
    w_out = din("w_out", [D, D], F32R)
    wq = din("wq", [D, D], F32R)
    wk = din("wk", [D, D], F32R)
    wv = din("wv", [D, D], F32R)
    wo = din("wo", [D, D], F32R)
    wr_d = din("wr", [D, 36])
    w1 = din("w1", [NE, D, DE], F32R)
    w3 = din("w3", [NE, D, DE], F32R)
    w2 = din("w2", [NE, DE, D], F32R)
    ident_d = din("ident", [128, 128])
    maskf_d = din("maskf", [128, 512])
    maskb_d = din("maskb", [128, 512])
    rmask_d = din("rmask", [128, 512])
    ones_d = din("ones", [128, 128], F32R)
    sl_d = din("sl", [128, 128])
    invcnt_d = din("invcnt", [NB, 4 * 512])
    cmask_d = din("cmask", [NB, 2 * NVB])
    ebase_d = din("ebase", [1, 32])
    out_d = nc.dram_tensor("out", [TOK, D], F32, kind="ExternalOutput").ap()

    mixT = dsc("mixT", [D, TOK], F32R)
    olocT = dsc("olocT", [HK, TOK])
    gsT = dsc("gsT", [HK, TOK])
    qdgT = dsc("qdgT", [2, HK, TOK], F32R)
    CCR = NVB * 2 * NH * 128
    Gd = dsc("Gd", [CCR, 129])
    x1_d = dsc("x1s", [TOK, D])
    qT_d = dsc("qTs", [D, TOK], F32R)
    kT_d = dsc("kTs", [D, MEM], F32R)
    v_d = dsc("vs", [MEM, D], F32R)
    oT_d = dsc("oTs", [D, TOK], F32R)
    x2_d = dsc("x2s", [TOK, D])
    Xs_d = dsc("Xs", [NSLOT, D])
    Y_d = dsc("Ys", [NSLOT, D])

    with contextlib.ExitStack() as es:
        cx = Ctx(nc, es)
        cc_sem = es.enter_context(nc.semaphore("cc_sem"))

        uid = [0]

        def sb(st, name, shape, dt=F32):
            uid[0] += 1
            return st.enter_context(nc.sbuf_tensor("%s_u%d" % (name, uid[0]), shape, dt))

        psb = [es.enter_context(nc.psum_tensor("ps%d" % i, [128, 512], F32)) for i in range(8)]
        psB = [Buf() for _ in range(8)]
        pstate = [0]

        def pbank():
            i = pstate[0]
            pstate[0] = (i + 1) % 6
            return psb[i], psB[i]

        ident = sb(es, "ident", [128, 128])
        ones = sb(es, "ones", [128, 128], F32R)
        small = sb(es, "small", [128, 64])
        idx_all = sb(es, "idx_all", [128, 2 * NB * 4], I32)
        cw_all = sb(es, "cw_all", [128, 2 * NB * 4])
        Mall = sb(es, "Mall", [128, NB * 4, 32])
        cB = Buf()
        cx.dma("sp", lambda q: q.dma_start(out=ident[:], in_=ident_d), writes=[cB])
        cx.dma("sp", lambda q: q.dma_start(out=ones[:], in_=ones_d), writes=[cB])
        idxB, cwB, MallB = Buf(), Buf(), Buf()

        def bl(x):
            return list(x) if isinstance(x, (list, tuple)) else [x]

        def load_w(dst, src_ap, B, extra_writes=()):
            return cx.dma("sp", lambda q: q.dma_start(out=dst, in_=src_ap.rearrange("(kc p) c -> p kc c", p=128)),
                          writes=bl(B) + list(extra_writes))

        def rstd_from_ss(ss, n, B, scale):
            cx.op("dve", lambda e: e.tensor_scalar(ss[0:n], ss[0:n], scale, EPS, op0=ALU.mult, op1=ALU.add), reads=[B], writes=[B])
            cx.op("act", lambda e: e.activation(out=ss[0:n], in_=ss[0:n], func=AF.Sqrt), reads=[B], writes=[B])
            cx.op("dve", lambda e: e.reciprocal(ss[0:n], ss[0:n]), reads=[B], writes=[B])

        def norm_transpose(st_name, row_src, tiles, gain_d, hT, hTB, out_dt_rows=None, keep_rows=None):
            with contextlib.ExitStack() as st:
                gbc = sb(st, st_name + "gbc", [128, D])
                xt = [sb(st, st_name + "xt%d" % i, [128, D]) for i in range(2)]
                xs = [sb(st, st_name + "xs%d" % i, [128, D]) for i in range(2)]
                junk = sb(st, st_name + "junk", [128, D])
                ss = sb(st, st_name + "ss", [128, 8])
                gB, jB = Buf(), Buf()
                xtB = [Buf(), Buf()]
                xsB = [Buf(), Buf()]
                ssB = [Buf() for _ in range(8)]
                cx.dma("sp", lambda q: q.dma_start(out=gbc[:], in_=gain_d[0].partition_broadcast(128)), writes=[gB])
                for i, (r0, n, c0) in enumerate(tiles):
                    a = i % 2
                    s1 = ss[:, (i % 8):(i % 8) + 1]
                    sB = ssB[i % 8]
                    cx.dma("sp", lambda q: q.dma_start(out=xt[a][0:n], in_=row_src(r0, n)), writes=[xtB[a]])
                    cx.op("act", lambda e: e.activation(out=junk[0:n], in_=xt[a][0:n], func=AF.Square, accum_out=s1[0:n]),
                          reads=[xtB[a]], writes=[jB, sB])
                    rstd_from_ss(s1, n, sB, 1.0 / D)
                    cx.op("dve", lambda e: e.scalar_tensor_tensor(out=xs[a][0:n], in0=xt[a][0:n], scalar=s1[0:n], in1=gbc[0:n],
                                                                  op0=ALU.mult, op1=ALU.mult),
                          reads=[xtB[a], sB, gB], writes=[xsB[a]])
                    if keep_rows is not None:
                        keep_rows(i, xs[a], xsB[a], n)
                    if hT is None:
                        continue
                    for k0 in range(0, KC, 4):
                        pt, pB = pbank()
                        kk = min(4, KC - k0)
                        cx.pe([(lambda t, c=c: t.transpose(out=pt[:, c * 128:c * 128 + n], in_=xs[a][0:n, (k0 + c) * 128:(k0 + c + 1) * 128],
                                                           identity=ident[0:n, 0:n])) for c in range(kk)],
                              reads=[xsB[a], cB], writes=[pB])
                        src = pt[:, 0:kk * 128].rearrange("p (c n) -> p c n", c=kk)[:, :, 0:n]
                        dst = hT[:, k0:k0 + kk, c0:c0 + n]
                        if (k0 // 4) % 2 == 0:
                            cx.op("act", lambda e: e.activation(out=dst, in_=src, func=AF.Copy), reads=[pB], writes=[hTB])
                        else:
                            cx.op("dve", lambda e: e.tensor_copy(dst, src), reads=[pB], writes=[hTB])
                cx.barrier()

        def gemm_fm(AT, ATB, ntok, W_ap, ncols, epilogue, wbufs, wB, tag=[0]):
            kc_n = AT.shape[1]
            WB = wbufs[0].shape[2]
            for c0 in range(0, ncols, WB):
                a = tag[0] % len(wbufs)
                tag[0] += 1
                wcur = min(WB, ncols - c0)
                load_w(wbufs[a][:, 0:kc_n, 0:wcur], W_ap[:, c0:c0 + wcur], wB[a])
                for c1 in range(0, wcur, 128):
                    for t0 in range(0, ntok, 512):
                        tn = min(512, ntok - t0)
                        pt, pB = pbank()
                        cx.pe([(lambda t, k=k: t.matmul(pt[:, 0:tn], wbufs[a][:, k, c1:c1 + 128], AT[:, k, t0:t0 + tn],
                                                        start=(k == 0), stop=(k == kc_n - 1))) for k in range(kc_n)],
                              reads=bl(wB[a]) + [ATB], writes=[pB])
                        epilogue((c0 + c1) // 128, pt, pB, t0, tn)

        def gemm_tm(AT, ATB, ntok, W_ap, ncols, epilogue, wbufs, wB, tag=[0]):
            kc_n = AT.shape[1]
            WB = wbufs[0].shape[2]
            for c0 in range(0, ncols, WB):
                a = tag[0] % len(wbufs)
                tag[0] += 1
                wcur = min(WB, ncols - c0)
                load_w(wbufs[a][:, 0:kc_n, 0:wcur], W_ap[:, c0:c0 + wcur], wB[a])
                for t0 in range(0, ntok, 128):
                    pt, pB = pbank()
                    cx.pe([(lambda t, k=k: t.matmul(pt[:, 0:wcur], AT[:, k, t0:t0 + 128], wbufs[a][:, k, 0:wcur],
                                                    start=(k == 0), stop=(k == kc_n - 1))) for k in range(kc_n)],
                          reads=bl(wB[a]) + [ATB], writes=[pB])
                    epilogue(c0, wcur, t0 // 128, pt, pB)

        def chk(n):
            if cfg.get("stop", 99) == n:
                cx.barrier()
                cx.dead = True

        def _body():
            for b in range(NVB):
                own = b < NB
                tsl = slice(b * TB, (b + 1) * TB) if own else None
                with contextlib.ExitStack() as st:
                    hT = sb(st, "hT", [128, KC, 528], F32R)
                    hTB = Buf()
                    tiles = [(r, 128, r) for r in range(0, 512, 128)] + [(512, 16, 512)]
                    norm_transpose("n1", lambda r0, n: xh[b, r0:r0 + n, :], tiles, gains["g_mix"], hT, hTB)
                    chk(1)
                    with contextlib.ExitStack() as st2:
                        wbig = sb(st2, "wA", [128, KC, 512], F32R)
                        wslot = [wbig[:, :, j * 128:(j + 1) * 128] for j in range(4)]
                        wsB = [Buf() for _ in range(4)]
                        wbufs = [wbig[:, :, 0:256], wbig[:, :, 256:512]]
                        wB = [[wsB[0], wsB[1]], [wsB[2], wsB[3]]]
                        wtagA = [0]
                        wtagS = [0]
                        with contextlib.ExitStack() as st3:
                          if own:
                            inv = sb(st3, "inv", [128, 4 * 512])
                            psc = sb(st3, "psc", [128, PW // 128])
                            pw = sb(st3, "pw", [128, PG // 128, PG], F32R)
                            uT = [sb(st3, "uT%d" % i, [128, 528]) for i in range(2)]
                            pa = [sb(st3, "pa%d" % i, [128, 528]) for i in range(2)]
                            pb_ = [sb(st3, "pb%d" % i, [128, 528]) for i in range(2)]
                            miT = sb(st3, "miT", [128, PG // 128, 512], F32R)
                            yp = [sb(st3, "yp%d" % i, [128, 512], F32R) for i in range(2)]
                            invB, pscB, pwB, miB = Buf(), Buf(), Buf(), Buf()
                            uB, paB, pbB, ypB = [Buf(), Buf()], [Buf(), Buf()], [Buf(), Buf()], [Buf(), Buf()]
                            cx.dma("sp", lambda q: q.dma_start(out=inv[:], in_=invcnt_d[b].partition_broadcast(128)), writes=[invB])
                            cx.dma("sp", lambda q: q.dma_start(out=psc[:], in_=psc_d), writes=[pscB])
                            cnt = [0]
                            for gi, w in enumerate((2, 4, 8, 16)):
                                cx.dma("sp", lambda q: q.dma_start(out=pw[:], in_=pool_w[gi].rearrange("(kc p) c -> p kc c", p=128)),
                                       writes=[pwB])

                                def ep_pool(cb, pt, pB, t0, tn, gi=gi, w=w):
                                    a = (cnt[0] // 2) % 2
                                    cnt[0] += 1
                                    if tn == 512:
                                        cx.op("act", lambda e: e.activation(out=uT[a][:, 0:512], in_=pt[:, 0:512], func=AF.Copy),
                                              reads=[pB], writes=[uB[a]])
                                        return
                                    cx.op("act", lambda e: e.activation(out=uT[a][:, 512:528], in_=pt[:, 0:16], func=AF.Copy),
                                          reads=[pB], writes=[uB[a]])
                                    cur, curB, m = uT[a], uB[a], 1
                                    alt = [(pa[a], paB[a]), (pb_[a], pbB[a])]
                                    k = 0
                                    while m < w // 2:
                                        nx, nxB = alt[k % 2]
                                        k += 1
                                        cx.op("pool", lambda e, nx=nx, cur=cur, m=m: e.tensor_tensor(out=nx[:, 2 * m - 1:528], in0=cur[:, 2 * m - 1:528],
                                                                                                     in1=cur[:, m - 1:528 - m], op=ALU.add),
                                              reads=[curB], writes=[nxB])
                                        cur, curB, m = nx, nxB, 2 * m
                                    h = w // 2
                                    nx, nxB = alt[k % 2]
                                    cx.op("dve", lambda e: e.tensor_tensor(out=nx[:, 8:520], in0=cur[:, 7:519], in1=cur[:, 7 + h:519 + h], op=ALU.add),
                                          reads=[curB], writes=[nxB])
                                    cx.op("dve", lambda e: e.tensor_tensor(out=nx[:, 8:520], in0=nx[:, 8:520], in1=inv[:, gi * 512:(gi + 1) * 512], op=ALU.mult),
                                          reads=[nxB, invB], writes=[nxB])
                                    cbl = cb
                                    cx.op("dve", lambda e: e.tensor_tensor(out=miT[:, cbl, :], in0=nx[:, 8:520], in1=uT[a][:, 8:520], op=ALU.subtract),
                                          reads=[nxB, uB[a]], writes=[miB])

                                gemm_fm(hT, hTB, 528, w_in[:, gi * PG:(gi + 1) * PG], PG, ep_pool, wbufs, wB, wtagA)
                                for db in range(PG // 128):
                                    pt, pB = pbank()
                                    ncb = PG // 128
                                    cx.pe([(lambda t, c=c: t.matmul(pt[:, :], pw[:, c, db * 128:(db + 1) * 128], miT[:, c, :],
                                                                    start=(c == 0), stop=(c == ncb - 1))) for c in range(ncb)],
                                          reads=[pwB, miB], writes=[pB])
                                    a = db % 2
                                    col = gi * (PG // 128) + db
                                    cx.op("dve", lambda e: e.tensor_scalar(yp[a][:], pt[:, :], psc[:, col:col + 1], None, op0=ALU.mult),
                                          reads=[pB, pscB], writes=[ypB[a]])
                                    r0 = gi * PG + db * 128
                                    cx.dma("pool", lambda q: q.dma_start(out=mixT[r0:r0 + 128, tsl], in_=yp[a][:]), reads=[ypB[a]])
                            cx.barrier()
                        chk(2)
                        with contextlib.ExitStack() as st3:
                            maskf = sb(st3, "maskf", [128, 512])
                            maskb = sb(st3, "maskb", [128, 512])
                            rmask = sb(st3, "rmask", [128, 512])
                            lbp = sb(st3, "lbp", [128, 2, 2, NH])
                            lb = sb(st3, "lb", [128, 2, NH])
                            oml = sb(st3, "oml", [128, 2, NH])
                            hgn = sb(st3, "hgn", [128, NH])
                            ones8 = sb(st3, "ones8", [128, 8])
                            kB = Buf()
                            for t_, d_ in ((maskf, maskf_d), (maskb, maskb_d), (rmask, rmask_d), (lbp, lbp_d), (hgn, hgn_d)):
                                cx.dma("sp", lambda q, t_=t_, d_=d_: q.dma_start(out=t_[:], in_=d_), writes=[kB])
                            cx.op("dve", lambda e: e.memset(ones8[:], 1.0), writes=[kB])
                            cx.op("dve", lambda e: e.tensor_tensor(out=lb[:], in0=lbp[:, :, 0, :], in1=lbp[:, :, 1, :], op=ALU.subtract), reads=[kB], writes=[kB])
                            cx.op("act", lambda e: e.activation(out=lb[:], in_=lb[:], func=AF.Sigmoid), reads=[kB], writes=[kB])
                            cx.op("dve", lambda e: e.tensor_scalar(oml[:], lb[:], -1.0, 1.0, op0=ALU.mult, op1=ALU.add), reads=[kB], writes=[kB])
                            NS = 2
                            names = ["qs", "t1", "t2", "t3", "t4", "t5", "t6"]
                            T = [{n: sb(st3, "%s_%d" % (n, s), [128, 512]) for n in names} for s in range(NS)]
                            TR = [{n: sb(st3, "%s_%d" % (n, s), [128, 512], F32R) for n in ("qd", "kd", "qg")} for s in range(NS)]
                            kend = [sb(st3, "kend%d" % s, [128, 2, 4, 128], F32R) for s in range(NS)]
                            Sst = [sb(st3, "Sst%d" % s, [128, 9, 128], F32R) for s in range(NS)]
                            vt = [sb(st3, "vt%d" % s, [128, 4, 256], F32R) for s in range(2)]
                            sm = [sb(st3, "sm%d" % s, [128, 40]) for s in range(NS)]
                            TBf = [{n: Buf() for n in names + ["qd", "kd", "qg", "kend", "S", "sm"]} for s in range(NS)]
                            for s_ in range(NS):
                                for al, tg in (("gs", "t6"), ("ke", "t2"), ("ol", "t2")):
                                    T[s_][al] = T[s_][tg]
                                    TBf[s_][al] = TBf[s_][tg]
                                TR[s_]["AT"] = TR[s_]["qg"]
                                TBf[s_]["AT"] = TBf[s_]["qg"]
                            vtB = [Buf(), Buf()]
                            psum_hold = {}

                            def ep_hold(key):
                                def ep(cb, pt, pB, t0, tn):
                                    psum_hold[(key, cb)] = (pt, pB)
                                return ep

                            for hp in range(NH // 2):
                                va = hp % 2

                                def ep_v(c0, wcur, ti, pt, pB, va=va):
                                    cx.op("act", lambda e: e.activation(out=vt[va][:, ti, :], in_=pt[:, 0:256], func=AF.Copy), reads=[pB], writes=[vtB[va]])
                                gemm_tm(hT[:, :, 8:520], hTB, 512, w_in[:, PW + 3 * HK + hp * 256:PW + 3 * HK + (hp + 1) * 256], 256, ep_v, wbufs, wB, wtagA)
                                def loadpair(region):
                                    c = PW + region * HK + hp * 256
                                    a = wtagA[0] % 2
                                    wtagA[0] += 1
                                    load_w(wbufs[a], w_in[:, c:c + 256], wB[a])
                                    return a

                                def projh(a, hh):
                                    pt, pB = pbank()
                                    cx.pe([(lambda t, k=k: t.matmul(pt[:, :], wbufs[a][:, k, hh * 128:(hh + 1) * 128], hT[:, k, 8:520], start=(k == 0), stop=(k == KC - 1)))
                                           for k in range(KC)], reads=bl(wB[a]) + [hTB], writes=[pB])
                                    return pt, pB
                                HV = []
                                for hh in range(2):
                                    h = hp * 2 + hh
                                    s = h % NS
                                    HV.append((h, s, T[s], TR[s], TBf[s], slice(h * 128, (h + 1) * 128), psb[6 + h % 2], psB[6 + h % 2], [True]))
                                _sv = cx.dead; cx.dead = _sv or not own
                                for region in (0, 4):
                                    aq = loadpair(region)
                                    for hh in range(2):
                                        h, s, tt, tr, bf, hs, ot, oB, first_o = HV[hh]
                                        pt, pB = projh(aq, hh)
                                        if region == 0:
                                            cx.op("act", lambda e: e.activation(out=tt["qs"][:], in_=pt[:, :], func=AF.Silu), reads=[pB], writes=[bf["qs"]])
                                        else:
                                            cx.op("act", lambda e: e.activation(out=tt["gs"][:], in_=pt[:, :], func=AF.Silu), reads=[pB], writes=[bf["gs"]])
                                            cx.op("dve", lambda e: e.tensor_scalar(tt["gs"][:], tt["gs"][:], hgn[:, h:h + 1], None, op0=ALU.mult),
                                                  reads=[bf["gs"], kB], writes=[bf["gs"]])
                                            cx.dma("pool", lambda q: q.dma_start(out=gsT[hs, tsl], in_=tt["gs"][:]), reads=[bf["gs"]])
                                cx.dead = _sv
                                for di in range(2):
                                    af = loadpair(1 + di)
                                    for hh in range(2):
                                        h, s, tt, tr, bf, hs, ot, oB, first_o = HV[hh]
                                        fwd = di == 0
                                        pt, pB = projh(af, hh)
                                        t1, t2, t3, t4, t5, t6, ke = (tt[n] for n in ("t1", "t2", "t3", "t4", "t5", "t6", "ke"))
                                        smt = sm[s]
                                        tot = t3[:, 63::64]
                                        inc8, ex8, dec, dt1 = smt[:, 0:8], smt[:, 8:16], smt[:, 16:24], smt[:, 24:25]
                                        cx.op("act", lambda e: e.activation(out=t1[:], in_=pt[:, :], func=AF.Sigmoid), reads=[pB], writes=[bf["t1"]])
                                        cx.op("dve", lambda e: e.tensor_scalar(t1[:], t1[:], oml[:, di, h:h + 1], lb[:, di, h:h + 1], op0=ALU.mult, op1=ALU.add),
                                              reads=[bf["t1"], kB], writes=[bf["t1"]])
                                        cx.op("act", lambda e: e.activation(out=t2[:], in_=t1[:], func=AF.Ln), reads=[bf["t1"]], writes=[bf["t2"]])
                                        cx.op("pool", lambda e: e.tensor_scalar(t1[:], t1[:], -1.0, 1.0, op0=ALU.mult, op1=ALU.add),
                                              reads=[bf["t1"]], writes=[bf["t1"]])
                                        cx.op("dve", lambda e: e.tensor_tensor_scan(out=t3[:], data0=rmask[:], data1=t2[:], initial=0.0, op0=ALU.mult, op1=ALU.add),
                                              reads=[bf["t2"], kB], writes=[bf["t3"]])
                                        if fwd:
                                            bb, bbB = t3, bf["t3"]
                                        else:
                                            cx.op("pool", lambda e: e.tensor_tensor(out=t4[:], in0=t2[:], in1=t3[:], op=ALU.subtract),
                                                  reads=[bf["t2"], bf["t3"]], writes=[bf["t4"]])
                                            cx.op("dve", lambda e: e.tensor_tensor(out=t4[:].rearrange("p (c n) -> p c n", c=8),
                                                                                   in0=t4[:].rearrange("p (c n) -> p c n", c=8),
                                                                                   in1=tot.unsqueeze(2).to_broadcast([128, 8, 64]), op=ALU.add),
                                                  reads=[bf["t4"], bf["t3"]], writes=[bf["t4"]])
                                            bb, bbB = t4, bf["t4"]
                                        cx.op("act", lambda e: e.activation(out=dec, in_=tot, func=AF.Exp), reads=[bf["t3"]], writes=[bf["sm"]])
                                        _sv = cx.dead; cx.dead = _sv or not own
                                        cx.op("act", lambda e: e.activation(out=t5[:], in_=bb[:], func=AF.Exp), reads=[bbB], writes=[bf["t5"]])
                                        cx.dead = _sv
                                        cx.op("act", lambda e: e.activation(out=t6[:], in_=bb[:], func=AF.Exp, scale=-1.0), reads=[bbB], writes=[bf["t6"]])
                                        _sv = cx.dead; cx.dead = _sv or not own
                                        cx.op("dve", lambda e: e.tensor_tensor(out=tr["qd"][:], in0=tt["qs"][:], in1=t5[:], op=ALU.mult),
                                              reads=[bf["qs"], bf["t5"]], writes=[bf["qd"]])
                                        cx.dead = _sv
                                        cx.op("dve", lambda e: e.tensor_tensor(out=tr["kd"][:], in0=t1[:], in1=t6[:], op=ALU.mult),
                                              reads=[bf["t1"], bf["t6"]], writes=[bf["kd"]])
                                        cx.op("dve", lambda e: e.tensor_tensor(out=ke[:].rearrange("p (c n) -> p c n", c=8),
                                                                               in0=tr["kd"][:].bitcast(F32).rearrange("p (c n) -> p c n", c=8),
                                                                               in1=dec.unsqueeze(2).to_broadcast([128, 8, 64]), op=ALU.mult),
                                              reads=[bf["kd"], bf["sm"]], writes=[bf["ke"]])
                                        cx.op("dve", lambda e: e.tensor_tensor_scan(out=inc8, data0=ones8[:], data1=tot, initial=0.0, op0=ALU.mult, op1=ALU.add),
                                              reads=[bf["t3"], kB, bf["sm"]], writes=[bf["sm"]])
                                        cx.op("act", lambda e: e.activation(out=dt1, in_=inc8[:, 7:8], func=AF.Exp), reads=[bf["sm"]], writes=[bf["sm"]])
                                        _sv = cx.dead; cx.dead = _sv or not own
                                        if fwd:
                                            cx.op("dve", lambda e: e.tensor_tensor(out=ex8, in0=inc8, in1=tot, op=ALU.subtract), reads=[bf["sm"], bf["t3"]], writes=[bf["sm"]])
                                        else:
                                            cx.op("dve", lambda e: e.tensor_scalar(ex8, inc8, -1.0, inc8[:, 7:8], op0=ALU.mult, op1=ALU.add), reads=[bf["sm"]], writes=[bf["sm"]])
                                        cx.op("act", lambda e: e.activation(out=ex8, in_=ex8, func=AF.Exp), reads=[bf["sm"]], writes=[bf["sm"]])
                                        cx.op("dve", lambda e: e.tensor_tensor(out=tr["qg"][:].rearrange("p (c n) -> p c n", c=8),
                                                                                in0=tr["qd"][:].bitcast(F32).rearrange("p (c n) -> p c n", c=8),
                                                                                in1=ex8.unsqueeze(2).to_broadcast([128, 8, 64]), op=ALU.mult),
                                              reads=[bf["qd"], bf["sm"]], writes=[bf["qg"]])
                                        cx.dma("pool", lambda q: q.dma_start(out=qdgT[di, hs, tsl], in_=tr["qg"][:]), reads=[bf["qg"]])
                                        cx.dead = _sv
                                        chk(21)
                                        kp, kpB = pbank()
                                        cx.pe([(lambda t, p=p: t.transpose(out=kp[:, p * 128:(p + 1) * 128], in_=ke[:, p * 128:(p + 1) * 128], identity=ident[:]))
                                               for p in range(4)], reads=[bf["ke"], cB], writes=[kpB])
                                        cx.op("dve", lambda e: e.tensor_scalar(kend[s][:, 0].rearrange("p a b -> p (a b)"), kp[:, :], maskf[:, 63:64], None, op0=ALU.mult),
                                              reads=[kpB, kB], writes=[bf["kend"]])
                                        cx.op("dve", lambda e: e.tensor_scalar(kend[s][:, 1].rearrange("p a b -> p (a b)"), kp[:, :], maskb[:, 64:65], None, op0=ALU.mult),
                                              reads=[kpB, kB, bf["kend"]], writes=[bf["kend"]])
                                        chk(22)
                                        _sv = cx.dead; cx.dead = _sv or not own
                                        ap_, apB = pbank()
                                        cx.pe([(lambda t, p=p: t.matmul(ap_[:, p * 128:(p + 1) * 128], tr["kd"][:, p * 128:(p + 1) * 128],
                                                                        tr["qd"][:, p * 128:(p + 1) * 128], start=True, stop=True)) for p in range(4)],
                                              reads=[bf["kd"], bf["qd"]], writes=[apB])
                                        mk = maskf if fwd else maskb
                                        cx.op("dve", lambda e: e.tensor_tensor(out=tr["AT"][:], in0=ap_[:, :], in1=mk[:], op=ALU.mult), reads=[apB, kB], writes=[bf["AT"]])
                                        chk(23)
                                        vv = vt[va]
                                        vcs = slice(hh * 128, (hh + 1) * 128)
                                        fns = []
                                        for p in range(4):
                                            fns.append(lambda t, p=p, st_=first_o[0] and p == 0: t.matmul(ot[:, p * 128:(p + 1) * 128], vv[:, p, vcs],
                                                                                                          tr["AT"][:, p * 128:(p + 1) * 128], start=st_, stop=False,
                                                                                                          skip_group_check=True))
                                        first_o[0] = False
                                        cx.pe(fns, reads=[vtB[va], bf["AT"]], writes=[oB])
                                        cx.dead = _sv
                                        chk(24)
                                        dps = []
                                        for half in range(2):
                                            dp, dpB = pbank()
                                            dps.append((dp, dpB))
                                            fns = []
                                            for j in range(4):
                                                n = half * 4 + j
                                                p, off = n // 2, (n % 2) * 64
                                                fns.append(lambda t, j=j, p=p, off=off: t.matmul(dp[:, j * 128:(j + 1) * 128], kend[s][:, off // 64, p, :],
                                                                                                 vv[:, p, vcs], start=True, stop=True))
                                            cx.pe(fns, reads=[bf["kend"], vtB[va]], writes=[dpB])
                                        chk(25)
                                        S = Sst[s]
                                        order = list(range(8)) if fwd else list(range(7, -1, -1))
                                        cx.op("pool", lambda e: e.memset(S[:, 0, :].bitcast(F32), 0.0), writes=[bf["S"]])
                                        for step, n in enumerate(order):
                                            dp, dpB = dps[n // 4]
                                            cx.op("dve", lambda e, step=step, n=n, dp=dp: e.scalar_tensor_tensor(
                                                out=S[:, step + 1, :], in0=S[:, step, :].bitcast(F32), scalar=dec[:, n:n + 1],
                                                in1=dp[:, (n % 4) * 128:(n % 4 + 1) * 128], op0=ALU.mult, op1=ALU.add),
                                                reads=[bf["S"], bf["sm"], dpB], writes=[bf["S"]])
                                            if step < 7 and own:
                                                n2 = order[step + 1]
                                                cx.pe([lambda t, step=step, n2=n2: t.matmul(ot[:, n2 * 64:(n2 + 1) * 64], S[:, step + 1, :],
                                                                                            tr["qd"][:, n2 * 64:(n2 + 1) * 64], start=False, stop=False,
                                                                                            skip_group_check=True)],
                                                      reads=[bf["S"], bf["qd"]], writes=[oB])
                                        chk(26)
                                        row0 = ((b * 2 + di) * NH + h) * 128
                                        cx.dma("pool", lambda q: q.dma_start(out=Gd[row0:row0 + 128, 0:128], in_=S[:, 8, :].bitcast(F32)), reads=[bf["S"]])
                                        cx.dma("pool", lambda q: q.dma_start(out=Gd[row0:row0 + 128, 128:129], in_=dt1, allow_slow_non_contiguous=True), reads=[bf["sm"]])
                                for hh in range(2):
                                    h, s, tt, tr, bf, hs, ot, oB, first_o = HV[hh]
                                    _sv = cx.dead; cx.dead = _sv or not own
                                    cx.op("act", lambda e: e.activation(out=tt["ol"][:], in_=ot[:, :], func=AF.Copy), reads=[oB], writes=[bf["ol"]])
                                    cx.dma("pool", lambda q: q.dma_start(out=olocT[hs, tsl], in_=tt["ol"][:]), reads=[bf["ol"]])
                                    cx.dead = _sv
                            cx.barrier()
                    cx.barrier()

            chk(3)
            cx.barrier()
            chk(4)
            with contextlib.ExitStack() as st:
                memT = sb(st, "memT", [128, KC, MEM], F32R)
                mB = Buf()
                norm_transpose("nm", lambda r0, n: mem[r0:r0 + n, :], [(0, 128, 0), (128, 128, 128)], gains["g_mem"], memT, mB)
                wbufs = [sb(st, "wK%d" % i, [128, KC, 256], F32R) for i in range(2)]
                wB = [Buf(), Buf()]
                kv = [sb(st, "kv%d" % i, [128, 256], F32R) for i in range(2)]
                kvB = [Buf(), Buf()]
                ci = [0]

                def ep_k(cb, pt, pB, t0, tn):
                    a = ci[0] % 2
                    ci[0] += 1
                    cx.op("act", lambda e: e.activation(out=kv[a][:], in_=pt[:, 0:256], func=AF.Copy), reads=[pB], writes=[kvB[a]])
                    cx.dma("pool", lambda q: q.dma_start(out=kT_d[cb * 128:(cb + 1) * 128, :], in_=kv[a][:]), reads=[kvB[a]])
                gemm_fm(memT, mB, MEM, wk, D, ep_k, wbufs, wB)

                def ep_vm(c0, wcur, ti, pt, pB):
                    a = ci[0] % 2
                    ci[0] += 1
                    cx.op("dve", lambda e: e.tensor_copy(kv[a][:], pt[:, 0:256]), reads=[pB], writes=[kvB[a]])
                    cx.dma("pool", lambda q: q.dma_start(out=v_d[ti * 128:(ti + 1) * 128, c0:c0 + 256], in_=kv[a][:]), reads=[kvB[a]])
                gemm_tm(memT, mB, MEM, wv, D, ep_vm, wbufs, wB)
                cx.barrier()

            chk(5)
            with contextlib.ExitStack() as st:
                cmk = sb(st, "cmk", [128, NB, 2 * NVB])
                G = [sb(st, "G%d" % i, [128, NVB * 2, 129]) for i in range(1)]
                acf = sb(st, "acf", [128, NVB])
                msk = sb(st, "msk", [128, 128])
                Sin = [sb(st, "Sin%d" % i, [128, 128]) for i in range(2)]
                SinR = [sb(st, "SinR%d" % i, [128, 128], F32R) for i in range(2)]
                qg = [sb(st, "qgl%d" % i, [128, 512], F32R) for i in range(2)]
                ol = sb(st, "oll", [128, 512])
                gsl = sb(st, "gsl", [128, 512])
                sq = sb(st, "sq", [128, 512], F32R)
                rs = sb(st, "rs", [128, 512])
                yo = sb(st, "yo", [128, 512], F32R)
                cmB, GB, acB, mskB, olB, gslB, sqB, rsB, yoB = (Buf() for _ in range(9))
                SinB, SinRB, qgB = [Buf(), Buf()], [Buf(), Buf()], [Buf(), Buf()]
                cx.dma("sp", lambda q: q.dma_start(out=cmk[:].rearrange("p a b -> p (a b)"),
                                                   in_=cmask_d.rearrange("a b -> (a b)").partition_broadcast(128)), writes=[cmB])
                gdv = Gd.rearrange("(x h k) c -> k x h c", h=NH, k=128)
                for h in range(NH):
                    cx.dma("sp", lambda q: q.dma_start(out=G[0][:], in_=gdv[:, :, h, :]), writes=[GB])
                    Gv = G[0][:].rearrange("p (v d) c -> p v d c", d=2)
                    for b in range(NB):
                        tsl = slice(b * TB, (b + 1) * TB)
                        hs = slice(h * 128, (h + 1) * 128)
                        for di in range(2):
                            mrow = cmk[:, b, di * NVB:(di + 1) * NVB]
                            cx.op("dve", lambda e: e.tensor_scalar(acf[:], Gv[:, :, di, 128], -1.0, None, op0=ALU.add), reads=[GB], writes=[acB])
                            cx.op("dve", lambda e: e.tensor_tensor(out=acf[:], in0=acf[:], in1=mrow, op=ALU.mult), reads=[acB, cmB], writes=[acB])
                            cx.op("dve", lambda e: e.tensor_scalar(acf[:], acf[:], 1.0, None, op0=ALU.add), reads=[acB], writes=[acB])
                            cx.op("pool", lambda e: e.memset(Sin[di][:], 0.0), writes=[SinB[di]])
                            order = (list(range(NB, NVB)) + list(range(NB))) if di == 0 else (list(range(NVB - 1, NB - 1, -1)) + list(range(NB - 1, -1, -1)))
                            for v in order:
                                cx.op("pool", lambda e, v=v: e.tensor_scalar(msk[:], Gv[:, v, di, 0:128], mrow[:, v:v + 1], None, op0=ALU.mult),
                                      reads=[GB, cmB], writes=[mskB])
                                cx.op("dve", lambda e, v=v: e.scalar_tensor_tensor(out=Sin[di][:], in0=Sin[di][:], scalar=acf[:, v:v + 1], in1=msk[:],
                                                                                   op0=ALU.mult, op1=ALU.add),
                                      reads=[SinB[di], acB, mskB], writes=[SinB[di]])
                            cx.op("act", lambda e: e.activation(out=SinR[di][:], in_=Sin[di][:], func=AF.Copy), reads=[SinB[di]], writes=[SinRB[di]])
                            cx.dma("sp", lambda q: q.dma_start(out=qg[di][:], in_=qdgT[di, hs, tsl]), writes=[qgB[di]])
                        cx.dma("sp", lambda q: q.dma_start(out=ol[:], in_=olocT[hs, tsl]), writes=[olB])
                        cx.dma("sp", lambda q: q.dma_start(out=gsl[:], in_=gsT[hs, tsl]), writes=[gslB])
                        pt, pB = pbank()
                        cx.pe([(lambda t, di=di: t.matmul(pt[:, :], SinR[di][:], qg[di][:], start=(di == 0), stop=(di == 1))) for di in range(2)],
                              reads=SinRB + qgB, writes=[pB])
                        cx.op("dve", lambda e: e.tensor_tensor(out=ol[:], in0=pt[:, :], in1=ol[:], op=ALU.add), reads=[pB, olB], writes=[olB])
                        cx.op("act", lambda e: e.activation(out=sq[:], in_=ol[:], func=AF.Square), reads=[olB], writes=[sqB])
                        p2, p2B = pbank()
                        cx.pe([lambda t: t.matmul(p2[:, :], ones[:], sq[:], start=True, stop=True)], reads=[cB, sqB], writes=[p2B])
                        cx.op("dve", lambda e: e.tensor_scalar(rs[:], p2[:, :], 1.0 / 128, EPS, op0=ALU.mult, op1=ALU.add), reads=[p2B], writes=[rsB])
                        cx.op("act", lambda e: e.activation(out=rs[:], in_=rs[:], func=AF.Sqrt), reads=[rsB], writes=[rsB])
                        cx.op("dve", lambda e: e.reciprocal(rs[:], rs[:]), reads=[rsB], writes=[rsB])
                        cx.op("dve", lambda e: e.tensor_tensor(out=rs[:], in0=rs[:], in1=ol[:], op=ALU.mult), reads=[rsB, olB], writes=[rsB])
                        cx.op("dve", lambda e: e.tensor_tensor(out=yo[:], in0=rs[:], in1=gsl[:], op=ALU.mult), reads=[rsB, gslB], writes=[yoB])
                        cx.dma("pool", lambda q: q.dma_start(out=mixT[PW + h * 128:PW + (h + 1) * 128, tsl], in_=yo[:]), reads=[yoB])
                cx.barrier()

            chk(6)
            def proj_residual(name, AT_d, W_d, res_src, dst_d, b):
                tsl = slice(b * TB, (b + 1) * TB)
                with contextlib.ExitStack() as st:
                    AT = sb(st, name + "AT", [128, KC, 512], F32R)
                    wbufs = [sb(st, name + "w%d" % i, [128, KC, 256], F32R) for i in range(2)]
                    xr = [sb(st, name + "xr%d" % i, [128, 4, 256]) for i in range(2)]
                    ATB, wB, xrB = Buf(), [Buf(), Buf()], [Buf(), Buf()]
                    cx.dma("sp", lambda q: q.dma_start(out=AT[:], in_=AT_d[:, tsl].rearrange("(kc p) t -> p kc t", p=128)), writes=[ATB])
                    st_ = {}

                    def ep(c0, wcur, ti, pt, pB):
                        a = (c0 // 256) % 2
                        if ti == 0:
                            cx.dma("sp", lambda q: q.dma_start(out=xr[a][:], in_=res_src(b)[:, c0:c0 + 256].rearrange("(t p) c -> p t c", p=128)),
                                   writes=[xrB[a]])
                        cx.op("dve", lambda e: e.tensor_tensor(out=xr[a][:, ti, :], in0=pt[:, 0:256], in1=xr[a][:, ti, :], op=ALU.add),
                              reads=[pB, xrB[a]], writes=[xrB[a]])
                        if ti == 3:
                            cx.dma("pool", lambda q: q.dma_start(out=dst_d[tsl, c0:c0 + 256].rearrange("(t p) c -> p t c", p=128), in_=xr[a][:]),
                                   reads=[xrB[a]])
                    gemm_tm(AT, ATB, 512, W_d, D, ep, wbufs, wB)
                    cx.barrier()

            for b in range(NB):
                tsl = slice(b * TB, (b + 1) * TB)
                proj_residual("s5", mixT, w_out, lambda b: xh[b, 8:520, :], x1_d, b)
                chk(7)
                with contextlib.ExitStack() as st:
                    h1T = sb(st, "h1T", [128, KC, 512], F32R)
                    h1B = Buf()
                    norm_transpose("n2", lambda r0, n: x1_d[b * TB + r0:b * TB + r0 + n, :], [(r, 128, r) for r in range(0, 512, 128)],
                                   gains["g_xat"], h1T, h1B)
                    wbufs = [sb(st, "wq%d" % i, [128, KC, 256], F32R) for i in range(2)]
                    wB = [Buf(), Buf()]
                    qo = [sb(st, "qo%d" % i, [128, 512], F32R) for i in range(2)]
                    qoB = [Buf(), Buf()]
                    ci = [0]

                    def ep_q(cb, pt, pB, t0, tn):
                        a = ci[0] % 2
                        ci[0] += 1
                        if a == 0:
                            cx.op("act", lambda e: e.activation(out=qo[a][:], in_=pt[:, :], func=AF.Copy), reads=[pB], writes=[qoB[a]])
                        else:
                            cx.op("dve", lambda e: e.tensor_copy(qo[a][:], pt[:, :]), reads=[pB], writes=[qoB[a]])
                        cx.dma("pool", lambda q: q.dma_start(out=qT_d[cb * 128:(cb + 1) * 128, tsl], in_=qo[a][:]), reads=[qoB[a]])
                    gemm_fm(h1T, h1B, 512, wq, D, ep_q, wbufs, wB)
                    cx.barrier()
                chk(8)
                with contextlib.ExitStack() as st:
                    qh = sb(st, "qh", [128, XC, 512], F32R)
                    kh = sb(st, "kh", [128, XC, MEM], F32R)
                    vh = sb(st, "vh", [128, 2, XD], F32R)
                    pe_ = [sb(st, "pe%d" % i, [128, MEM]) for i in range(2)]
                    pT = sb(st, "pT", [128, 2, 512], F32R)
                    oh = [sb(st, "oh%d" % i, [128, 512], F32R) for i in range(2)]
                    smx = sb(st, "smx", [128, 16])
                    qhB, khB, vhB, pTB = Buf(), Buf(), Buf(), Buf()
                    peB, ohB = [Buf(), Buf()], [Buf(), Buf()]
                    smB = [Buf() for _ in range(4)]
                    sc = float(XD) ** -0.5
                    for hx in range(XH):
                        es_ = slice(hx * XD, (hx + 1) * XD)
                        cx.dma("sp", lambda q: q.dma_start(out=qh[:], in_=qT_d[es_, tsl].rearrange("(kc p) t -> p kc t", p=128)), writes=[qhB])
                        cx.dma("sp", lambda q: q.dma_start(out=kh[:], in_=kT_d[es_, :].rearrange("(kc p) t -> p kc t", p=128)), writes=[khB])
                        cx.dma("sp", lambda q: q.dma_start(out=vh[:], in_=v_d[:, es_].rearrange("(mt p) c -> p mt c", p=128)), writes=[vhB])
                        for ti in range(4):
                            a = ti % 2
                            pt, pB = pbank()
                            cx.pe([(lambda t, k=k: t.matmul(pt[:, 0:MEM], qh[:, k, ti * 128:(ti + 1) * 128], kh[:, k, :], start=(k == 0), stop=(k == XC - 1)))
                                   for k in range(XC)], reads=[qhB, khB], writes=[pB])
                            mx, nmx, ssum = smx[:, ti * 4:ti * 4 + 1], smx[:, ti * 4 + 1:ti * 4 + 2], smx[:, ti * 4 + 2:ti * 4 + 3]
                            cx.op("dve", lambda e: e.tensor_reduce(out=mx, in_=pt[:, 0:MEM], axis=AX.X, op=ALU.max), reads=[pB], writes=[smB[ti]])
                            cx.op("dve", lambda e: e.tensor_scalar(nmx, mx, -sc, None, op0=ALU.mult), reads=[smB[ti]], writes=[smB[ti]])
                            cx.op("act", lambda e: e.activation(out=pe_[a][:], in_=pt[:, 0:MEM], func=AF.Exp, bias=nmx, scale=sc, accum_out=ssum),
                                  reads=[pB, smB[ti]], writes=[peB[a], smB[ti]])
                            cx.op("dve", lambda e: e.reciprocal(ssum, ssum), reads=[smB[ti]], writes=[smB[ti]])
                            cx.op("dve", lambda e: e.tensor_scalar(pe_[a][:], pe_[a][:], ssum, None, op0=ALU.mult), reads=[peB[a], smB[ti]], writes=[peB[a]])
                            p2, p2B = pbank()
                            cx.pe([(lambda t, m=m: t.transpose(out=p2[:, m * 128:(m + 1) * 128], in_=pe_[a][:, m * 128:(m + 1) * 128], identity=ident[:]))
                                   for m in range(2)], reads=[peB[a], cB], writes=[p2B])
                            cx.op("act", lambda e: e.activation(out=pT[:, :, ti * 128:(ti + 1) * 128], in_=p2[:, 0:256].rearrange("p (m t) -> p m t", m=2),
                                                                func=AF.Copy), reads=[p2B], writes=[pTB])
                        for eb in range(XC):
                            a = eb % 2
                            pt, pB = pbank()
                            cx.pe([(lambda t, m=m: t.matmul(pt[:, :], vh[:, m, eb * 128:(eb + 1) * 128], pT[:, m, :], start=(m == 0), stop=(m == 1)))
                                   for m in range(2)], reads=[vhB, pTB], writes=[pB])
                            cx.op("dve" if a else "act", (lambda e: e.tensor_copy(oh[a][:], pt[:, :])) if a else
                                  (lambda e: e.activation(out=oh[a][:], in_=pt[:, :], func=AF.Copy)), reads=[pB], writes=[ohB[a]])
                            r0 = hx * XD + eb * 128
                            cx.dma("pool", lambda q: q.dma_start(out=oT_d[r0:r0 + 128, tsl], in_=oh[a][:]), reads=[ohB[a]])
                    cx.barrier()
                chk(9)
                proj_residual("s6", oT_d, wo, lambda b: x1_d[b * TB:(b + 1) * TB, :], x2_d, b)
                chk(10)
                with contextlib.ExitStack() as st:
                    h3T = sb(st, "h3T", [128, KC, 512])
                    h3B = Buf()
                    wr = sb(st, "wr", [128, KC, 36])
                    sl = sb(st, "sl", [128, 128])
                    ebase = sb(st, "ebase", [128, 32])
                    rB = Buf()
                    cx.dma("sp", lambda q: q.dma_start(out=wr[:], in_=wr_d.rearrange("(kc p) c -> p kc c", p=128)), writes=[rB])
                    cx.dma("sp", lambda q: q.dma_start(out=sl[:], in_=sl_d), writes=[rB])
                    cx.dma("sp", lambda q: q.dma_start(out=ebase[:], in_=ebase_d[0].partition_broadcast(128)), writes=[rB])
                    RT = [{n_: sb(st, "%s%d" % (n_, s_), shp) for n_, shp in (("lg", [128, 36]), ("oh4", [128, 4]), ("t32", [128, 32]), ("sel", [128, 8]),
                                                                              ("m8", [128, 8]), ("o1", [128, 8]), ("o2", [128, 8]), ("M1", [128, 32]),
                                                                              ("M2", [128, 32]), ("pos", [128, 32]), ("sc", [128, 16]))} for s_ in range(2)]
                    idf = [sb(st, "idf%d" % s_, [128, 2]) for s_ in range(2)]
                    RB = [Buf(), Buf()]

                    for ti in range(4):
                        gt = b * 4 + ti
                        norm_transpose("n3", lambda r0, n: x2_d[b * TB + r0:b * TB + r0 + n, :], [(ti * 128, 128, ti * 128)], gains["g_moe"], h3T, h3B)
                        R = RT[ti % 2]
                        rb = RB[ti % 2]
                        pt, pB = pbank()
                        cx.pe([(lambda t, k=k: t.matmul(pt[:, 0:36], h3T[:, k, ti * 128:(ti + 1) * 128], wr[:, k, :], start=(k == 0), stop=(k == KC - 1)))
                               for k in range(KC)], reads=[h3B, rB], writes=[pB])
                        lg, oh4, t32, sel, m8, o1, o2, M1, M2, pos, scr = (R[k] for k in ("lg", "oh4", "t32", "sel", "m8", "o1", "o2", "M1", "M2", "pos", "sc"))
                        V = lambda f: cx.op("dve", f, reads=[rb, rB, MallB], writes=[rb])
                        cx.op("dve", lambda e: e.tensor_copy(lg[:], pt[:, 0:36]), reads=[pB], writes=[rb])
                        gmx, gsum, gw, d21, w1c, w2c = (scr[:, j:j + 1] for j in range(6))
                        V(lambda e: e.tensor_reduce(out=gmx, in_=lg[:, 0:4], axis=AX.X, op=ALU.max))
                        V(lambda e: e.tensor_scalar(oh4[:], lg[:, 0:4], gmx, None, op0=ALU.is_equal))
                        V(lambda e: e.tensor_scalar(scr[:, 6:7], gmx, -1.0, None, op0=ALU.mult))
                        cx.op("act", lambda e: e.activation(out=scr[:, 8:12], in_=lg[:, 0:4], func=AF.Exp, bias=scr[:, 6:7], scale=1.0, accum_out=gsum),
                              reads=[rb], writes=[rb])
                        V(lambda e: e.reciprocal(gw, gsum))
                        V(lambda e: e.tensor_tensor(out=t32[:].rearrange("p (g j) -> p g j", g=4), in0=lg[:, 4:36].rearrange("p (g j) -> p g j", g=4),
                                                    in1=oh4[:].unsqueeze(2).to_broadcast([128, 4, 8]), op=ALU.mult))
                        V(lambda e: e.tensor_reduce(out=sel[:], in_=t32[:].rearrange("p (g j) -> p j g", g=4), axis=AX.X, op=ALU.add))
                        V(lambda e: e.max(out=m8[:], in_=sel[:]))
                        V(lambda e: e.tensor_scalar(o1[:], sel[:], m8[:, 0:1], None, op0=ALU.is_equal))
                        V(lambda e: e.tensor_scalar(o2[:], sel[:], m8[:, 1:2], None, op0=ALU.is_equal))
                        V(lambda e: e.tensor_tensor(out=d21, in0=m8[:, 1:2], in1=m8[:, 0:1], op=ALU.subtract))
                        cx.op("act", lambda e: e.activation(out=d21, in_=d21, func=AF.Exp), reads=[rb], writes=[rb])
                        V(lambda e: e.tensor_scalar(d21, d21, 1.0, None, op0=ALU.add))
                        V(lambda e: e.reciprocal(w1c, d21))
                        V(lambda e: e.tensor_scalar(w2c, w1c, -1.0, 1.0, op0=ALU.mult, op1=ALU.add))
                        cx.op("dve", lambda e: e.tensor_scalar(cw_all[:, gt * 2:gt * 2 + 1], w1c, gw, None, op0=ALU.mult), reads=[rb], writes=[cwB])
                        cx.op("dve", lambda e: e.tensor_scalar(cw_all[:, gt * 2 + 1:gt * 2 + 2], w2c, gw, None, op0=ALU.mult), reads=[rb], writes=[cwB])
                        for Mx, ox in ((M1, o1), (M2, o2)):
                            V(lambda e, Mx=Mx, ox=ox: e.tensor_tensor(out=Mx[:].rearrange("p (g j) -> p g j", g=4),
                                                                      in0=oh4[:].unsqueeze(2).to_broadcast([128, 4, 8]),
                                                                      in1=ox[:].unsqueeze(1).to_broadcast([128, 4, 8]), op=ALU.mult))
                        cx.op("dve", lambda e: e.tensor_tensor(out=Mall[:, gt, :], in0=M1[:], in1=M2[:], op=ALU.add), reads=[rb], writes=[MallB])
                        pp, ppB = pbank()
                        fns = [(lambda t, j=j: t.matmul(pp[:, 0:32], ones[:].bitcast(F32), Mall[:, j, :], start=(j == 0), stop=False)) for j in range(gt)]
                        fns.append(lambda t: t.matmul(pp[:, 0:32], sl[:], Mall[:, gt, :], start=(gt == 0), stop=True))
                        cx.pe(fns, reads=[MallB, cB, rB], writes=[ppB])
                        cx.op("dve", lambda e: e.tensor_tensor(out=pos[:], in0=pp[:, 0:32], in1=ebase[:], op=ALU.add), reads=[ppB, rB], writes=[rb])
                        for j, Mx in enumerate((M1, M2)):
                            V(lambda e, Mx=Mx: e.tensor_tensor(out=t32[:], in0=pos[:], in1=Mx[:], op=ALU.mult))
                            V(lambda e, j=j: e.tensor_reduce(out=idf[ti % 2][:, j:j + 1], in_=t32[:], axis=AX.X, op=ALU.add))
                        cx.op("dve", lambda e: e.tensor_copy(idx_all[:, gt * 2:gt * 2 + 2], idf[ti % 2][:]), reads=[rb], writes=[idxB])
                    def scat(i, xs_, xsB_, n):
                        gt = b * 4 + i
                        for j in range(2):
                            cx.dma("pool", lambda q, j=j: q.indirect_dma_start(out=Xs_d[:, :], out_offset=bass.IndirectOffsetOnAxis(ap=idx_all[:, gt * 2 + j:gt * 2 + j + 1], axis=0),
                                                                              in_=xs_[:, :], in_offset=None), reads=[xsB_, idxB])
                    norm_transpose("n4", lambda r0, n: x2_d[b * TB + r0:b * TB + r0 + n, :], [(r, 128, r) for r in range(0, 512, 128)], gains["g_moe"],
                                   None, None, keep_rows=scat)
                    cx.barrier()

            chk(11)
            with contextlib.ExitStack() as st:
                Xe = [sb(st, "Xe%d" % i, [128, D]) for i in range(2)]
                XeT = sb(st, "XeT", [128, KC, 128], F32R)
                wbufs = [sb(st, "wE%d" % i, [128, KC, 256], F32R) for i in range(3)]
                hm = sb(st, "hm", [128, DE])
                a_s = sb(st, "a_s", [128, DE])
                hmT = sb(st, "hmT", [128, DC, 128], F32R)
                ye = [sb(st, "ye%d" % i, [128, D]) for i in range(2)]
                XeB, XeTB, hmB, asB, hmTB = [Buf(), Buf()], Buf(), Buf(), Buf(), Buf()
                wB = [Buf(), Buf(), Buf()]
                yeB = [Buf(), Buf()]
                wtag = [0]
                for e_ in range(NE):
                    a = e_ % 2
                    cx.dma("sp", lambda q: q.dma_start(out=Xe[a][:], in_=Xs_d[e_ * 128:(e_ + 1) * 128, :]), writes=[XeB[a]])
                    for k0 in range(0, KC, 4):
                        pt, pB = pbank()
                        cx.pe([(lambda t, c=c: t.transpose(out=pt[:, c * 128:(c + 1) * 128], in_=Xe[a][:, (k0 + c) * 128:(k0 + c + 1) * 128], identity=ident[:]))
                               for c in range(4)], reads=[XeB[a], cB], writes=[pB])
                        src = pt[:, :].rearrange("p (c n) -> p c n", c=4)
                        if (k0 // 4) % 2:
                            cx.op("dve", lambda e: e.tensor_copy(XeT[:, k0:k0 + 4, :], src), reads=[pB], writes=[XeTB])
                        else:
                            cx.op("act", lambda e: e.activation(out=XeT[:, k0:k0 + 4, :], in_=src, func=AF.Copy), reads=[pB], writes=[XeTB])

                    def ep_a(c0, wcur, ti, pt, pB):
                        cx.op("act", lambda e: e.activation(out=a_s[:, c0:c0 + wcur], in_=pt[:, 0:wcur], func=AF.Silu), reads=[pB], writes=[asB])

                    def ep_c(c0, wcur, ti, pt, pB):
                        cx.op("dve", lambda e: e.tensor_tensor(out=hm[:, c0:c0 + wcur], in0=pt[:, 0:wcur], in1=a_s[:, c0:c0 + wcur], op=ALU.mult),
                              reads=[pB, asB], writes=[hmB])
                    gemm_tm(XeT, XeTB, 128, w1[e_], DE, ep_a, wbufs, wB, wtag)
                    gemm_tm(XeT, XeTB, 128, w3[e_], DE, ep_c, wbufs, wB, wtag)
                    for k0 in range(0, DC, 4):
                        kk = min(4, DC - k0)
                        pt, pB = pbank()
                        cx.pe([(lambda t, c=c: t.transpose(out=pt[:, c * 128:(c + 1) * 128], in_=hm[:, (k0 + c) * 128:(k0 + c + 1) * 128], identity=ident[:]))
                               for c in range(kk)], reads=[hmB, cB], writes=[pB])
                        cx.op("act", lambda e: e.activation(out=hmT[:, k0:k0 + kk, :], in_=pt[:, 0:kk * 128].rearrange("p (c n) -> p c n", c=kk), func=AF.Copy),
                              reads=[pB], writes=[hmTB])
                    WB2 = (KC * 256) // DC
                    WB2 = min(WB2, D)
                    for c0 in range(0, D, WB2):
                        wa = wtag[0] % 3
                        wtag[0] += 1
                        wv_ = wbufs[wa][:].rearrange("p k c -> p (k c)")[:, 0:DC * WB2].rearrange("p (k c) -> p k c", k=DC)
                        load_w(wv_, w2[e_][:, c0:c0 + WB2], wB[wa])
                        for c1 in range(0, WB2, 512):
                            cw = min(512, WB2 - c1)
                            pt, pB = pbank()
                            cx.pe([(lambda t, k=k: t.matmul(pt[:, 0:cw], hmT[:, k, :], wv_[:, k, c1:c1 + cw], start=(k == 0), stop=(k == DC - 1))) for k in range(DC)],
                                  reads=[hmTB, wB[wa]], writes=[pB])
                            dst = ye[a][:, c0 + c1:c0 + c1 + cw]
                            if (c1 // 512) % 2:
                                cx.op("dve", lambda e: e.tensor_copy(dst, pt[:, 0:cw]), reads=[pB], writes=[yeB[a]])
                            else:
                                cx.op("act", lambda e: e.activation(out=dst, in_=pt[:, 0:cw], func=AF.Copy), reads=[pB], writes=[yeB[a]])
                    cx.dma("pool", lambda q: q.dma_start(out=Y_d[e_ * 128:(e_ + 1) * 128, :], in_=ye[a][:]), reads=[yeB[a]])
                cx.barrier()

            chk(12)
            with contextlib.ExitStack() as st:
                gbc = sb(st, "fgbc", [128, D])
                y1 = [sb(st, "y1_%d" % i, [128, D]) for i in range(2)]
                y2 = [sb(st, "y2_%d" % i, [128, D]) for i in range(2)]
                xx = [sb(st, "xx_%d" % i, [128, D]) for i in range(2)]
                junk = sb(st, "fjunk", [128, D])
                ss = sb(st, "fss", [128, 8])
                gB, jB = Buf(), Buf()
                y1B, y2B, xxB = [Buf(), Buf()], [Buf(), Buf()], [Buf(), Buf()]
                ssB = [Buf() for _ in range(8)]
                cx.dma("sp", lambda q: q.dma_start(out=gbc[:], in_=gains["g_fin"][0].partition_broadcast(128)), writes=[gB])
                for gt in range(NB * 4):
                    a = gt % 2
                    cx.dma("sp", lambda q: q.dma_start(out=xx[a][:], in_=x2_d[gt * 128:(gt + 1) * 128, :]), writes=[xxB[a]])
                    for j, (yy, yyB) in enumerate(((y1[a], y1B[a]), (y2[a], y2B[a]))):
                        cx.dma("pool", lambda q, yy=yy, j=j: q.indirect_dma_start(out=yy[:, :], out_offset=None, in_=Y_d[:, :],
                                                                                 in_offset=bass.IndirectOffsetOnAxis(ap=idx_all[:, gt * 2 + j:gt * 2 + j + 1], axis=0)),
                               reads=[idxB], writes=[yyB])
                        cx.op("dve", lambda e, yy=yy, j=j: e.scalar_tensor_tensor(out=xx[a][:], in0=yy[:], scalar=cw_all[:, gt * 2 + j:gt * 2 + j + 1], in1=xx[a][:],
                                                                                 op0=ALU.mult, op1=ALU.add), reads=[yyB, cwB, xxB[a]], writes=[xxB[a]])
                    s1 = ss[:, gt % 8:gt % 8 + 1]
                    sB = ssB[gt % 8]
                    cx.op("act", lambda e: e.activation(out=junk[:], in_=xx[a][:], func=AF.Square, accum_out=s1), reads=[xxB[a]], writes=[jB, sB])
                    rstd_from_ss(s1, 128, sB, 1.0 / D)
                    cx.op("dve", lambda e: e.scalar_tensor_tensor(out=xx[a][:], in0=xx[a][:], scalar=s1, in1=gbc[:], op0=ALU.mult, op1=ALU.mult),
                          reads=[xxB[a], sB, gB], writes=[xxB[a]])
                    cx.dma("sp", lambda q: q.dma_start(out=out_d[gt * 128:(gt + 1) * 128, :], in_=xx[a][:]), reads=[xxB[a]])
                cx.barrier()
        _body()
        cx.dead = False
        cx.barrier()
    return nc


def host_inputs(cfg, inp):
    g = dims(cfg)
    D, NB, NC, TOK, PW, PG, HK, NH, NVB = g["D"], g["NB"], g["NC"], g["TOK"], g["PW"], g["PG"], g["HK"], g["NH"], g["NVB"]
    f = np.float32
    x = np.asarray(inp["x"], f)[0]
    S = x.shape[0]
    xp = np.zeros((S + 16, D), f)
    xp[8:8 + S] = x
    common = dict(
        mem=np.ascontiguousarray(np.asarray(inp["mem"], f)[0]),
        g_mix=np.asarray(inp["norm_mix"], f).reshape(1, D), g_xat=np.asarray(inp["norm_xattn"], f).reshape(1, D),
        g_mem=np.asarray(inp["norm_mem"], f).reshape(1, D), g_moe=np.asarray(inp["norm_moe"], f).reshape(1, D),
        g_fin=np.asarray(inp["norm_final"], f).reshape(1, D),
        w_in=np.asarray(inp["w_in"], f)[0], pool_w=np.asarray(inp["pool_w"], f)[0],
        psc=np.ascontiguousarray(np.asarray(inp["pool_scale"], f)[0].reshape(PW // 128, 128).T),
        lbp=np.ascontiguousarray(np.stack([np.asarray(inp["lb_fwd"], f), np.asarray(inp["lb_bwd"], f)], 0).reshape(2, 2, NH, 128).transpose(3, 0, 1, 2)),
        hgn=np.ascontiguousarray(np.asarray(inp["hgrn_norm"], f)[0].reshape(NH, 128).T),
        w_out=np.asarray(inp["w_out"], f)[0], wq=np.asarray(inp["w_q"], f)[0], wk=np.asarray(inp["w_k"], f)[0],
        wv=np.asarray(inp["w_v"], f)[0], wo=np.asarray(inp["w_o"], f)[0],
        wr=np.ascontiguousarray(np.concatenate([np.asarray(inp["w_router_group"], f)[0],
                                                np.asarray(inp["w_router_expert"], f)[0].transpose(1, 0, 2).reshape(D, 32)], 1)),
        w1=np.asarray(inp["w1"], f)[0], w3=np.asarray(inp["w3"], f)[0], w2=np.asarray(inp["w2"], f)[0],
        ident=np.eye(128, dtype=f), ones=np.ones((128, 128), f),
        sl=np.triu(np.ones((128, 128), f), 1),
        ebase=(np.arange(32, dtype=f) * 128).reshape(1, 32),
    )
    i = np.arange(128)
    same = (i[:, None] // 64) == (i[None, :] // 64)
    common["maskf"] = np.tile((same & (i[:, None] <= i[None, :])).astype(f), (1, 4))
    common["maskb"] = np.tile((same & (i[:, None] >= i[None, :])).astype(f), (1, 4))
    common["rmask"] = np.tile(((np.arange(512) % 64) != 0).astype(f)[None, :], (128, 1))
    t = np.arange(S)
    inv = np.zeros((4, S), f)
    for gi, w in enumerate((2, 4, 8, 16)):
        lo = np.clip(t - w // 2, 0, S - 1)
        hi = np.clip(t + w // 2 - 1, 0, S - 1)
        inv[gi] = 1.0 / (hi - lo + 1)
    maps = []
    for c in range(NC):
        m = dict(common)
        own = [c * NB + b for b in range(NB)]
        slots = own + [v for v in range(NVB) if v not in own]
        m["xh"] = np.stack([xp[v * TB: v * TB + 528] for v in slots], 0)
        m["invcnt"] = np.stack([inv[:, v * TB:(v + 1) * TB].reshape(-1) for v in own], 0)
        cm = np.zeros((NB, 2, NVB), f)
        for b in range(NB):
            for j, v in enumerate(slots):
                cm[b, 0, j] = 1.0 if v < own[b] else 0.0
                cm[b, 1, j] = 1.0 if v > own[b] else 0.0
        m["cmask"] = cm.reshape(NB, 2 * NVB)
        maps.append(m)
    return maps


FULL = dict(D=4096, NB=2, NC=8)
_NC_CACHE = {}


def run(cfg, inp):
    key = tuple(sorted(cfg.items()))
    if key not in _NC_CACHE:
        _NC_CACHE[key] = build(cfg)
    nc = _NC_CACHE[key]
    maps = host_inputs(cfg, inp)
    res = run_bass_kernel_spmd(nc, maps, core_ids=list(range(cfg["NC"])))
    out = np.concatenate([r["out"] for r in res.results], axis=0)
    return out[None].astype(np.float32)


def kernel(**inputs):
    return run(FULL, inputs)
```

```python
import contextlib
import numpy as np
import concourse.bass as bass
import concourse.mybir as mybir
from concourse.bass_utils import run_bass_kernel_spmd

F32 = mybir.dt.float32
F32R = mybir.dt.float32r
I32 = mybir.dt.int32
AF = mybir.ActivationFunctionType
ALU = mybir.AluOpType
AX = mybir.AxisListType
EPS = 1e-6
TB = 512
NSLOT = 8192


class Stop(Exception):
    pass


class Buf:
    __slots__ = ("w", "r")

    def __init__(self):
        self.w = None
        self.r = []


class Ctx:
    def __init__(self, nc, es):
        self.nc = nc
        self.eng = dict(pe=nc.tensor, act=nc.scalar, dve=nc.vector, pool=nc.gpsimd, sp=nc.sync)
        self.sem = {e: es.enter_context(nc.semaphore("s_" + e)) for e in self.eng}
        self.cnt = {e: 0 for e in self.eng}
        self.seen = {e: {} for e in self.eng}
        self.dsem = {q: [es.enter_context(nc.semaphore("d_%s%d" % (q, i))) for i in range(8)] for q in ("sp", "pool")}
        self.dcnt = {q: [0] * 8 for q in ("sp", "pool")}
        self.dnext = {"sp": 0, "pool": 0}
        self.alltoks = []
        self.dead = False

    def wait(self, e, toks):
        best = {}
        for t in toks:
            if t is None:
                continue
            sem, val, key = t
            if e == "pe" and key == "s_pe":
                continue
            if self.seen[e].get(key, 0) >= val:
                continue
            if key not in best or best[key][1] < val:
                best[key] = t
        for key, (sem, val, _) in best.items():
            self.eng[e].wait_ge(sem, val)
            self.seen[e][key] = val

    def _deps(self, reads, writes):
        toks = []
        for b in reads:
            toks.append(b.w)
        for b in writes:
            toks.append(b.w)
            toks.extend(b.r)
        return toks

    def _commit(self, tok, reads, writes):
        for b in reads:
            b.r.append(tok)
        for b in writes:
            b.w = tok
            b.r = []

    def op(self, e, fn, reads=(), writes=()):
        if self.dead:
            return None
        self.wait(e, self._deps(reads, writes))
        inst = fn(self.eng[e])
        self.cnt[e] += 1
        inst.then_inc(self.sem[e], 1)
        tok = (self.sem[e], self.cnt[e], "s_" + e)
        self._commit(tok, reads, writes)
        return tok

    def pe(self, fns, reads=(), writes=()):
        if self.dead:
            return None
        self.wait("pe", self._deps(reads, writes))
        inst = None
        for fn in fns:
            inst = fn(self.nc.tensor)
        self.cnt["pe"] += 1
        inst.then_inc(self.sem["pe"], 1)
        tok = (self.sem["pe"], self.cnt["pe"], "s_pe")
        self._commit(tok, reads, writes)
        return tok

    def dma(self, q, fn, reads=(), writes=()):
        if self.dead:
            return None
        i = self.dnext[q]
        self.dnext[q] = (i + 1) % 8
        sem = self.dsem[q][i]
        key = "d_%s%d" % (q, i)
        toks = self._deps(reads, writes)
        if self.dcnt[q][i] > 0:
            toks.append((sem, self.dcnt[q][i], key))
        self.wait(q, toks)
        inst = fn(self.eng[q])
        self.dcnt[q][i] += 16
        inst.then_inc(sem, 16)
        tok = (sem, self.dcnt[q][i], key)
        self._commit(tok, reads, writes)
        self.alltoks.append(tok)
        return tok

    def barrier(self):
        if self.dead:
            return
        toks = [(self.sem[e], self.cnt[e], "s_" + e) for e in self.eng if self.cnt[e] > 0]
        for q in ("sp", "pool"):
            for i in range(8):
                if self.dcnt[q][i] > 0:
                    toks.append((self.dsem[q][i], self.dcnt[q][i], "d_%s%d" % (q, i)))
        for e in self.eng:
            self.wait(e, [t for t in toks if not (e == "pe" and t[2] == "s_pe")])


def dims(cfg):
    D = cfg["D"]
    d = dict(D=D, NB=cfg["NB"], NC=cfg["NC"], KC=D // 128, TOK=cfg["NB"] * TB, PW=D // 2, PG=D // 8,
             HK=D // 2, NH=D // 256, XH=4, XD=D // 4, XC=D // 512, MEM=256, NE=32, DE=D // 4, DC=D // 512)
    d["NVB"] = d["NC"] * d["NB"]
    return d


def build(cfg):
    g = dims(cfg)
    D, NB, NC, KC, TOK, PW, PG, HK, NH = g["D"], g["NB"], g["NC"], g["KC"], g["TOK"], g["PW"], g["PG"], g["HK"], g["NH"]
    XH, XD, XC, MEM, NE, DE, DC, NVB = g["XH"], g["XD"], g["XC"], g["MEM"], g["NE"], g["DE"], g["DC"], g["NVB"]
    nc = bass.Bass("TRN2", target_bir_lowering=False)
    nc.dge_precook = False

    def din(name, shape, dt=F32):
        return nc.dram_tensor(name, shape, dt, kind="ExternalInput").ap()

    def dsc(name, shape, dt=F32, **kw):
        return nc.dram_tensor(name, shape, dt, **kw).ap()

    xh = din("xh", [NVB, 528, D])
    mem = din("mem", [MEM, D])
    gains = {n: din(n, [1, D]) for n in ("g_mix", "g_xat", "g_mem", "g_moe", "g_fin")}
    w_in = din("w_in", [D, 3 * D], F32R)
    pool_w = din("pool_w", [4, PG, PG], F32R)
    psc_d = din("psc", [128, PW // 128])
    lbp_d = din("lbp", [128, 2, 2, NH])
    hgn_d = din("hgn", [128, NH])
    w_out = din("w_out", [D, D], F32R)
    wq = din("wq", [D, D], F32R)
    wk = din("wk", [D, D], F32R)
    wv = din("wv", [D, D], F32R)
    wo = din("wo", [D, D], F32R)
    wr_d = din("wr", [D, 36])
    w1 = din("w1", [NE, D, DE], F32R)
    w3 = din("w3", [NE, D, DE], F32R)
    w2 = din("w2", [NE, DE, D], F32R)
    ident_d = din("ident", [128, 128])
    maskf_d = din("maskf", [128, 512])
    maskb_d = din("maskb", [128, 512])
    rmask_d = din("rmask", [128, 512])
    ones_d = din("ones", [128, 128], F32R)
    sl_d = din("sl", [128, 128])
    invcnt_d = din("invcnt", [NB, 4 * 512])
    cmask_d = din("cmask", [NB, 2 * NVB])
    ebase_d = din("ebase", [1, 32])
    out_d = nc.dram_tensor("out", [TOK, D], F32, kind="ExternalOutput").ap()

    mixT = dsc("mixT", [D, TOK], F32R)
    olocT = dsc("olocT", [HK, TOK])
    gsT = dsc("gsT", [HK, TOK])
    qdgT = dsc("qdgT", [2, HK, TOK], F32R)
    CCR = NVB * 2 * NH * 128
    Gd = dsc("Gd", [CCR, 129])
    x1_d = dsc("x1s", [TOK, D])
    qT_d = dsc("qTs", [D, TOK], F32R)
    kT_d = dsc("kTs", [D, MEM], F32R)
    v_d = dsc("vs", [MEM, D], F32R)
    oT_d = dsc("oTs", [D, TOK], F32R)
    x2_d = dsc("x2s", [TOK, D])
    Xs_d = dsc("Xs", [NSLOT, D])
    Y_d = dsc("Ys", [NSLOT, D])

    with contextlib.ExitStack() as es:
        cx = Ctx(nc, es)
        cc_sem = es.enter_context(nc.semaphore("cc_sem"))

        uid = [0]

        def sb(st, name, shape, dt=F32):
            uid[0] += 1
            return st.enter_context(nc.sbuf_tensor("%s_u%d" % (name, uid[0]), shape, dt))

        psb = [es.enter_context(nc.psum_tensor("ps%d" % i, [128, 512], F32)) for i in range(8)]
        psB = [Buf() for _ in range(8)]
        pstate = [0]

        def pbank():
            i = pstate[0]
            pstate[0] = (i + 1) % 6
            return psb[i], psB[i]

        ident = sb(es, "ident", [128, 128])
        ones = sb(es, "ones", [128, 128], F32R)
        small = sb(es, "small", [128, 64])
        idx_all = sb(es, "idx_all", [128, 2 * NB * 4], I32)
        cw_all = sb(es, "cw_all", [128, 2 * NB * 4])
        Mall = sb(es, "Mall", [128, NB * 4, 32])
        cB = Buf()
        cx.dma("sp", lambda q: q.dma_start(out=ident[:], in_=ident_d), writes=[cB])
        cx.dma("sp", lambda q: q.dma_start(out=ones[:], in_=ones_d), writes=[cB])
        idxB, cwB, MallB = Buf(), Buf(), Buf()

        def bl(x):
            return list(x) if isinstance(x, (list, tuple)) else [x]

        def load_w(dst, src_ap, B, extra_writes=()):
            return cx.dma("sp", lambda q: q.dma_start(out=dst, in_=src_ap.rearrange("(kc p) c -> p kc c", p=128)),
                          writes=bl(B) + list(extra_writes))

        def rstd_from_ss(ss, n, B, scale):
            cx.op("dve", lambda e: e.tensor_scalar(ss[0:n], ss[0:n], scale, EPS, op0=ALU.mult, op1=ALU.add), reads=[B], writes=[B])
            cx.op("act", lambda e: e.activation(out=ss[0:n], in_=ss[0:n], func=AF.Sqrt), reads=[B], writes=[B])
            cx.op("dve", lambda e: e.reciprocal(ss[0:n], ss[0:n]), reads=[B], writes=[B])

        def norm_transpose(st_name, row_src, tiles, gain_d, hT, hTB, out_dt_rows=None, keep_rows=None):
            with contextlib.ExitStack() as st:
                gbc = sb(st, st_name + "gbc", [128, D])
                xt = [sb(st, st_name + "xt%d" % i, [128, D]) for i in range(2)]
                xs = [sb(st, st_name + "xs%d" % i, [128, D]) for i in range(2)]
                junk = sb(st, st_name + "junk", [128, D])
                ss = sb(st, st_name + "ss", [128, 8])
                gB, jB = Buf(), Buf()
                xtB = [Buf(), Buf()]
                xsB = [Buf(), Buf()]
                ssB = [Buf() for _ in range(8)]
                cx.dma("sp", lambda q: q.dma_start(out=gbc[:], in_=gain_d[0].partition_broadcast(128)), writes=[gB])
                for i, (r0, n, c0) in enumerate(tiles):
                    a = i % 2
                    s1 = ss[:, (i % 8):(i % 8) + 1]
                    sB = ssB[i % 8]
                    cx.dma("sp", lambda q: q.dma_start(out=xt[a][0:n], in_=row_src(r0, n)), writes=[xtB[a]])
                    cx.op("act", lambda e: e.activation(out=junk[0:n], in_=xt[a][0:n], func=AF.Square, accum_out=s1[0:n]),
                          reads=[xtB[a]], writes=[jB, sB])
                    rstd_from_ss(s1, n, sB, 1.0 / D)
                    cx.op("dve", lambda e: e.scalar_tensor_tensor(out=xs[a][0:n], in0=xt[a][0:n], scalar=s1[0:n], in1=gbc[0:n],
                                                                  op0=ALU.mult, op1=ALU.mult),
                          reads=[xtB[a], sB, gB], writes=[xsB[a]])
                    if keep_rows is not None:
                        keep_rows(i, xs[a], xsB[a], n)
                    if hT is None:
                        continue
                    for k0 in range(0, KC, 4):
                        pt, pB = pbank()
                        kk = min(4, KC - k0)
                        cx.pe([(lambda t, c=c: t.transpose(out=pt[:, c * 128:c * 128 + n], in_=xs[a][0:n, (k0 + c) * 128:(k0 + c + 1) * 128],
                                                           identity=ident[0:n, 0:n])) for c in range(kk)],
                              reads=[xsB[a], cB], writes=[pB])
                        src = pt[:, 0:kk * 128].rearrange("p (c n) -> p c n", c=kk)[:, :, 0:n]
                        dst = hT[:, k0:k0 + kk, c0:c0 + n]
                        if (k0 // 4) % 2 == 0:
                            cx.op("act", lambda e: e.activation(out=dst, in_=src, func=AF.Copy), reads=[pB], writes=[hTB])
                        else:
                            cx.op("dve", lambda e: e.tensor_copy(dst, src), reads=[pB], writes=[hTB])
                cx.barrier()

        def gemm_fm(AT, ATB, ntok, W_ap, ncols, epilogue, wbufs, wB, tag=[0]):
            kc_n = AT.shape[1]
            WB = wbufs[0].shape[2]
            for c0 in range(0, ncols, WB):
                a = tag[0] % len(wbufs)
                tag[0] += 1
                wcur = min(WB, ncols - c0)
                load_w(wbufs[a][:, 0:kc_n, 0:wcur], W_ap[:, c0:c0 + wcur], wB[a])
                for c1 in range(0, wcur, 128):
                    for t0 in range(0, ntok, 512):
                        tn = min(512, ntok - t0)
                        pt, pB = pbank()
                        cx.pe([(lambda t, k=k: t.matmul(pt[:, 0:tn], wbufs[a][:, k, c1:c1 + 128], AT[:, k, t0:t0 + tn],
                                                        start=(k == 0), stop=(k == kc_n - 1))) for k in range(kc_n)],
                              reads=bl(wB[a]) + [ATB], writes=[pB])
                        epilogue((c0 + c1) // 128, pt, pB, t0, tn)

        def gemm_tm(AT, ATB, ntok, W_ap, ncols, epilogue, wbufs, wB, tag=[0]):
            kc_n = AT.shape[1]
            WB = wbufs[0].shape[2]
            for c0 in range(0, ncols, WB):
                a = tag[0] % len(wbufs)
                tag[0] += 1
                wcur = min(WB, ncols - c0)
                load_w(wbufs[a][:, 0:kc_n, 0:wcur], W_ap[:, c0:c0 + wcur], wB[a])
                for t0 in range(0, ntok, 128):
                    pt, pB = pbank()
                    cx.pe([(lambda t, k=k: t.matmul(pt[:, 0:wcur], AT[:, k, t0:t0 + 128], wbufs[a][:, k, 0:wcur],
                                                    start=(k == 0), stop=(k == kc_n - 1))) for k in range(kc_n)],
                          reads=bl(wB[a]) + [ATB], writes=[pB])
                    epilogue(c0, wcur, t0 // 128, pt, pB)

        def chk(n):
            if cfg.get("stop", 99) == n:
                cx.barrier()
                cx.dead = True

        def _body():
            for b in range(NVB):
                own = b < NB
                tsl = slice(b * TB, (b + 1) * TB) if own else None
                with contextlib.ExitStack() as st:
                    hT = sb(st, "hT", [128, KC, 528], F32R)
                    hTB = Buf()
                    tiles = [(r, 128, r) for r in range(0, 512, 128)] + [(512, 16, 512)]
                    norm_transpose("n1", lambda r0, n: xh[b, r0:r0 + n, :], tiles, gains["g_mix"], hT, hTB)
                    chk(1)
                    with contextlib.ExitStack() as st2:
                        wbig = sb(st2, "wA", [128, KC, 512], F32R)
                        wslot = [wbig[:, :, j * 128:(j + 1) * 128] for j in range(4)]
                        wsB = [Buf() for _ in range(4)]
                        wbufs = [wbig[:, :, 0:256], wbig[:, :, 256:512]]
                        wB = [[wsB[0], wsB[1]], [wsB[2], wsB[3]]]
                        wtagA = [0]
                        wtagS = [0]
                        with contextlib.ExitStack() as st3:
                          if own:
                            inv = sb(st3, "inv", [128, 4 * 512])
                            psc = sb(st3, "psc", [128, PW // 128])
                            pw = sb(st3, "pw", [128, PG // 128, PG], F32R)
                            uT = [sb(st3, "uT%d" % i, [128, 528]) for i in range(2)]
                            pa = [sb(st3, "pa%d" % i, [128, 528]) for i in range(2)]
                            pb_ = [sb(st3, "pb%d" % i, [128, 528]) for i in range(2)]
                            miT = sb(st3, "miT", [128, PG // 128, 512], F32R)
                            yp = [sb(st3, "yp%d" % i, [128, 512], F32R) for i in range(2)]
                            invB, pscB, pwB, miB = Buf(), Buf(), Buf(), Buf()
                            uB, paB, pbB, ypB = [Buf(), Buf()], [Buf(), Buf()], [Buf(), Buf()], [Buf(), Buf()]
                            cx.dma("sp", lambda q: q.dma_start(out=inv[:], in_=invcnt_d[b].partition_broadcast(128)), writes=[invB])
                            cx.dma("sp", lambda q: q.dma_start(out=psc[:], in_=psc_d), writes=[pscB])
                            cnt = [0]
                            for gi, w in enumerate((2, 4, 8, 16)):
                                cx.dma("sp", lambda q: q.dma_start(out=pw[:], in_=pool_w[gi].rearrange("(kc p) c -> p kc c", p=128)),
                                       writes=[pwB])

                                def ep_pool(cb, pt, pB, t0, tn, gi=gi, w=w):
                                    a = (cnt[0] // 2) % 2
                                    cnt[0] += 1
                                    if tn == 512:
                                        cx.op("act", lambda e: e.activation(out=uT[a][:, 0:512], in_=pt[:, 0:512], func=AF.Copy),
                                              reads=[pB], writes=[uB[a]])
                                        return
                                    cx.op("act", lambda e: e.activation(out=uT[a][:, 512:528], in_=pt[:, 0:16], func=AF.Copy),
                                          reads=[pB], writes=[uB[a]])
                                    cur, curB, m = uT[a], uB[a], 1
                                    alt = [(pa[a], paB[a]), (pb_[a], pbB[a])]
                                    k = 0
                                    while m < w // 2:
                                        nx, nxB = alt[k % 2]
                                        k += 1
                                        cx.op("pool", lambda e, nx=nx, cur=cur, m=m: e.tensor_tensor(out=nx[:, 2 * m - 1:528], in0=cur[:, 2 * m - 1:528],
                                                                                                     in1=cur[:, m - 1:528 - m], op=ALU.add),
                                              reads=[curB], writes=[nxB])
                                        cur, curB, m = nx, nxB, 2 * m
                                    h = w // 2
                                    nx, nxB = alt[k % 2]
                                    cx.op("dve", lambda e: e.tensor_tensor(out=nx[:, 8:520], in0=cur[:, 7:519], in1=cur[:, 7 + h:519 + h], op=ALU.add),
                                          reads=[curB], writes=[nxB])
                                    cx.op("dve", lambda e: e.tensor_tensor(out=nx[:, 8:520], in0=nx[:, 8:520], in1=inv[:, gi * 512:(gi + 1) * 512], op=ALU.mult),
                                          reads=[nxB, invB], writes=[nxB])
                                    cbl = cb
                                    cx.op("dve", lambda e: e.tensor_tensor(out=miT[:, cbl, :], in0=nx[:, 8:520], in1=uT[a][:, 8:520], op=ALU.subtract),
                                          reads=[nxB, uB[a]], writes=[miB])

                                gemm_fm(hT, hTB, 528, w_in[:, gi * PG:(gi + 1) * PG], PG, ep_pool, wbufs, wB, wtagA)
                                for db in range(PG // 128):
                                    pt, pB = pbank()
                                    ncb = PG // 128
                                    cx.pe([(lambda t, c=c: t.matmul(pt[:, :], pw[:, c, db * 128:(db + 1) * 128], miT[:, c, :],
                                                                    start=(c == 0), stop=(c == ncb - 1))) for c in range(ncb)],
                                          reads=[pwB, miB], writes=[pB])
                                    a = db % 2
                                    col = gi * (PG // 128) + db
                                    cx.op("dve", lambda e: e.tensor_scalar(yp[a][:], pt[:, :], psc[:, col:col + 1], None, op0=ALU.mult),
                                          reads=[pB, pscB], writes=[ypB[a]])
                                    r0 = gi * PG + db * 128
                                    cx.dma("pool", lambda q: q.dma_start(out=mixT[r0:r0 + 128, tsl], in_=yp[a][:]), reads=[ypB[a]])
                            cx.barrier()
                        chk(2)
                        with contextlib.ExitStack() as st3:
                            maskf = sb(st3, "maskf", [128, 512])
                            maskb = sb(st3, "maskb", [128, 512])
                            rmask = sb(st3, "rmask", [128, 512])
                            lbp = sb(st3, "lbp", [128, 2, 2, NH])
                            lb = sb(st3, "lb", [128, 2, NH])
                            oml = sb(st3, "oml", [128, 2, NH])
                            hgn = sb(st3, "hgn", [128, NH])
                            ones8 = sb(st3, "ones8", [128, 8])
                            kB = Buf()
                            for t_, d_ in ((maskf, maskf_d), (maskb, maskb_d), (rmask, rmask_d), (lbp, lbp_d), (hgn, hgn_d)):
                                cx.dma("sp", lambda q, t_=t_, d_=d_: q.dma_start(out=t_[:], in_=d_), writes=[kB])
                            cx.op("dve", lambda e: e.memset(ones8[:], 1.0), writes=[kB])
                            cx.op("dve", lambda e: e.tensor_tensor(out=lb[:], in0=lbp[:, :, 0, :], in1=lbp[:, :, 1, :], op=ALU.subtract), reads=[kB], writes=[kB])
                            cx.op("act", lambda e: e.activation(out=lb[:], in_=lb[:], func=AF.Sigmoid), reads=[kB], writes=[kB])
                            cx.op("dve", lambda e: e.tensor_scalar(oml[:], lb[:], -1.0, 1.0, op0=ALU.mult, op1=ALU.add), reads=[kB], writes=[kB])
                            NS = 2
                            names = ["qs", "t1", "t2", "t3", "t4", "t5", "t6"]
                            T = [{n: sb(st3, "%s_%d" % (n, s), [128, 512]) for n in names} for s in range(NS)]
                            TR = [{n: sb(st3, "%s_%d" % (n, s), [128, 512], F32R) for n in ("qd", "kd", "qg")} for s in range(NS)]
                            kend = [sb(st3, "kend%d" % s, [128, 2, 4, 128], F32R) for s in range(NS)]
                            Sst = [sb(st3, "Sst%d" % s, [128, 9, 128], F32R) for s in range(NS)]
                            vt = [sb(st3, "vt%d" % s, [128, 4, 256], F32R) for s in range(2)]
                            sm = [sb(st3, "sm%d" % s, [128, 40]) for s in range(NS)]
                            TBf = [{n: Buf() for n in names + ["qd", "kd", "qg", "kend", "S", "sm"]} for s in range(NS)]
                            for s_ in range(NS):
                                for al, tg in (("gs", "t6"), ("ke", "t2"), ("ol", "t2")):
                                    T[s_][al] = T[s_][tg]
                                    TBf[s_][al] = TBf[s_][tg]
                                TR[s_]["AT"] = TR[s_]["qg"]
                                TBf[s_]["AT"] = TBf[s_]["qg"]
                            vtB = [Buf(), Buf()]
                            psum_hold = {}

                            def ep_hold(key):
                                def ep(cb, pt, pB, t0, tn):
                                    psum_hold[(key, cb)] = (pt, pB)
                                return ep

                            for hp in range(NH // 2):
                                va = hp % 2

                                def ep_v(c0, wcur, ti, pt, pB, va=va):
                                    cx.op("act", lambda e: e.activation(out=vt[va][:, ti, :], in_=pt[:, 0:256], func=AF.Copy), reads=[pB], writes=[vtB[va]])
                                gemm_tm(hT[:, :, 8:520], hTB, 512, w_in[:, PW + 3 * HK + hp * 256:PW + 3 * HK + (hp + 1) * 256], 256, ep_v, wbufs, wB, wtagA)
                                def loadpair(region):
                                    c = PW + region * HK + hp * 256
                                    a = wtagA[0] % 2
                                    wtagA[0] += 1
                                    load_w(wbufs[a], w_in[:, c:c + 256], wB[a])
                                    return a

                                def projh(a, hh):
                                    pt, pB = pbank()
                                    cx.pe([(lambda t, k=k: t.matmul(pt[:, :], wbufs[a][:, k, hh * 128:(hh + 1) * 128], hT[:, k, 8:520], start=(k == 0), stop=(k == KC - 1)))
                                           for k in range(KC)], reads=bl(wB[a]) + [hTB], writes=[pB])
                                    return pt, pB
                                HV = []
                                for hh in range(2):
                                    h = hp * 2 + hh
                                    s = h % NS
                                    HV.append((h, s, T[s], TR[s], TBf[s], slice(h * 128, (h + 1) * 128), psb[6 + h % 2], psB[6 + h % 2], [True]))
                                _sv = cx.dead; cx.dead = _sv or not own
                                for region in (0, 4):
                                    aq = loadpair(region)
                                    for hh in range(2):
                                        h, s, tt, tr, bf, hs, ot, oB, first_o = HV[hh]
                                        pt, pB = projh(aq, hh)
                                        if region == 0:
                                            cx.op("act", lambda e: e.activation(out=tt["qs"][:], in_=pt[:, :], func=AF.Silu), reads=[pB], writes=[bf["qs"]])
                                        else:
                                            cx.op("act", lambda e: e.activation(out=tt["gs"][:], in_=pt[:, :], func=AF.Silu), reads=[pB], writes=[bf["gs"]])
                                            cx.op("dve", lambda e: e.tensor_scalar(tt["gs"][:], tt["gs"][:], hgn[:, h:h + 1], None, op0=ALU.mult),
                                                  reads=[bf["gs"], kB], writes=[bf["gs"]])
                                            cx.dma("pool", lambda q: q.dma_start(out=gsT[hs, tsl], in_=tt["gs"][:]), reads=[bf["gs"]])
                                cx.dead = _sv
                                for di in range(2):
                                    af = loadpair(1 + di)

                                    def unit(hh, di=di, af=af):
                                        h, s, tt, tr, bf, hs, ot, oB, first_o = HV[hh]
                                        fwd = di == 0
                                        pt, pB = projh(af, hh)
                                        yield
                                        t1, t2, t3, t4, t5, t6, ke = (tt[n] for n in ("t1", "t2", "t3", "t4", "t5", "t6", "ke"))
                                        smt = sm[s]
                                        tot = t3[:, 63::64]
                                        inc8, ex8, dec, dt1 = smt[:, 0:8], smt[:, 8:16], smt[:, 16:24], smt[:, 24:25]
                                        cx.op("act", lambda e: e.activation(out=t1[:], in_=pt[:, :], func=AF.Sigmoid), reads=[pB], writes=[bf["t1"]])
                                        cx.op("dve", lambda e: e.tensor_scalar(t1[:], t1[:], oml[:, di, h:h + 1], lb[:, di, h:h + 1], op0=ALU.mult, op1=ALU.add),
                                              reads=[bf["t1"], kB], writes=[bf["t1"]])
                                        cx.op("act", lambda e: e.activation(out=t2[:], in_=t1[:], func=AF.Ln), reads=[bf["t1"]], writes=[bf["t2"]])
                                        cx.op("pool", lambda e: e.tensor_scalar(t1[:], t1[:], -1.0, 1.0, op0=ALU.mult, op1=ALU.add),
                                              reads=[bf["t1"]], writes=[bf["t1"]])
                                        cx.op("dve", lambda e: e.tensor_tensor_scan(out=t3[:], data0=rmask[:], data1=t2[:], initial=0.0, op0=ALU.mult, op1=ALU.add),
                                              reads=[bf["t2"], kB], writes=[bf["t3"]])
                                        if fwd:
                                            bb, bbB = t3, bf["t3"]
                                        else:
                                            cx.op("pool", lambda e: e.tensor_tensor(out=t4[:], in0=t2[:], in1=t3[:], op=ALU.subtract),
                                                  reads=[bf["t2"], bf["t3"]], writes=[bf["t4"]])
                                            cx.op("dve", lambda e: e.tensor_tensor(out=t4[:].rearrange("p (c n) -> p c n", c=8),
                                                                                   in0=t4[:].rearrange("p (c n) -> p c n", c=8),
                                                                                   in1=tot.unsqueeze(2).to_broadcast([128, 8, 64]), op=ALU.add),
                                                  reads=[bf["t4"], bf["t3"]], writes=[bf["t4"]])
                                            bb, bbB = t4, bf["t4"]
                                        cx.op("act", lambda e: e.activation(out=dec, in_=tot, func=AF.Exp), reads=[bf["t3"]], writes=[bf["sm"]])
                                        _sv = cx.dead; cx.dead = _sv or not own
                                        cx.op("act", lambda e: e.activation(out=t5[:], in_=bb[:], func=AF.Exp), reads=[bbB], writes=[bf["t5"]])
                                        cx.dead = _sv
                                        cx.op("act", lambda e: e.activation(out=t6[:], in_=bb[:], func=AF.Exp, scale=-1.0), reads=[bbB], writes=[bf["t6"]])
                                        _sv = cx.dead; cx.dead = _sv or not own
                                        cx.op("dve", lambda e: e.tensor_tensor(out=tr["qd"][:], in0=tt["qs"][:], in1=t5[:], op=ALU.mult),
                                              reads=[bf["qs"], bf["t5"]], writes=[bf["qd"]])
                                        cx.dead = _sv
                                        cx.op("dve", lambda e: e.tensor_tensor(out=tr["kd"][:], in0=t1[:], in1=t6[:], op=ALU.mult),
                                              reads=[bf["t1"], bf["t6"]], writes=[bf["kd"]])
                                        cx.op("dve", lambda e: e.tensor_tensor(out=ke[:].rearrange("p (c n) -> p c n", c=8),
                                                                               in0=tr["kd"][:].bitcast(F32).rearrange("p (c n) -> p c n", c=8),
                                                                               in1=dec.unsqueeze(2).to_broadcast([128, 8, 64]), op=ALU.mult),
                                              reads=[bf["kd"], bf["sm"]], writes=[bf["ke"]])
                                        cx.op("dve", lambda e: e.tensor_tensor_scan(out=inc8, data0=ones8[:], data1=tot, initial=0.0, op0=ALU.mult, op1=ALU.add),
                                              reads=[bf["t3"], kB, bf["sm"]], writes=[bf["sm"]])
                                        cx.op("act", lambda e: e.activation(out=dt1, in_=inc8[:, 7:8], func=AF.Exp), reads=[bf["sm"]], writes=[bf["sm"]])
                                        _sv = cx.dead; cx.dead = _sv or not own
                                        if fwd:
                                            cx.op("dve", lambda e: e.tensor_tensor(out=ex8, in0=inc8, in1=tot, op=ALU.subtract), reads=[bf["sm"], bf["t3"]], writes=[bf["sm"]])
                                        else:
                                            cx.op("dve", lambda e: e.tensor_scalar(ex8, inc8, -1.0, inc8[:, 7:8], op0=ALU.mult, op1=ALU.add), reads=[bf["sm"]], writes=[bf["sm"]])
                                        cx.op("act", lambda e: e.activation(out=ex8, in_=ex8, func=AF.Exp), reads=[bf["sm"]], writes=[bf["sm"]])
                                        cx.op("dve", lambda e: e.tensor_tensor(out=tr["qg"][:].rearrange("p (c n) -> p c n", c=8),
                                                                                in0=tr["qd"][:].bitcast(F32).rearrange("p (c n) -> p c n", c=8),
                                                                                in1=ex8.unsqueeze(2).to_broadcast([128, 8, 64]), op=ALU.mult),
                                              reads=[bf["qd"], bf["sm"]], writes=[bf["qg"]])
                                        cx.dma("pool", lambda q: q.dma_start(out=qdgT[di, hs, tsl], in_=tr["qg"][:]), reads=[bf["qg"]])
                                        cx.dead = _sv
                                        chk(21)
                                        yield
                                        kp, kpB = pbank()
                                        cx.pe([(lambda t, p=p: t.transpose(out=kp[:, p * 128:(p + 1) * 128], in_=ke[:, p * 128:(p + 1) * 128], identity=ident[:]))
                                               for p in range(4)], reads=[bf["ke"], cB], writes=[kpB])
                                        cx.op("dve", lambda e: e.tensor_scalar(kend[s][:, 0].rearrange("p a b -> p (a b)"), kp[:, :], maskf[:, 63:64], None, op0=ALU.mult),
                                              reads=[kpB, kB], writes=[bf["kend"]])
                                        cx.op("dve", lambda e: e.tensor_scalar(kend[s][:, 1].rearrange("p a b -> p (a b)"), kp[:, :], maskb[:, 64:65], None, op0=ALU.mult),
                                              reads=[kpB, kB, bf["kend"]], writes=[bf["kend"]])
                                        chk(22)
                                        yield
                                        _sv = cx.dead; cx.dead = _sv or not own
                                        ap_, apB = pbank()
                                        cx.pe([(lambda t, p=p: t.matmul(ap_[:, p * 128:(p + 1) * 128], tr["kd"][:, p * 128:(p + 1) * 128],
                                                                        tr["qd"][:, p * 128:(p + 1) * 128], start=True, stop=True)) for p in range(4)],
                                              reads=[bf["kd"], bf["qd"]], writes=[apB])
                                        mk = maskf if fwd else maskb
                                        cx.op("dve", lambda e: e.tensor_tensor(out=tr["AT"][:], in0=ap_[:, :], in1=mk[:], op=ALU.mult), reads=[apB, kB], writes=[bf["AT"]])
                                        chk(23)
                                        vv = vt[va]
                                        vcs = slice(hh * 128, (hh + 1) * 128)
                                        fns = []
                                        for p in range(4):
                                            fns.append(lambda t, p=p, st_=first_o[0] and p == 0: t.matmul(ot[:, p * 128:(p + 1) * 128], vv[:, p, vcs],
                                                                                                          tr["AT"][:, p * 128:(p + 1) * 128], start=st_, stop=False,
                                                                                                          skip_group_check=True))
                                        first_o[0] = False
                                        cx.pe(fns, reads=[vtB[va], bf["AT"]], writes=[oB])
                                        cx.dead = _sv
                                        chk(24)
                                        yield
                                        dps = []
                                        for half in range(2):
                                            dp, dpB = pbank()
                                            dps.append((dp, dpB))
                                            fns = []
                                            for j in range(4):
                                                n = half * 4 + j
                                                p, off = n // 2, (n % 2) * 64
                                                fns.append(lambda t, j=j, p=p, off=off: t.matmul(dp[:, j * 128:(j + 1) * 128], kend[s][:, off // 64, p, :],
                                                                                                 vv[:, p, vcs], start=True, stop=True))
                                            cx.pe(fns, reads=[bf["kend"], vtB[va]], writes=[dpB])
                                        chk(25)
                                        yield
                                        S = Sst[s]
                                        order = list(range(8)) if fwd else list(range(7, -1, -1))
                                        cx.op("pool", lambda e: e.memset(S[:, 0, :].bitcast(F32), 0.0), writes=[bf["S"]])
                                        for step, n in enumerate(order):
                                            yield
                                            dp, dpB = dps[n // 4]
                                            cx.op("dve", lambda e, step=step, n=n, dp=dp: e.scalar_tensor_tensor(
                                                out=S[:, step + 1, :], in0=S[:, step, :].bitcast(F32), scalar=dec[:, n:n + 1],
                                                in1=dp[:, (n % 4) * 128:(n % 4 + 1) * 128], op0=ALU.mult, op1=ALU.add),
                                                reads=[bf["S"], bf["sm"], dpB], writes=[bf["S"]])
                                            if step < 7 and own:
                                                n2 = order[step + 1]
                                                cx.pe([lambda t, step=step, n2=n2: t.matmul(ot[:, n2 * 64:(n2 + 1) * 64], S[:, step + 1, :],
                                                                                            tr["qd"][:, n2 * 64:(n2 + 1) * 64], start=False, stop=False,
                                                                                            skip_group_check=True)],
                                                      reads=[bf["S"], bf["qd"]], writes=[oB])
                                        chk(26)
                                        row0 = ((b * 2 + di) * NH + h) * 128
                                        cx.dma("pool", lambda q: q.dma_start(out=Gd[row0:row0 + 128, 0:128], in_=S[:, 8, :].bitcast(F32)), reads=[bf["S"]])
                                        cx.dma("pool", lambda q: q.dma_start(out=Gd[row0:row0 + 128, 128:129], in_=dt1, allow_slow_non_contiguous=True), reads=[bf["sm"]])
                                    gens = [unit(0), unit(1)]
                                    while gens:
                                        for g_ in list(gens):
                                            try:
                                                next(g_)
                                            except StopIteration:
                                                gens.remove(g_)
                                for hh in range(2):
                                    h, s, tt, tr, bf, hs, ot, oB, first_o = HV[hh]
                                    _sv = cx.dead; cx.dead = _sv or not own
                                    cx.op("act", lambda e: e.activation(out=tt["ol"][:], in_=ot[:, :], func=AF.Copy), reads=[oB], writes=[bf["ol"]])
                                    cx.dma("pool", lambda q: q.dma_start(out=olocT[hs, tsl], in_=tt["ol"][:]), reads=[bf["ol"]])
                                    cx.dead = _sv
                            cx.barrier()
                    cx.barrier()

            chk(3)
            cx.barrier()
            chk(4)
            with contextlib.ExitStack() as st:
                memT = sb(st, "memT", [128, KC, MEM], F32R)
                mB = Buf()
                norm_transpose("nm", lambda r0, n: mem[r0:r0 + n, :], [(0, 128, 0), (128, 128, 128)], gains["g_mem"], memT, mB)
                wbufs = [sb(st, "wK%d" % i, [128, KC, 256], F32R) for i in range(2)]
                wB = [Buf(), Buf()]
                kv = [sb(st, "kv%d" % i, [128, 256], F32R) for i in range(2)]
                kvB = [Buf(), Buf()]
                ci = [0]

                def ep_k(cb, pt, pB, t0, tn):
                    a = ci[0] % 2
                    ci[0] += 1
                    cx.op("act", lambda e: e.activation(out=kv[a][:], in_=pt[:, 0:256], func=AF.Copy), reads=[pB], writes=[kvB[a]])
                    cx.dma("pool", lambda q: q.dma_start(out=kT_d[cb * 128:(cb + 1) * 128, :], in_=kv[a][:]), reads=[kvB[a]])
                gemm_fm(memT, mB, MEM, wk, D, ep_k, wbufs, wB)

                def ep_vm(c0, wcur, ti, pt, pB):
                    a = ci[0] % 2
                    ci[0] += 1
                    cx.op("dve", lambda e: e.tensor_copy(kv[a][:], pt[:, 0:256]), reads=[pB], writes=[kvB[a]])
                    cx.dma("pool", lambda q: q.dma_start(out=v_d[ti * 128:(ti + 1) * 128, c0:c0 + 256], in_=kv[a][:]), reads=[kvB[a]])
                gemm_tm(memT, mB, MEM, wv, D, ep_vm, wbufs, wB)
                cx.barrier()

            chk(5)
            with contextlib.ExitStack() as st:
                cmk = sb(st, "cmk", [128, NB, 2 * NVB])
                G = [sb(st, "G%d" % i, [128, NVB * 2, 129]) for i in range(1)]
                acf = sb(st, "acf", [128, NVB])
                msk = sb(st, "msk", [128, 128])
                Sin = [sb(st, "Sin%d" % i, [128, 128]) for i in range(2)]
                SinR = [sb(st, "SinR%d" % i, [128, 128], F32R) for i in range(2)]
                qg = [sb(st, "qgl%d" % i, [128, 512], F32R) for i in range(2)]
                ol = sb(st, "oll", [128, 512])
                gsl = sb(st, "gsl", [128, 512])
                sq = sb(st, "sq", [128, 512], F32R)
                rs = sb(st, "rs", [128, 512])
                yo = sb(st, "yo", [128, 512], F32R)
                cmB, GB, acB, mskB, olB, gslB, sqB, rsB, yoB = (Buf() for _ in range(9))
                SinB, SinRB, qgB = [Buf(), Buf()], [Buf(), Buf()], [Buf(), Buf()]
                cx.dma("sp", lambda q: q.dma_start(out=cmk[:].rearrange("p a b -> p (a b)"),
                                                   in_=cmask_d.rearrange("a b -> (a b)").partition_broadcast(128)), writes=[cmB])
                gdv = Gd.rearrange("(x h k) c -> k x h c", h=NH, k=128)
                for h in range(NH):
                    cx.dma("sp", lambda q: q.dma_start(out=G[0][:], in_=gdv[:, :, h, :]), writes=[GB])
                    Gv = G[0][:].rearrange("p (v d) c -> p v d c", d=2)
                    for b in range(NB):
                        tsl = slice(b * TB, (b + 1) * TB)
                        hs = slice(h * 128, (h + 1) * 128)
                        for di in range(2):
                            mrow = cmk[:, b, di * NVB:(di + 1) * NVB]
                            cx.op("dve", lambda e: e.tensor_scalar(acf[:], Gv[:, :, di, 128], -1.0, None, op0=ALU.add), reads=[GB], writes=[acB])
                            cx.op("dve", lambda e: e.tensor_tensor(out=acf[:], in0=acf[:], in1=mrow, op=ALU.mult), reads=[acB, cmB], writes=[acB])
                            cx.op("dve", lambda e: e.tensor_scalar(acf[:], acf[:], 1.0, None, op0=ALU.add), reads=[acB], writes=[acB])
                            cx.op("pool", lambda e: e.memset(Sin[di][:], 0.0), writes=[SinB[di]])
                            order = (list(range(NB, NVB)) + list(range(NB))) if di == 0 else (list(range(NVB - 1, NB - 1, -1)) + list(range(NB - 1, -1, -1)))
                            for v in order:
                                cx.op("pool", lambda e, v=v: e.tensor_scalar(msk[:], Gv[:, v, di, 0:128], mrow[:, v:v + 1], None, op0=ALU.mult),
                                      reads=[GB, cmB], writes=[mskB])
                                cx.op("dve", lambda e, v=v: e.scalar_tensor_tensor(out=Sin[di][:], in0=Sin[di][:], scalar=acf[:, v:v + 1], in1=msk[:],
                                                                                   op0=ALU.mult, op1=ALU.add),
                                      reads=[SinB[di], acB, mskB], writes=[SinB[di]])
                            cx.op("act", lambda e: e.activation(out=SinR[di][:], in_=Sin[di][:], func=AF.Copy), reads=[SinB[di]], writes=[SinRB[di]])
                            cx.dma("sp", lambda q: q.dma_start(out=qg[di][:], in_=qdgT[di, hs, tsl]), writes=[qgB[di]])
                        cx.dma("sp", lambda q: q.dma_start(out=ol[:], in_=olocT[hs, tsl]), writes=[olB])
                        cx.dma("sp", lambda q: q.dma_start(out=gsl[:], in_=gsT[hs, tsl]), writes=[gslB])
                        pt, pB = pbank()
                        cx.pe([(lambda t, di=di: t.matmul(pt[:, :], SinR[di][:], qg[di][:], start=(di == 0), stop=(di == 1))) for di in range(2)],
                              reads=SinRB + qgB, writes=[pB])
                        cx.op("dve", lambda e: e.tensor_tensor(out=ol[:], in0=pt[:, :], in1=ol[:], op=ALU.add), reads=[pB, olB], writes=[olB])
                        cx.op("act", lambda e: e.activation(out=sq[:], in_=ol[:], func=AF.Square), reads=[olB], writes=[sqB])
                        p2, p2B = pbank()
                        cx.pe([lambda t: t.matmul(p2[:, :], ones[:], sq[:], start=True, stop=True)], reads=[cB, sqB], writes=[p2B])
                        cx.op("dve", lambda e: e.tensor_scalar(rs[:], p2[:, :], 1.0 / 128, EPS, op0=ALU.mult, op1=ALU.add), reads=[p2B], writes=[rsB])
                        cx.op("act", lambda e: e.activation(out=rs[:], in_=rs[:], func=AF.Sqrt), reads=[rsB], writes=[rsB])
                        cx.op("dve", lambda e: e.reciprocal(rs[:], rs[:]), reads=[rsB], writes=[rsB])
                        cx.op("dve", lambda e: e.tensor_tensor(out=rs[:], in0=rs[:], in1=ol[:], op=ALU.mult), reads=[rsB, olB], writes=[rsB])
                        cx.op("dve", lambda e: e.tensor_tensor(out=yo[:], in0=rs[:], in1=gsl[:], op=ALU.mult), reads=[rsB, gslB], writes=[yoB])
                        cx.dma("pool", lambda q: q.dma_start(out=mixT[PW + h * 128:PW + (h + 1) * 128, tsl], in_=yo[:]), reads=[yoB])
                cx.barrier()

            chk(6)
            def proj_residual(name, AT_d, W_d, res_src, dst_d, b):
                tsl = slice(b * TB, (b + 1) * TB)
                with contextlib.ExitStack() as st:
                    AT = sb(st, name + "AT", [128, KC, 512], F32R)
                    wbufs = [sb(st, name + "w%d" % i, [128, KC, 256], F32R) for i in range(2)]
                    xr = [sb(st, name + "xr%d" % i, [128, 4, 256]) for i in range(2)]
                    ATB, wB, xrB = Buf(), [Buf(), Buf()], [Buf(), Buf()]
                    cx.dma("sp", lambda q: q.dma_start(out=AT[:], in_=AT_d[:, tsl].rearrange("(kc p) t -> p kc t", p=128)), writes=[ATB])
                    st_ = {}

                    def ep(c0, wcur, ti, pt, pB):
                        a = (c0 // 256) % 2
                        if ti == 0:
                            cx.dma("sp", lambda q: q.dma_start(out=xr[a][:], in_=res_src(b)[:, c0:c0 + 256].rearrange("(t p) c -> p t c", p=128)),
                                   writes=[xrB[a]])
                        cx.op("dve", lambda e: e.tensor_tensor(out=xr[a][:, ti, :], in0=pt[:, 0:256], in1=xr[a][:, ti, :], op=ALU.add),
                              reads=[pB, xrB[a]], writes=[xrB[a]])
                        if ti == 3:
                            cx.dma("pool", lambda q: q.dma_start(out=dst_d[tsl, c0:c0 + 256].rearrange("(t p) c -> p t c", p=128), in_=xr[a][:]),
                                   reads=[xrB[a]])
                    gemm_tm(AT, ATB, 512, W_d, D, ep, wbufs, wB)
                    cx.barrier()

            for b in range(NB):
                tsl = slice(b * TB, (b + 1) * TB)
                proj_residual("s5", mixT, w_out, lambda b: xh[b, 8:520, :], x1_d, b)
                chk(7)
                with contextlib.ExitStack() as st:
                    h1T = sb(st, "h1T", [128, KC, 512], F32R)
                    h1B = Buf()
                    norm_transpose("n2", lambda r0, n: x1_d[b * TB + r0:b * TB + r0 + n, :], [(r, 128, r) for r in range(0, 512, 128)],
                                   gains["g_xat"], h1T, h1B)
                    wbufs = [sb(st, "wq%d" % i, [128, KC, 256], F32R) for i in range(2)]
                    wB = [Buf(), Buf()]
                    qo = [sb(st, "qo%d" % i, [128, 512], F32R) for i in range(2)]
                    qoB = [Buf(), Buf()]
                    ci = [0]

                    def ep_q(cb, pt, pB, t0, tn):
                        a = ci[0] % 2
                        ci[0] += 1
                        if a == 0:
                            cx.op("act", lambda e: e.activation(out=qo[a][:], in_=pt[:, :], func=AF.Copy), reads=[pB], writes=[qoB[a]])
                        else:
                            cx.op("dve", lambda e: e.tensor_copy(qo[a][:], pt[:, :]), reads=[pB], writes=[qoB[a]])
                        cx.dma("pool", lambda q: q.dma_start(out=qT_d[cb * 128:(cb + 1) * 128, tsl], in_=qo[a][:]), reads=[qoB[a]])
                    gemm_fm(h1T, h1B, 512, wq, D, ep_q, wbufs, wB)
                    cx.barrier()
                chk(8)
                with contextlib.ExitStack() as st:
                    qh = sb(st, "qh", [128, XC, 512], F32R)
                    kh = sb(st, "kh", [128, XC, MEM], F32R)
                    vh = sb(st, "vh", [128, 2, XD], F32R)
                    pe_ = [sb(st, "pe%d" % i, [128, MEM]) for i in range(2)]
                    pT = sb(st, "pT", [128, 2, 512], F32R)
                    oh = [sb(st, "oh%d" % i, [128, 512], F32R) for i in range(2)]
                    smx = sb(st, "smx", [128, 16])
                    qhB, khB, vhB, pTB = Buf(), Buf(), Buf(), Buf()
                    peB, ohB = [Buf(), Buf()], [Buf(), Buf()]
                    smB = [Buf() for _ in range(4)]
                    sc = float(XD) ** -0.5
                    for hx in range(XH):
                        es_ = slice(hx * XD, (hx + 1) * XD)
                        cx.dma("sp", lambda q: q.dma_start(out=qh[:], in_=qT_d[es_, tsl].rearrange("(kc p) t -> p kc t", p=128)), writes=[qhB])
                        cx.dma("sp", lambda q: q.dma_start(out=kh[:], in_=kT_d[es_, :].rearrange("(kc p) t -> p kc t", p=128)), writes=[khB])
                        cx.dma("sp", lambda q: q.dma_start(out=vh[:], in_=v_d[:, es_].rearrange("(mt p) c -> p mt c", p=128)), writes=[vhB])
                        for ti in range(4):
                            a = ti % 2
                            pt, pB = pbank()
                            cx.pe([(lambda t, k=k: t.matmul(pt[:, 0:MEM], qh[:, k, ti * 128:(ti + 1) * 128], kh[:, k, :], start=(k == 0), stop=(k == XC - 1)))
                                   for k in range(XC)], reads=[qhB, khB], writes=[pB])
                            mx, nmx, ssum = smx[:, ti * 4:ti * 4 + 1], smx[:, ti * 4 + 1:ti * 4 + 2], smx[:, ti * 4 + 2:ti * 4 + 3]
                            cx.op("dve", lambda e: e.tensor_reduce(out=mx, in_=pt[:, 0:MEM], axis=AX.X, op=ALU.max), reads=[pB], writes=[smB[ti]])
                            cx.op("dve", lambda e: e.tensor_scalar(nmx, mx, -sc, None, op0=ALU.mult), reads=[smB[ti]], writes=[smB[ti]])
                            cx.op("act", lambda e: e.activation(out=pe_[a][:], in_=pt[:, 0:MEM], func=AF.Exp, bias=nmx, scale=sc, accum_out=ssum),
                                  reads=[pB, smB[ti]], writes=[peB[a], smB[ti]])
                            cx.op("dve", lambda e: e.reciprocal(ssum, ssum), reads=[smB[ti]], writes=[smB[ti]])
                            cx.op("dve", lambda e: e.tensor_scalar(pe_[a][:], pe_[a][:], ssum, None, op0=ALU.mult), reads=[peB[a], smB[ti]], writes=[peB[a]])
                            p2, p2B = pbank()
                            cx.pe([(lambda t, m=m: t.transpose(out=p2[:, m * 128:(m + 1) * 128], in_=pe_[a][:, m * 128:(m + 1) * 128], identity=ident[:]))
                                   for m in range(2)], reads=[peB[a], cB], writes=[p2B])
                            cx.op("act", lambda e: e.activation(out=pT[:, :, ti * 128:(ti + 1) * 128], in_=p2[:, 0:256].rearrange("p (m t) -> p m t", m=2),
                                                                func=AF.Copy), reads=[p2B], writes=[pTB])
                        for eb in range(XC):
                            a = eb % 2
                            pt, pB = pbank()
                            cx.pe([(lambda t, m=m: t.matmul(pt[:, :], vh[:, m, eb * 128:(eb + 1) * 128], pT[:, m, :], start=(m == 0), stop=(m == 1)))
                                   for m in range(2)], reads=[vhB, pTB], writes=[pB])
                            cx.op("dve" if a else "act", (lambda e: e.tensor_copy(oh[a][:], pt[:, :])) if a else
                                  (lambda e: e.activation(out=oh[a][:], in_=pt[:, :], func=AF.Copy)), reads=[pB], writes=[ohB[a]])
                            r0 = hx * XD + eb * 128
                            cx.dma("pool", lambda q: q.dma_start(out=oT_d[r0:r0 + 128, tsl], in_=oh[a][:]), reads=[ohB[a]])
                    cx.barrier()
                chk(9)
                proj_residual("s6", oT_d, wo, lambda b: x1_d[b * TB:(b + 1) * TB, :], x2_d, b)
                chk(10)
                with contextlib.ExitStack() as st:
                    h3T = sb(st, "h3T", [128, KC, 512])
                    h3B = Buf()
                    wr = sb(st, "wr", [128, KC, 36])
                    sl = sb(st, "sl", [128, 128])
                    ebase = sb(st, "ebase", [128, 32])
                    rB = Buf()
                    cx.dma("sp", lambda q: q.dma_start(out=wr[:], in_=wr_d.rearrange("(kc p) c -> p kc c", p=128)), writes=[rB])
                    cx.dma("sp", lambda q: q.dma_start(out=sl[:], in_=sl_d), writes=[rB])
                    cx.dma("sp", lambda q: q.dma_start(out=ebase[:], in_=ebase_d[0].partition_broadcast(128)), writes=[rB])
                    RT = [{n_: sb(st, "%s%d" % (n_, s_), shp) for n_, shp in (("lg", [128, 36]), ("oh4", [128, 4]), ("t32", [128, 32]), ("sel", [128, 8]),
                                                                              ("m8", [128, 8]), ("o1", [128, 8]), ("o2", [128, 8]), ("M1", [128, 32]),
                                                                              ("M2", [128, 32]), ("pos", [128, 32]), ("sc", [128, 16]))} for s_ in range(2)]
                    idf = [sb(st, "idf%d" % s_, [128, 2]) for s_ in range(2)]
                    RB = [Buf(), Buf()]

                    for ti in range(4):
                        gt = b * 4 + ti
                        norm_transpose("n3", lambda r0, n: x2_d[b * TB + r0:b * TB + r0 + n, :], [(ti * 128, 128, ti * 128)], gains["g_moe"], h3T, h3B)
                        R = RT[ti % 2]
                        rb = RB[ti % 2]
                        pt, pB = pbank()
                        cx.pe([(lambda t, k=k: t.matmul(pt[:, 0:36], h3T[:, k, ti * 128:(ti + 1) * 128], wr[:, k, :], start=(k == 0), stop=(k == KC - 1)))
                               for k in range(KC)], reads=[h3B, rB], writes=[pB])
                        lg, oh4, t32, sel, m8, o1, o2, M1, M2, pos, scr = (R[k] for k in ("lg", "oh4", "t32", "sel", "m8", "o1", "o2", "M1", "M2", "pos", "sc"))
                        V = lambda f: cx.op("dve", f, reads=[rb, rB, MallB], writes=[rb])
                        cx.op("dve", lambda e: e.tensor_copy(lg[:], pt[:, 0:36]), reads=[pB], writes=[rb])
                        gmx, gsum, gw, d21, w1c, w2c = (scr[:, j:j + 1] for j in range(6))
                        V(lambda e: e.tensor_reduce(out=gmx, in_=lg[:, 0:4], axis=AX.X, op=ALU.max))
                        V(lambda e: e.tensor_scalar(oh4[:], lg[:, 0:4], gmx, None, op0=ALU.is_equal))
                        V(lambda e: e.tensor_scalar(scr[:, 6:7], gmx, -1.0, None, op0=ALU.mult))
                        cx.op("act", lambda e: e.activation(out=scr[:, 8:12], in_=lg[:, 0:4], func=AF.Exp, bias=scr[:, 6:7], scale=1.0, accum_out=gsum),
                              reads=[rb], writes=[rb])
                        V(lambda e: e.reciprocal(gw, gsum))
                        V(lambda e: e.tensor_tensor(out=t32[:].rearrange("p (g j) -> p g j", g=4), in0=lg[:, 4:36].rearrange("p (g j) -> p g j", g=4),
                                                    in1=oh4[:].unsqueeze(2).to_broadcast([128, 4, 8]), op=ALU.mult))
                        V(lambda e: e.tensor_reduce(out=sel[:], in_=t32[:].rearrange("p (g j) -> p j g", g=4), axis=AX.X, op=ALU.add))
                        V(lambda e: e.max(out=m8[:], in_=sel[:]))
                        V(lambda e: e.tensor_scalar(o1[:], sel[:], m8[:, 0:1], None, op0=ALU.is_equal))
                        V(lambda e: e.tensor_scalar(o2[:], sel[:], m8[:, 1:2], None, op0=ALU.is_equal))
                        V(lambda e: e.tensor_tensor(out=d21, in0=m8[:, 1:2], in1=m8[:, 0:1], op=ALU.subtract))
                        cx.op("act", lambda e: e.activation(out=d21, in_=d21, func=AF.Exp), reads=[rb], writes=[rb])
                        V(lambda e: e.tensor_scalar(d21, d21, 1.0, None, op0=ALU.add))
                        V(lambda e: e.reciprocal(w1c, d21))
                        V(lambda e: e.tensor_scalar(w2c, w1c, -1.0, 1.0, op0=ALU.mult, op1=ALU.add))
                        cx.op("dve", lambda e: e.tensor_scalar(cw_all[:, gt * 2:gt * 2 + 1], w1c, gw, None, op0=ALU.mult), reads=[rb], writes=[cwB])
                        cx.op("dve", lambda e: e.tensor_scalar(cw_all[:, gt * 2 + 1:gt * 2 + 2], w2c, gw, None, op0=ALU.mult), reads=[rb], writes=[cwB])
                        for Mx, ox in ((M1, o1), (M2, o2)):
                            V(lambda e, Mx=Mx, ox=ox: e.tensor_tensor(out=Mx[:].rearrange("p (g j) -> p g j", g=4),
                                                                      in0=oh4[:].unsqueeze(2).to_broadcast([128, 4, 8]),
                                                                      in1=ox[:].unsqueeze(1).to_broadcast([128, 4, 8]), op=ALU.mult))
                        cx.op("dve", lambda e: e.tensor_tensor(out=Mall[:, gt, :], in0=M1[:], in1=M2[:], op=ALU.add), reads=[rb], writes=[MallB])
                        pp, ppB = pbank()
                        fns = [(lambda t, j=j: t.matmul(pp[:, 0:32], ones[:].bitcast(F32), Mall[:, j, :], start=(j == 0), stop=False)) for j in range(gt)]
                        fns.append(lambda t: t.matmul(pp[:, 0:32], sl[:], Mall[:, gt, :], start=(gt == 0), stop=True))
                        cx.pe(fns, reads=[MallB, cB, rB], writes=[ppB])
                        cx.op("dve", lambda e: e.tensor_tensor(out=pos[:], in0=pp[:, 0:32], in1=ebase[:], op=ALU.add), reads=[ppB, rB], writes=[rb])
                        for j, Mx in enumerate((M1, M2)):
                            V(lambda e, Mx=Mx: e.tensor_tensor(out=t32[:], in0=pos[:], in1=Mx[:], op=ALU.mult))
                            V(lambda e, j=j: e.tensor_reduce(out=idf[ti % 2][:, j:j + 1], in_=t32[:], axis=AX.X, op=ALU.add))
                        cx.op("dve", lambda e: e.tensor_copy(idx_all[:, gt * 2:gt * 2 + 2], idf[ti % 2][:]), reads=[rb], writes=[idxB])
                    def scat(i, xs_, xsB_, n):
                        gt = b * 4 + i
                        for j in range(2):
                            cx.dma("pool", lambda q, j=j: q.indirect_dma_start(out=Xs_d[:, :], out_offset=bass.IndirectOffsetOnAxis(ap=idx_all[:, gt * 2 + j:gt * 2 + j + 1], axis=0),
                                                                              in_=xs_[:, :], in_offset=None), reads=[xsB_, idxB])
                    norm_transpose("n4", lambda r0, n: x2_d[b * TB + r0:b * TB + r0 + n, :], [(r, 128, r) for r in range(0, 512, 128)], gains["g_moe"],
                                   None, None, keep_rows=scat)
                    cx.barrier()

            chk(11)
            with contextlib.ExitStack() as st:
                Xe = [sb(st, "Xe%d" % i, [128, D]) for i in range(2)]
                XeT = sb(st, "XeT", [128, KC, 128], F32R)
                wbufs = [sb(st, "wE%d" % i, [128, KC, 256], F32R) for i in range(3)]
                hm = sb(st, "hm", [128, DE])
                a_s = sb(st, "a_s", [128, DE])
                hmT = sb(st, "hmT", [128, DC, 128], F32R)
                ye = [sb(st, "ye%d" % i, [128, D]) for i in range(2)]
                XeB, XeTB, hmB, asB, hmTB = [Buf(), Buf()], Buf(), Buf(), Buf(), Buf()
                wB = [Buf(), Buf(), Buf()]
                yeB = [Buf(), Buf()]
                wtag = [0]
                for e_ in range(NE):
                    a = e_ % 2
                    cx.dma("sp", lambda q: q.dma_start(out=Xe[a][:], in_=Xs_d[e_ * 128:(e_ + 1) * 128, :]), writes=[XeB[a]])
                    for k0 in range(0, KC, 4):
                        pt, pB = pbank()
                        cx.pe([(lambda t, c=c: t.transpose(out=pt[:, c * 128:(c + 1) * 128], in_=Xe[a][:, (k0 + c) * 128:(k0 + c + 1) * 128], identity=ident[:]))
                               for c in range(4)], reads=[XeB[a], cB], writes=[pB])
                        src = pt[:, :].rearrange("p (c n) -> p c n", c=4)
                        if (k0 // 4) % 2:
                            cx.op("dve", lambda e: e.tensor_copy(XeT[:, k0:k0 + 4, :], src), reads=[pB], writes=[XeTB])
                        else:
                            cx.op("act", lambda e: e.activation(out=XeT[:, k0:k0 + 4, :], in_=src, func=AF.Copy), reads=[pB], writes=[XeTB])

                    def ep_a(c0, wcur, ti, pt, pB):
                        cx.op("act", lambda e: e.activation(out=a_s[:, c0:c0 + wcur], in_=pt[:, 0:wcur], func=AF.Silu), reads=[pB], writes=[asB])

                    def ep_c(c0, wcur, ti, pt, pB):
                        cx.op("dve", lambda e: e.tensor_tensor(out=hm[:, c0:c0 + wcur], in0=pt[:, 0:wcur], in1=a_s[:, c0:c0 + wcur], op=ALU.mult),
                              reads=[pB, asB], writes=[hmB])
                    gemm_tm(XeT, XeTB, 128, w1[e_], DE, ep_a, wbufs, wB, wtag)
                    gemm_tm(XeT, XeTB, 128, w3[e_], DE, ep_c, wbufs, wB, wtag)
                    for k0 in range(0, DC, 4):
                        kk = min(4, DC - k0)
                        pt, pB = pbank()
                        cx.pe([(lambda t, c=c: t.transpose(out=pt[:, c * 128:(c + 1) * 128], in_=hm[:, (k0 + c) * 128:(k0 + c + 1) * 128], identity=ident[:]))
                               for c in range(kk)], reads=[hmB, cB], writes=[pB])
                        cx.op("act", lambda e: e.activation(out=hmT[:, k0:k0 + kk, :], in_=pt[:, 0:kk * 128].rearrange("p (c n) -> p c n", c=kk), func=AF.Copy),
                              reads=[pB], writes=[hmTB])
                    WB2 = (KC * 256) // DC
                    WB2 = min(WB2, D)
                    for c0 in range(0, D, WB2):
                        wa = wtag[0] % 3
                        wtag[0] += 1
                        wv_ = wbufs[wa][:].rearrange("p k c -> p (k c)")[:, 0:DC * WB2].rearrange("p (k c) -> p k c", k=DC)
                        load_w(wv_, w2[e_][:, c0:c0 + WB2], wB[wa])
                        for c1 in range(0, WB2, 512):
                            cw = min(512, WB2 - c1)
                            pt, pB = pbank()
                            cx.pe([(lambda t, k=k: t.matmul(pt[:, 0:cw], hmT[:, k, :], wv_[:, k, c1:c1 + cw], start=(k == 0), stop=(k == DC - 1))) for k in range(DC)],
                                  reads=[hmTB, wB[wa]], writes=[pB])
                            dst = ye[a][:, c0 + c1:c0 + c1 + cw]
                            if (c1 // 512) % 2:
                                cx.op("dve", lambda e: e.tensor_copy(dst, pt[:, 0:cw]), reads=[pB], writes=[yeB[a]])
                            else:
                                cx.op("act", lambda e: e.activation(out=dst, in_=pt[:, 0:cw], func=AF.Copy), reads=[pB], writes=[yeB[a]])
                    cx.dma("pool", lambda q: q.dma_start(out=Y_d[e_ * 128:(e_ + 1) * 128, :], in_=ye[a][:]), reads=[yeB[a]])
                cx.barrier()

            chk(12)
            with contextlib.ExitStack() as st:
                gbc = sb(st, "fgbc", [128, D])
                y1 = [sb(st, "y1_%d" % i, [128, D]) for i in range(2)]
                y2 = [sb(st, "y2_%d" % i, [128, D]) for i in range(2)]
                xx = [sb(st, "xx_%d" % i, [128, D]) for i in range(2)]
                junk = sb(st, "fjunk", [128, D])
                ss = sb(st, "fss", [128, 8])
                gB, jB = Buf(), Buf()
                y1B, y2B, xxB = [Buf(), Buf()], [Buf(), Buf()], [Buf(), Buf()]
                ssB = [Buf() for _ in range(8)]
                cx.dma("sp", lambda q: q.dma_start(out=gbc[:], in_=gains["g_fin"][0].partition_broadcast(128)), writes=[gB])
                for gt in range(NB * 4):
                    a = gt % 2
                    cx.dma("sp", lambda q: q.dma_start(out=xx[a][:], in_=x2_d[gt * 128:(gt + 1) * 128, :]), writes=[xxB[a]])
                    for j, (yy, yyB) in enumerate(((y1[a], y1B[a]), (y2[a], y2B[a]))):
                        cx.dma("pool", lambda q, yy=yy, j=j: q.indirect_dma_start(out=yy[:, :], out_offset=None, in_=Y_d[:, :],
                                                                                 in_offset=bass.IndirectOffsetOnAxis(ap=idx_all[:, gt * 2 + j:gt * 2 + j + 1], axis=0)),
                               reads=[idxB], writes=[yyB])
                        cx.op("dve", lambda e, yy=yy, j=j: e.scalar_tensor_tensor(out=xx[a][:], in0=yy[:], scalar=cw_all[:, gt * 2 + j:gt * 2 + j + 1], in1=xx[a][:],
                                                                                 op0=ALU.mult, op1=ALU.add), reads=[yyB, cwB, xxB[a]], writes=[xxB[a]])
                    s1 = ss[:, gt % 8:gt % 8 + 1]
                    sB = ssB[gt % 8]
                    cx.op("act", lambda e: e.activation(out=junk[:], in_=xx[a][:], func=AF.Square, accum_out=s1), reads=[xxB[a]], writes=[jB, sB])
                    rstd_from_ss(s1, 128, sB, 1.0 / D)
                    cx.op("dve", lambda e: e.scalar_tensor_tensor(out=xx[a][:], in0=xx[a][:], scalar=s1, in1=gbc[:], op0=ALU.mult, op1=ALU.mult),
                          reads=[xxB[a], sB, gB], writes=[xxB[a]])
                    cx.dma("sp", lambda q: q.dma_start(out=out_d[gt * 128:(gt + 1) * 128, :], in_=xx[a][:]), reads=[xxB[a]])
                cx.barrier()
        _body()
        cx.dead = False
        cx.barrier()
    return nc


def host_inputs(cfg, inp):
    g = dims(cfg)
    D, NB, NC, TOK, PW, PG, HK, NH, NVB = g["D"], g["NB"], g["NC"], g["TOK"], g["PW"], g["PG"], g["HK"], g["NH"], g["NVB"]
    f = np.float32
    x = np.asarray(inp["x"], f)[0]
    S = x.shape[0]
    xp = np.zeros((S + 16, D), f)
    xp[8:8 + S] = x
    common = dict(
        mem=np.ascontiguousarray(np.asarray(inp["mem"], f)[0]),
        g_mix=np.asarray(inp["norm_mix"], f).reshape(1, D), g_xat=np.asarray(inp["norm_xattn"], f).reshape(1, D),
        g_mem=np.asarray(inp["norm_mem"], f).reshape(1, D), g_moe=np.asarray(inp["norm_moe"], f).reshape(1, D),
        g_fin=np.asarray(inp["norm_final"], f).reshape(1, D),
        w_in=np.asarray(inp["w_in"], f)[0], pool_w=np.asarray(inp["pool_w"], f)[0],
        psc=np.ascontiguousarray(np.asarray(inp["pool_scale"], f)[0].reshape(PW // 128, 128).T),
        lbp=np.ascontiguousarray(np.stack([np.asarray(inp["lb_fwd"], f), np.asarray(inp["lb_bwd"], f)], 0).reshape(2, 2, NH, 128).transpose(3, 0, 1, 2)),
        hgn=np.ascontiguousarray(np.asarray(inp["hgrn_norm"], f)[0].reshape(NH, 128).T),
        w_out=np.asarray(inp["w_out"], f)[0], wq=np.asarray(inp["w_q"], f)[0], wk=np.asarray(inp["w_k"], f)[0],
        wv=np.asarray(inp["w_v"], f)[0], wo=np.asarray(inp["w_o"], f)[0],
        wr=np.ascontiguousarray(np.concatenate([np.asarray(inp["w_router_group"], f)[0],
                                                np.asarray(inp["w_router_expert"], f)[0].transpose(1, 0, 2).reshape(D, 32)], 1)),
        w1=np.asarray(inp["w1"], f)[0], w3=np.asarray(inp["w3"], f)[0], w2=np.asarray(inp["w2"], f)[0],
        ident=np.eye(128, dtype=f), ones=np.ones((128, 128), f),
        sl=np.triu(np.ones((128, 128), f), 1),
        ebase=(np.arange(32, dtype=f) * 128).reshape(1, 32),
    )
    i = np.arange(128)
    same = (i[:, None] // 64) == (i[None, :] // 64)
    common["maskf"] = np.tile((same & (i[:, None] <= i[None, :])).astype(f), (1, 4))
    common["maskb"] = np.tile((same & (i[:, None] >= i[None, :])).astype(f), (1, 4))
    common["rmask"] = np.tile(((np.arange(512) % 64) != 0).astype(f)[None, :], (128, 1))
    t = np.arange(S)
    inv = np.zeros((4, S), f)
    for gi, w in enumerate((2, 4, 8, 16)):
        lo = np.clip(t - w // 2, 0, S - 1)
        hi = np.clip(t + w // 2 - 1, 0, S - 1)
        inv[gi] = 1.0 / (hi - lo + 1)
    maps = []
    for c in range(NC):
        m = dict(common)
        own = [c * NB + b for b in range(NB)]
        slots = own + [v for v in range(NVB) if v not in own]
        m["xh"] = np.stack([xp[v * TB: v * TB + 528] for v in slots], 0)
        m["invcnt"] = np.stack([inv[:, v * TB:(v + 1) * TB].reshape(-1) for v in own], 0)
        cm = np.zeros((NB, 2, NVB), f)
        for b in range(NB):
            for j, v in enumerate(slots):
                cm[b, 0, j] = 1.0 if v < own[b] else 0.0
                cm[b, 1, j] = 1.0 if v > own[b] else 0.0
        m["cmask"] = cm.reshape(NB, 2 * NVB)
        maps.append(m)
    return maps


FULL = dict(D=4096, NB=2, NC=8)
_NC_CACHE = {}


def run(cfg, inp):
    key = tuple(sorted(cfg.items()))
    if key not in _NC_CACHE:
        _NC_CACHE[key] = build(cfg)
    nc = _NC_CACHE[key]
    maps = host_inputs(cfg, inp)
    res = run_bass_kernel_spmd(nc, maps, core_ids=list(range(cfg["NC"])))
    out = np.concatenate([r["out"] for r in res.results], axis=0)
    return out[None].astype(np.float32)


def kernel(**inputs):
    return run(FULL, inputs)
```

```python
import contextlib
import numpy as np
import concourse.bass as bass
import concourse.mybir as mybir
from concourse.bass_utils import run_bass_kernel_spmd

F32 = mybir.dt.float32
F32R = mybir.dt.float32r
I32 = mybir.dt.int32
AF = mybir.ActivationFunctionType
ALU = mybir.AluOpType
AX = mybir.AxisListType
EPS = 1e-6
TB = 512
NSLOT = 8192


class Stop(Exception):
    pass


class Buf:
    __slots__ = ("w", "r")

    def __init__(self):
        self.w = None
        self.r = []


class Ctx:
    def __init__(self, nc, es):
        self.nc = nc
        self.eng = dict(pe=nc.tensor, act=nc.scalar, dve=nc.vector, pool=nc.gpsimd, sp=nc.sync)
        self.sem = {e: es.enter_context(nc.semaphore("s_" + e)) for e in self.eng}
        self.cnt = {e: 0 for e in self.eng}
        self.seen = {e: {} for e in self.eng}
        self.dsem = {q: [es.enter_context(nc.semaphore("d_%s%d" % (q, i))) for i in range(8)] for q in ("sp", "pool")}
        self.dcnt = {q: [0] * 8 for q in ("sp", "pool")}
        self.dnext = {"sp": 0, "pool": 0}
        self.alltoks = []
        self.dead = False

    def wait(self, e, toks):
        best = {}
        for t in toks:
            if t is None:
                continue
            sem, val, key = t
            if e == "pe" and key == "s_pe":
                continue
            if self.seen[e].get(key, 0) >= val:
                continue
            if key not in best or best[key][1] < val:
                best[key] = t
        for key, (sem, val, _) in best.items():
            self.eng[e].wait_ge(sem, val)
            self.seen[e][key] = val

    def _deps(self, reads, writes):
        toks = []
        for b in reads:
            toks.append(b.w)
        for b in writes:
            toks.append(b.w)
            toks.extend(b.r)
        return toks

    def _commit(self, tok, reads, writes):
        for b in reads:
            b.r.append(tok)
        for b in writes:
            b.w = tok
            b.r = []

    def op(self, e, fn, reads=(), writes=()):
        if self.dead:
            return None
        self.wait(e, self._deps(reads, writes))
        inst = fn(self.eng[e])
        self.cnt[e] += 1
        inst.then_inc(self.sem[e], 1)
        tok = (self.sem[e], self.cnt[e], "s_" + e)
        self._commit(tok, reads, writes)
        return tok

    def pe(self, fns, reads=(), writes=()):
        if self.dead:
            return None
        self.wait("pe", self._deps(reads, writes))
        inst = None
        for fn in fns:
            inst = fn(self.nc.tensor)
        self.cnt["pe"] += 1
        inst.then_inc(self.sem["pe"], 1)
        tok = (self.sem["pe"], self.cnt["pe"], "s_pe")
        self._commit(tok, reads, writes)
        return tok

    def dma(self, q, fn, reads=(), writes=()):
        if self.dead:
            return None
        i = self.dnext[q]
        self.dnext[q] = (i + 1) % 8
        sem = self.dsem[q][i]
        key = "d_%s%d" % (q, i)
        toks = self._deps(reads, writes)
        if self.dcnt[q][i] > 0:
            toks.append((sem, self.dcnt[q][i], key))
        self.wait(q, toks)
        inst = fn(self.eng[q])
        self.dcnt[q][i] += 16
        inst.then_inc(sem, 16)
        tok = (sem, self.dcnt[q][i], key)
        self._commit(tok, reads, writes)
        self.alltoks.append(tok)
        return tok

    def barrier(self):
        if self.dead:
            return
        toks = [(self.sem[e], self.cnt[e], "s_" + e) for e in self.eng if self.cnt[e] > 0]
        for q in ("sp", "pool"):
            for i in range(8):
                if self.dcnt[q][i] > 0:
                    toks.append((self.dsem[q][i], self.dcnt[q][i], "d_%s%d" % (q, i)))
        for e in self.eng:
            self.wait(e, [t for t in toks if not (e == "pe" and t[2] == "s_pe")])


def dims(cfg):
    D = cfg["D"]
    d = dict(D=D, NB=cfg["NB"], NC=cfg["NC"], KC=D // 128, TOK=cfg["NB"] * TB, PW=D // 2, PG=D // 8,
             HK=D // 2, NH=D // 256, XH=4, XD=D // 4, XC=D // 512, MEM=256, NE=32, DE=D // 4, DC=D // 512)
    d["NVB"] = d["NC"] * d["NB"]
    return d


def build(cfg):
    g = dims(cfg)
    D, NB, NC, KC, TOK, PW, PG, HK, NH = g["D"], g["NB"], g["NC"], g["KC"], g["TOK"], g["PW"], g["PG"], g["HK"], g["NH"]
    XH, XD, XC, MEM, NE, DE, DC, NVB = g["XH"], g["XD"], g["XC"], g["MEM"], g["NE"], g["DE"], g["DC"], g["NVB"]
    nc = bass.Bass("TRN2", target_bir_lowering=False)
    nc.dge_precook = False

    def din(name, shape, dt=F32):
        return nc.dram_tensor(name, shape, dt, kind="ExternalInput").ap()

    def dsc(name, shape, dt=F32, **kw):
        return nc.dram_tensor(name, shape, dt, **kw).ap()

    xh = din("xh", [NVB, 528, D])
    mem = din("mem", [MEM, D])
    gains = {n: din(n, [1, D]) for n in ("g_mix", "g_xat", "g_mem", "g_moe", "g_fin")}
    w_in = din("w_in", [D, 3 * D], F32R)
    pool_w = din("pool_w", [4, PG, PG], F32R)
    psc_d = din("psc", [128, PW // 128])
    lbp_d = din("lbp", [128, 2, 2, NH])
    hgn_d = din("hgn", [128, NH])
    w_out = din("w_out", [D, D], F32R)
    wq = din("wq", [D, D], F32R)
    wk = din("wk", [D, D], F32R)
    wv = din("wv", [D, D], F32R)
    wo = din("wo", [D, D], F32R)
    wr_d = din("wr", [D, 36])
    w1 = din("w1", [NE, D, DE], F32R)
    w3 = din("w3", [NE, D, DE], F32R)
    w2 = din("w2", [NE, DE, D], F32R)
    ident_d = din("ident", [128, 128])
    maskf_d = din("maskf", [128, 512])
    maskb_d = din("maskb", [128, 512])
    rmask_d = din("rmask", [128, 512])
    ones_d = din("ones", [128, 128], F32R)
    sl_d = din("sl", [128, 128])
    invcnt_d = din("invcnt", [NB, 4 * 512])
    cmask_d = din("cmask", [NB, 2 * NVB])
    ebase_d = din("ebase", [1, 32])
    out_d = nc.dram_tensor("out", [TOK, D], F32, kind="ExternalOutput").ap()

    mixT = dsc("mixT", [D, TOK], F32R)
    olocT = dsc("olocT", [HK, TOK])
    gsT = dsc("gsT", [HK, TOK])
    qdgT = dsc("qdgT", [2, HK, TOK], F32R)
    CCR = NVB * 2 * NH * 128
    Gd = dsc("Gd", [CCR, 129])
    x1_d = dsc("x1s", [TOK, D])
    qT_d = dsc("qTs", [D, TOK], F32R)
    kT_d = dsc("kTs", [D, MEM], F32R)
    v_d = dsc("vs", [MEM, D], F32R)
    oT_d = dsc("oTs", [D, TOK], F32R)
    x2_d = dsc("x2s", [TOK, D])
    Xs_d = dsc("Xs", [NSLOT, D])
    Y_d = dsc("Ys", [NSLOT, D])

    with contextlib.ExitStack() as es:
        cx = Ctx(nc, es)
        cc_sem = es.enter_context(nc.semaphore("cc_sem"))

        uid = [0]

        def sb(st, name, shape, dt=F32):
            uid[0] += 1
            return st.enter_context(nc.sbuf_tensor("%s_u%d" % (name, uid[0]), shape, dt))

        psb = [es.enter_context(nc.psum_tensor("ps%d" % i, [128, 512], F32)) for i in range(8)]
        psB = [Buf() for _ in range(8)]
        pstate = [0]

        def pbank():
            i = pstate[0]
            pstate[0] = (i + 1) % 6
            return psb[i], psB[i]

        ident = sb(es, "ident", [128, 128])
        ones = sb(es, "ones", [128, 128], F32R)
        small = sb(es, "small", [128, 64])
        idx_all = sb(es, "idx_all", [128, 2 * NB * 4], I32)
        cw_all = sb(es, "cw_all", [128, 2 * NB * 4])
        Mall = sb(es, "Mall", [128, NB * 4, 32])
        cB = Buf()
        cx.dma("sp", lambda q: q.dma_start(out=ident[:], in_=ident_d), writes=[cB])
        cx.dma("sp", lambda q: q.dma_start(out=ones[:], in_=ones_d), writes=[cB])
        idxB, cwB, MallB = Buf(), Buf(), Buf()

        def bl(x):
            return list(x) if isinstance(x, (list, tuple)) else [x]

        def load_w(dst, src_ap, B, extra_writes=()):
            return cx.dma("sp", lambda q: q.dma_start(out=dst, in_=src_ap.rearrange("(kc p) c -> p kc c", p=128)),
                          writes=bl(B) + list(extra_writes))

        def rstd_from_ss(ss, n, B, scale):
            cx.op("dve", lambda e: e.tensor_scalar(ss[0:n], ss[0:n], scale, EPS, op0=ALU.mult, op1=ALU.add), reads=[B], writes=[B])
            cx.op("act", lambda e: e.activation(out=ss[0:n], in_=ss[0:n], func=AF.Sqrt), reads=[B], writes=[B])
            cx.op("dve", lambda e: e.reciprocal(ss[0:n], ss[0:n]), reads=[B], writes=[B])

        def norm_transpose(st_name, row_src, tiles, gain_d, hT, hTB, out_dt_rows=None, keep_rows=None):
            with contextlib.ExitStack() as st:
                gbc = sb(st, st_name + "gbc", [128, D])
                xt = [sb(st, st_name + "xt%d" % i, [128, D]) for i in range(2)]
                xs = [sb(st, st_name + "xs%d" % i, [128, D]) for i in range(2)]
                junk = sb(st, st_name + "junk", [128, D])
                ss = sb(st, st_name + "ss", [128, 8])
                gB, jB = Buf(), Buf()
                xtB = [Buf(), Buf()]
                xsB = [Buf(), Buf()]
                ssB = [Buf() for _ in range(8)]
                cx.dma("sp", lambda q: q.dma_start(out=gbc[:], in_=gain_d[0].partition_broadcast(128)), writes=[gB])
                for i, (r0, n, c0) in enumerate(tiles):
                    a = i % 2
                    s1 = ss[:, (i % 8):(i % 8) + 1]
                    sB = ssB[i % 8]
                    cx.dma("sp", lambda q: q.dma_start(out=xt[a][0:n], in_=row_src(r0, n)), writes=[xtB[a]])
                    cx.op("act", lambda e: e.activation(out=junk[0:n], in_=xt[a][0:n], func=AF.Square, accum_out=s1[0:n]),
                          reads=[xtB[a]], writes=[jB, sB])
                    rstd_from_ss(s1, n, sB, 1.0 / D)
                    cx.op("dve", lambda e: e.scalar_tensor_tensor(out=xs[a][0:n], in0=xt[a][0:n], scalar=s1[0:n], in1=gbc[0:n],
                                                                  op0=ALU.mult, op1=ALU.mult),
                          reads=[xtB[a], sB, gB], writes=[xsB[a]])
                    if keep_rows is not None:
                        keep_rows(i, xs[a], xsB[a], n)
                    if hT is None:
                        continue
                    for k0 in range(0, KC, 4):
                        pt, pB = pbank()
                        kk = min(4, KC - k0)
                        cx.pe([(lambda t, c=c: t.transpose(out=pt[:, c * 128:c * 128 + n], in_=xs[a][0:n, (k0 + c) * 128:(k0 + c + 1) * 128],
                                                           identity=ident[0:n, 0:n])) for c in range(kk)],
                              reads=[xsB[a], cB], writes=[pB])
                        src = pt[:, 0:kk * 128].rearrange("p (c n) -> p c n", c=kk)[:, :, 0:n]
                        dst = hT[:, k0:k0 + kk, c0:c0 + n]
                        if (k0 // 4) % 2 == 0:
                            cx.op("act", lambda e: e.activation(out=dst, in_=src, func=AF.Copy), reads=[pB], writes=[hTB])
                        else:
                            cx.op("dve", lambda e: e.tensor_copy(dst, src), reads=[pB], writes=[hTB])
                cx.barrier()

        def gemm_fm(AT, ATB, ntok, W_ap, ncols, epilogue, wbufs, wB, tag=[0]):
            kc_n = AT.shape[1]
            WB = wbufs[0].shape[2]
            for c0 in range(0, ncols, WB):
                a = tag[0] % len(wbufs)
                tag[0] += 1
                wcur = min(WB, ncols - c0)
                load_w(wbufs[a][:, 0:kc_n, 0:wcur], W_ap[:, c0:c0 + wcur], wB[a])
                for c1 in range(0, wcur, 128):
                    for t0 in range(0, ntok, 512):
                        tn = min(512, ntok - t0)
                        pt, pB = pbank()
                        cx.pe([(lambda t, k=k: t.matmul(pt[:, 0:tn], wbufs[a][:, k, c1:c1 + 128], AT[:, k, t0:t0 + tn],
                                                        start=(k == 0), stop=(k == kc_n - 1))) for k in range(kc_n)],
                              reads=bl(wB[a]) + [ATB], writes=[pB])
                        epilogue((c0 + c1) // 128, pt, pB, t0, tn)

        def gemm_tm(AT, ATB, ntok, W_ap, ncols, epilogue, wbufs, wB, tag=[0]):
            kc_n = AT.shape[1]
            WB = wbufs[0].shape[2]
            for c0 in range(0, ncols, WB):
                a = tag[0] % len(wbufs)
                tag[0] += 1
                wcur = min(WB, ncols - c0)
                load_w(wbufs[a][:, 0:kc_n, 0:wcur], W_ap[:, c0:c0 + wcur], wB[a])
                for t0 in range(0, ntok, 128):
                    pt, pB = pbank()
                    cx.pe([(lambda t, k=k: t.matmul(pt[:, 0:wcur], AT[:, k, t0:t0 + 128], wbufs[a][:, k, 0:wcur],
                                                    start=(k == 0), stop=(k == kc_n - 1))) for k in range(kc_n)],
                          reads=bl(wB[a]) + [ATB], writes=[pB])
                    epilogue(c0, wcur, t0 // 128, pt, pB)

        def chk(n):
            if cfg.get("stop", 99) == n:
                cx.barrier()
                cx.dead = True

        def _body():
            for b in range(NVB):
                own = b < NB
                tsl = slice(b * TB, (b + 1) * TB) if own else None
                with contextlib.ExitStack() as st:
                    hT = sb(st, "hT", [128, KC, 528], F32R)
                    hTB = Buf()
                    tiles = [(r, 128, r) for r in range(0, 512, 128)] + [(512, 16, 512)]
                    norm_transpose("n1", lambda r0, n: xh[b, r0:r0 + n, :], tiles, gains["g_mix"], hT, hTB)
                    chk(1)
                    with contextlib.ExitStack() as st2:
                        wbig = sb(st2, "wA", [128, KC, 512], F32R)
                        wslot = [wbig[:, :, j * 128:(j + 1) * 128] for j in range(4)]
                        wsB = [Buf() for _ in range(4)]
                        wbufs = [wbig[:, :, 0:256], wbig[:, :, 256:512]]
                        wB = [[wsB[0], wsB[1]], [wsB[2], wsB[3]]]
                        wtagA = [0]
                        wtagS = [0]
                        with contextlib.ExitStack() as st3:
                          if own:
                            inv = sb(st3, "inv", [128, 4 * 512])
                            psc = sb(st3, "psc", [128, PW // 128])
                            pw = sb(st3, "pw", [128, PG // 128, PG], F32R)
                            uT = [sb(st3, "uT%d" % i, [128, 528]) for i in range(2)]
                            pa = [sb(st3, "pa%d" % i, [128, 528]) for i in range(2)]
                            pb_ = [sb(st3, "pb%d" % i, [128, 528]) for i in range(2)]
                            miT = sb(st3, "miT", [128, PG // 128, 512], F32R)
                            yp = [sb(st3, "yp%d" % i, [128, 512], F32R) for i in range(2)]
                            invB, pscB, pwB, miB = Buf(), Buf(), Buf(), Buf()
                            uB, paB, pbB, ypB = [Buf(), Buf()], [Buf(), Buf()], [Buf(), Buf()], [Buf(), Buf()]
                            cx.dma("sp", lambda q: q.dma_start(out=inv[:], in_=invcnt_d[b].partition_broadcast(128)), writes=[invB])
                            cx.dma("sp", lambda q: q.dma_start(out=psc[:], in_=psc_d), writes=[pscB])
                            cnt = [0]
                            for gi, w in enumerate((2, 4, 8, 16)):
                                cx.dma("sp", lambda q: q.dma_start(out=pw[:], in_=pool_w[gi].rearrange("(kc p) c -> p kc c", p=128)),
                                       writes=[pwB])

                                def ep_pool(cb, pt, pB, t0, tn, gi=gi, w=w):
                                    a = (cnt[0] // 2) % 2
                                    cnt[0] += 1
                                    if tn == 512:
                                        cx.op("act", lambda e: e.activation(out=uT[a][:, 0:512], in_=pt[:, 0:512], func=AF.Copy),
                                              reads=[pB], writes=[uB[a]])
                                        return
                                    cx.op("act", lambda e: e.activation(out=uT[a][:, 512:528], in_=pt[:, 0:16], func=AF.Copy),
                                          reads=[pB], writes=[uB[a]])
                                    cur, curB, m = uT[a], uB[a], 1
                                    alt = [(pa[a], paB[a]), (pb_[a], pbB[a])]
                                    k = 0
                                    while m < w // 2:
                                        nx, nxB = alt[k % 2]
                                        k += 1
                                        cx.op("pool", lambda e, nx=nx, cur=cur, m=m: e.tensor_tensor(out=nx[:, 2 * m - 1:528], in0=cur[:, 2 * m - 1:528],
                                                                                                     in1=cur[:, m - 1:528 - m], op=ALU.add),
                                              reads=[curB], writes=[nxB])
                                        cur, curB, m = nx, nxB, 2 * m
                                    h = w // 2
                                    nx, nxB = alt[k % 2]
                                    cx.op("dve", lambda e: e.tensor_tensor(out=nx[:, 8:520], in0=cur[:, 7:519], in1=cur[:, 7 + h:519 + h], op=ALU.add),
                                          reads=[curB], writes=[nxB])
                                    cx.op("dve", lambda e: e.tensor_tensor(out=nx[:, 8:520], in0=nx[:, 8:520], in1=inv[:, gi * 512:(gi + 1) * 512], op=ALU.mult),
                                          reads=[nxB, invB], writes=[nxB])
                                    cbl = cb
                                    cx.op("dve", lambda e: e.tensor_tensor(out=miT[:, cbl, :], in0=nx[:, 8:520], in1=uT[a][:, 8:520], op=ALU.subtract),
                                          reads=[nxB, uB[a]], writes=[miB])

                                gemm_fm(hT, hTB, 528, w_in[:, gi * PG:(gi + 1) * PG], PG, ep_pool, wbufs, wB, wtagA)
                                for db in range(PG // 128):
                                    pt, pB = pbank()
                                    ncb = PG // 128
                                    cx.pe([(lambda t, c=c: t.matmul(pt[:, :], pw[:, c, db * 128:(db + 1) * 128], miT[:, c, :],
                                                                    start=(c == 0), stop=(c == ncb - 1))) for c in range(ncb)],
                                          reads=[pwB, miB], writes=[pB])
                                    a = db % 2
                                    col = gi * (PG // 128) + db
                                    cx.op("dve", lambda e: e.tensor_scalar(yp[a][:], pt[:, :], psc[:, col:col + 1], None, op0=ALU.mult),
                                          reads=[pB, pscB], writes=[ypB[a]])
                                    r0 = gi * PG + db * 128
                                    cx.dma("pool", lambda q: q.dma_start(out=mixT[r0:r0 + 128, tsl], in_=yp[a][:]), reads=[ypB[a]])
                            cx.barrier()
                        chk(2)
                        with contextlib.ExitStack() as st3:
                            maskf = sb(st3, "maskf", [128, 512])
                            maskb = sb(st3, "maskb", [128, 512])
                            rmask = sb(st3, "rmask", [128, 512])
                            lbp = sb(st3, "lbp", [128, 2, 2, NH])
                            lb = sb(st3, "lb", [128, 2, NH])
                            oml = sb(st3, "oml", [128, 2, NH])
                            hgn = sb(st3, "hgn", [128, NH])
                            ones8 = sb(st3, "ones8", [128, 8])
                            kB = Buf()
                            for t_, d_ in ((maskf, maskf_d), (maskb, maskb_d), (rmask, rmask_d), (lbp, lbp_d), (hgn, hgn_d)):
                                cx.dma("sp", lambda q, t_=t_, d_=d_: q.dma_start(out=t_[:], in_=d_), writes=[kB])
                            cx.op("dve", lambda e: e.memset(ones8[:], 1.0), writes=[kB])
                            cx.op("dve", lambda e: e.tensor_tensor(out=lb[:], in0=lbp[:, :, 0, :], in1=lbp[:, :, 1, :], op=ALU.subtract), reads=[kB], writes=[kB])
                            cx.op("act", lambda e: e.activation(out=lb[:], in_=lb[:], func=AF.Sigmoid), reads=[kB], writes=[kB])
                            cx.op("dve", lambda e: e.tensor_scalar(oml[:], lb[:], -1.0, 1.0, op0=ALU.mult, op1=ALU.add), reads=[kB], writes=[kB])
                            NS = 2
                            names = ["qs", "t1", "t2", "t3", "t4", "t5", "t6"]
                            T = [{n: sb(st3, "%s_%d" % (n, s), [128, 512]) for n in names} for s in range(NS)]
                            TR = [{n: sb(st3, "%s_%d" % (n, s), [128, 512], F32R) for n in ("qd", "kd", "qg")} for s in range(NS)]
                            kend = [sb(st3, "kend%d" % s, [128, 2, 4, 128], F32R) for s in range(NS)]
                            Sst = [sb(st3, "Sst%d" % s, [128, 9, 128], F32R) for s in range(NS)]
                            vt = [sb(st3, "vt%d" % s, [128, 4, 256], F32R) for s in range(2)]
                            sm = [sb(st3, "sm%d" % s, [128, 40]) for s in range(NS)]
                            TBf = [{n: Buf() for n in names + ["qd", "kd", "qg", "kend", "S", "sm"]} for s in range(NS)]
                            for s_ in range(NS):
                                for al, tg in (("gs", "t6"), ("ke", "t2"), ("ol", "t2")):
                                    T[s_][al] = T[s_][tg]
                                    TBf[s_][al] = TBf[s_][tg]
                                TR[s_]["AT"] = TR[s_]["qg"]
                                TBf[s_]["AT"] = TBf[s_]["qg"]
                            vtB = [Buf(), Buf()]
                            psum_hold = {}

                            def ep_hold(key):
                                def ep(cb, pt, pB, t0, tn):
                                    psum_hold[(key, cb)] = (pt, pB)
                                return ep

                            for hp in range(NH // 2):
                                va = hp % 2

                                def ep_v(c0, wcur, ti, pt, pB, va=va):
                                    cx.op("act", lambda e: e.activation(out=vt[va][:, ti, :], in_=pt[:, 0:256], func=AF.Copy), reads=[pB], writes=[vtB[va]])
                                gemm_tm(hT[:, :, 8:520], hTB, 512, w_in[:, PW + 3 * HK + hp * 256:PW + 3 * HK + (hp + 1) * 256], 256, ep_v, wbufs, wB, wtagA)
                                def loadpair(region):
                                    c = PW + region * HK + hp * 256
                                    a = wtagA[0] % 2
                                    wtagA[0] += 1
                                    load_w(wbufs[a], w_in[:, c:c + 256], wB[a])
                                    return a

                                def projh(a, hh):
                                    pt, pB = pbank()
                                    cx.pe([(lambda t, k=k: t.matmul(pt[:, :], wbufs[a][:, k, hh * 128:(hh + 1) * 128], hT[:, k, 8:520], start=(k == 0), stop=(k == KC - 1)))
                                           for k in range(KC)], reads=bl(wB[a]) + [hTB], writes=[pB])
                                    return pt, pB
                                HV = []
                                for hh in range(2):
                                    h = hp * 2 + hh
                                    s = h % NS
                                    HV.append((h, s, T[s], TR[s], TBf[s], slice(h * 128, (h + 1) * 128), psb[6 + h % 2], psB[6 + h % 2], [True]))
                                _sv = cx.dead; cx.dead = _sv or not own
                                for region in (0, 4):
                                    aq = loadpair(region)
                                    for hh in range(2):
                                        h, s, tt, tr, bf, hs, ot, oB, first_o = HV[hh]
                                        pt, pB = projh(aq, hh)
                                        if region == 0:
                                            cx.op("act", lambda e: e.activation(out=tt["qs"][:], in_=pt[:, :], func=AF.Silu), reads=[pB], writes=[bf["qs"]])
                                        else:
                                            cx.op("act", lambda e: e.activation(out=tt["gs"][:], in_=pt[:, :], func=AF.Silu), reads=[pB], writes=[bf["gs"]])
                                            cx.op("dve", lambda e: e.tensor_scalar(tt["gs"][:], tt["gs"][:], hgn[:, h:h + 1], None, op0=ALU.mult),
                                                  reads=[bf["gs"], kB], writes=[bf["gs"]])
                                            cx.dma("pool", lambda q: q.dma_start(out=gsT[hs, tsl], in_=tt["gs"][:]), reads=[bf["gs"]])
                                cx.dead = _sv
                                for di in range(2):
                                    af = loadpair(1 + di)

                                    def unit(hh, di=di, af=af):
                                        h, s, tt, tr, bf, hs, ot, oB, first_o = HV[hh]
                                        fwd = di == 0
                                        pt, pB = projh(af, hh)
                                        yield
                                        t1, t2, t3, t4, t5, t6, ke = (tt[n] for n in ("t1", "t2", "t3", "t4", "t5", "t6", "ke"))
                                        smt = sm[s]
                                        tot = t3[:, 63::64]
                                        inc8, ex8, dec, dt1 = smt[:, 0:8], smt[:, 8:16], smt[:, 16:24], smt[:, 24:25]
                                        cx.op("act", lambda e: e.activation(out=t1[:], in_=pt[:, :], func=AF.Sigmoid), reads=[pB], writes=[bf["t1"]])
                                        cx.op("dve", lambda e: e.tensor_scalar(t1[:], t1[:], oml[:, di, h:h + 1], lb[:, di, h:h + 1], op0=ALU.mult, op1=ALU.add),
                                              reads=[bf["t1"], kB], writes=[bf["t1"]])
                                        cx.op("act", lambda e: e.activation(out=t2[:], in_=t1[:], func=AF.Ln), reads=[bf["t1"]], writes=[bf["t2"]])
                                        cx.op("pool", lambda e: e.tensor_scalar(t1[:], t1[:], -1.0, 1.0, op0=ALU.mult, op1=ALU.add),
                                              reads=[bf["t1"]], writes=[bf["t1"]])
                                        cx.op("dve", lambda e: e.tensor_tensor_scan(out=t3[:], data0=rmask[:], data1=t2[:], initial=0.0, op0=ALU.mult, op1=ALU.add),
                                              reads=[bf["t2"], kB], writes=[bf["t3"]])
                                        if fwd:
                                            bb, bbB = t3, bf["t3"]
                                        else:
                                            cx.op("pool", lambda e: e.tensor_tensor(out=t4[:], in0=t2[:], in1=t3[:], op=ALU.subtract),
                                                  reads=[bf["t2"], bf["t3"]], writes=[bf["t4"]])
                                            cx.op("dve", lambda e: e.tensor_tensor(out=t4[:].rearrange("p (c n) -> p c n", c=8),
                                                                                   in0=t4[:].rearrange("p (c n) -> p c n", c=8),
                                                                                   in1=tot.unsqueeze(2).to_broadcast([128, 8, 64]), op=ALU.add),
                                                  reads=[bf["t4"], bf["t3"]], writes=[bf["t4"]])
                                            bb, bbB = t4, bf["t4"]
                                        cx.op("act", lambda e: e.activation(out=dec, in_=tot, func=AF.Exp), reads=[bf["t3"]], writes=[bf["sm"]])
                                        _sv = cx.dead; cx.dead = _sv or not own
                                        cx.op("act", lambda e: e.activation(out=t5[:], in_=bb[:], func=AF.Exp), reads=[bbB], writes=[bf["t5"]])
                                        cx.dead = _sv
                                        cx.op("act", lambda e: e.activation(out=t6[:], in_=bb[:], func=AF.Exp, scale=-1.0), reads=[bbB], writes=[bf["t6"]])
                                        _sv = cx.dead; cx.dead = _sv or not own
                                        cx.op("dve", lambda e: e.tensor_tensor(out=tr["qd"][:], in0=tt["qs"][:], in1=t5[:], op=ALU.mult),
                                              reads=[bf["qs"], bf["t5"]], writes=[bf["qd"]])
                                        cx.dead = _sv
                                        cx.op("dve", lambda e: e.tensor_tensor(out=tr["kd"][:], in0=t1[:], in1=t6[:], op=ALU.mult),
                                              reads=[bf["t1"], bf["t6"]], writes=[bf["kd"]])
                                        cx.op("dve", lambda e: e.tensor_tensor(out=ke[:].rearrange("p (c n) -> p c n", c=8),
                                                                               in0=tr["kd"][:].bitcast(F32).rearrange("p (c n) -> p c n", c=8),
                                                                               in1=dec.unsqueeze(2).to_broadcast([128, 8, 64]), op=ALU.mult),
                                              reads=[bf["kd"], bf["sm"]], writes=[bf["ke"]])
                                        cx.op("dve", lambda e: e.tensor_tensor_scan(out=inc8, data0=ones8[:], data1=tot, initial=0.0, op0=ALU.mult, op1=ALU.add),
                                              reads=[bf["t3"], kB, bf["sm"]], writes=[bf["sm"]])
                                        cx.op("act", lambda e: e.activation(out=dt1, in_=inc8[:, 7:8], func=AF.Exp), reads=[bf["sm"]], writes=[bf["sm"]])
                                        _sv = cx.dead; cx.dead = _sv or not own
                                        if fwd:
                                            cx.op("dve", lambda e: e.tensor_tensor(out=ex8, in0=inc8, in1=tot, op=ALU.subtract), reads=[bf["sm"], bf["t3"]], writes=[bf["sm"]])
                                        else:
                                            cx.op("dve", lambda e: e.tensor_scalar(ex8, inc8, -1.0, inc8[:, 7:8], op0=ALU.mult, op1=ALU.add), reads=[bf["sm"]], writes=[bf["sm"]])
                                        cx.op("act", lambda e: e.activation(out=ex8, in_=ex8, func=AF.Exp), reads=[bf["sm"]], writes=[bf["sm"]])
                                        cx.op("dve", lambda e: e.tensor_tensor(out=tr["qg"][:].rearrange("p (c n) -> p c n", c=8),
                                                                                in0=tr["qd"][:].bitcast(F32).rearrange("p (c n) -> p c n", c=8),
                                                                                in1=ex8.unsqueeze(2).to_broadcast([128, 8, 64]), op=ALU.mult),
                                              reads=[bf["qd"], bf["sm"]], writes=[bf["qg"]])
                                        cx.dma("pool", lambda q: q.dma_start(out=qdgT[di, hs, tsl], in_=tr["qg"][:]), reads=[bf["qg"]])
                                        cx.dead = _sv
                                        chk(21)
                                        yield
                                        kp, kpB = pbank()
                                        cx.pe([(lambda t, p=p: t.transpose(out=kp[:, p * 128:(p + 1) * 128], in_=ke[:, p * 128:(p + 1) * 128], identity=ident[:]))
                                               for p in range(4)], reads=[bf["ke"], cB], writes=[kpB])
                                        cx.op("dve", lambda e: e.tensor_scalar(kend[s][:, 0].rearrange("p a b -> p (a b)"), kp[:, :], maskf[:, 63:64], None, op0=ALU.mult),
                                              reads=[kpB, kB], writes=[bf["kend"]])
                                        cx.op("dve", lambda e: e.tensor_scalar(kend[s][:, 1].rearrange("p a b -> p (a b)"), kp[:, :], maskb[:, 64:65], None, op0=ALU.mult),
                                              reads=[kpB, kB, bf["kend"]], writes=[bf["kend"]])
                                        chk(22)
                                        yield
                                        _sv = cx.dead; cx.dead = _sv or not own
                                        ap_, apB = pbank()
                                        cx.pe([(lambda t, p=p: t.matmul(ap_[:, p * 128:(p + 1) * 128], tr["kd"][:, p * 128:(p + 1) * 128],
                                                                        tr["qd"][:, p * 128:(p + 1) * 128], start=True, stop=True)) for p in range(4)],
                                              reads=[bf["kd"], bf["qd"]], writes=[apB])
                                        mk = maskf if fwd else maskb
                                        cx.op("dve", lambda e: e.tensor_tensor(out=tr["AT"][:], in0=ap_[:, :], in1=mk[:], op=ALU.mult), reads=[apB, kB], writes=[bf["AT"]])
                                        chk(23)
                                        vv = vt[va]
                                        vcs = slice(hh * 128, (hh + 1) * 128)
                                        fns = []
                                        for p in range(4):
                                            fns.append(lambda t, p=p, st_=first_o[0] and p == 0: t.matmul(ot[:, p * 128:(p + 1) * 128], vv[:, p, vcs],
                                                                                                          tr["AT"][:, p * 128:(p + 1) * 128], start=st_, stop=False,
                                                                                                          skip_group_check=True))
                                        first_o[0] = False
                                        cx.pe(fns, reads=[vtB[va], bf["AT"]], writes=[oB])
                                        cx.dead = _sv
                                        chk(24)
                                        yield
                                        dps = []
                                        for half in range(2):
                                            dp, dpB = pbank()
                                            dps.append((dp, dpB))
                                            fns = []
                                            for j in range(4):
                                                n = half * 4 + j
                                                p, off = n // 2, (n % 2) * 64
                                                fns.append(lambda t, j=j, p=p, off=off: t.matmul(dp[:, j * 128:(j + 1) * 128], kend[s][:, off // 64, p, :],
                                                                                                 vv[:, p, vcs], start=True, stop=True))
                                            cx.pe(fns, reads=[bf["kend"], vtB[va]], writes=[dpB])
                                        chk(25)
                                        yield
                                        S = Sst[s]
                                        order = list(range(8)) if fwd else list(range(7, -1, -1))
                                        cx.op("pool", lambda e: e.memset(S[:, 0, :].bitcast(F32), 0.0), writes=[bf["S"]])
                                        for step, n in enumerate(order):
                                            yield
                                            dp, dpB = dps[n // 4]
                                            cx.op("dve", lambda e, step=step, n=n, dp=dp: e.scalar_tensor_tensor(
                                                out=S[:, step + 1, :], in0=S[:, step, :].bitcast(F32), scalar=dec[:, n:n + 1],
                                                in1=dp[:, (n % 4) * 128:(n % 4 + 1) * 128], op0=ALU.mult, op1=ALU.add),
                                                reads=[bf["S"], bf["sm"], dpB], writes=[bf["S"]])
                                            if step < 7 and own:
                                                n2 = order[step + 1]
                                                cx.pe([lambda t, step=step, n2=n2: t.matmul(ot[:, n2 * 64:(n2 + 1) * 64], S[:, step + 1, :],
                                                                                            tr["qd"][:, n2 * 64:(n2 + 1) * 64], start=False, stop=False,
                                                                                            skip_group_check=True)],
                                                      reads=[bf["S"], bf["qd"]], writes=[oB])
                                        chk(26)
                                        row0 = ((b * 2 + di) * NH + h) * 128
                                        cx.dma("pool", lambda q: q.dma_start(out=Gd[row0:row0 + 128, 0:128], in_=S[:, 8, :].bitcast(F32)), reads=[bf["S"]])
                                        cx.dma("pool", lambda q: q.dma_start(out=Gd[row0:row0 + 128, 128:129], in_=dt1, allow_slow_non_contiguous=True), reads=[bf["sm"]])
                                    gens = [unit(0), unit(1)]
                                    while gens:
                                        for g_ in list(gens):
                                            try:
                                                next(g_)
                                            except StopIteration:
                                                gens.remove(g_)
                                for hh in range(2):
                                    h, s, tt, tr, bf, hs, ot, oB, first_o = HV[hh]
                                    _sv = cx.dead; cx.dead = _sv or not own
                                    cx.op("act", lambda e: e.activation(out=tt["ol"][:], in_=ot[:, :], func=AF.Copy), reads=[oB], writes=[bf["ol"]])
                                    cx.dma("pool", lambda q: q.dma_start(out=olocT[hs, tsl], in_=tt["ol"][:]), reads=[bf["ol"]])
                                    cx.dead = _sv
                            cx.barrier()
                    cx.barrier()

            chk(3)
            cx.barrier()
            chk(4)
            with contextlib.ExitStack() as st:
                memT = sb(st, "memT", [128, KC, MEM], F32R)
                mB = Buf()
                norm_transpose("nm", lambda r0, n: mem[r0:r0 + n, :], [(0, 128, 0), (128, 128, 128)], gains["g_mem"], memT, mB)
                wbufs = [sb(st, "wK%d" % i, [128, KC, 256], F32R) for i in range(2)]
                wB = [Buf(), Buf()]
                kv = [sb(st, "kv%d" % i, [128, 256], F32R) for i in range(2)]
                kvB = [Buf(), Buf()]
                ci = [0]

                def ep_k(cb, pt, pB, t0, tn):
                    a = ci[0] % 2
                    ci[0] += 1
                    cx.op("act", lambda e: e.activation(out=kv[a][:], in_=pt[:, 0:256], func=AF.Copy), reads=[pB], writes=[kvB[a]])
                    cx.dma("pool", lambda q: q.dma_start(out=kT_d[cb * 128:(cb + 1) * 128, :], in_=kv[a][:]), reads=[kvB[a]])
                gemm_fm(memT, mB, MEM, wk, D, ep_k, wbufs, wB)

                def ep_vm(c0, wcur, ti, pt, pB):
                    a = ci[0] % 2
                    ci[0] += 1
                    cx.op("dve", lambda e: e.tensor_copy(kv[a][:], pt[:, 0:256]), reads=[pB], writes=[kvB[a]])
                    cx.dma("pool", lambda q: q.dma_start(out=v_d[ti * 128:(ti + 1) * 128, c0:c0 + 256], in_=kv[a][:]), reads=[kvB[a]])
                gemm_tm(memT, mB, MEM, wv, D, ep_vm, wbufs, wB)
                cx.barrier()

            chk(5)
            with contextlib.ExitStack() as st:
                cmk = sb(st, "cmk", [128, NB, 2 * NVB])
                G = [sb(st, "G%d" % i, [128, NVB * 2, 129]) for i in range(1)]
                acf = sb(st, "acf", [128, NVB])
                msk = sb(st, "msk", [128, 128])
                Sin = [sb(st, "Sin%d" % i, [128, 128]) for i in range(2)]
                SinR = [sb(st, "SinR%d" % i, [128, 128], F32R) for i in range(2)]
                qg = [sb(st, "qgl%d" % i, [128, 512], F32R) for i in range(2)]
                ol = sb(st, "oll", [128, 512])
                gsl = sb(st, "gsl", [128, 512])
                sq = sb(st, "sq", [128, 512], F32R)
                rs = sb(st, "rs", [128, 512])
                yo = sb(st, "yo", [128, 512], F32R)
                cmB, GB, acB, mskB, olB, gslB, sqB, rsB, yoB = (Buf() for _ in range(9))
                SinB, SinRB, qgB = [Buf(), Buf()], [Buf(), Buf()], [Buf(), Buf()]
                cx.dma("sp", lambda q: q.dma_start(out=cmk[:].rearrange("p a b -> p (a b)"),
                                                   in_=cmask_d.rearrange("a b -> (a b)").partition_broadcast(128)), writes=[cmB])
                gdv = Gd.rearrange("(x h k) c -> k x h c", h=NH, k=128)
                for h in range(NH):
                    cx.dma("sp", lambda q: q.dma_start(out=G[0][:], in_=gdv[:, :, h, :]), writes=[GB])
                    Gv = G[0][:].rearrange("p (v d) c -> p v d c", d=2)
                    for b in range(NB):
                        tsl = slice(b * TB, (b + 1) * TB)
                        hs = slice(h * 128, (h + 1) * 128)
                        for di in range(2):
                            mrow = cmk[:, b, di * NVB:(di + 1) * NVB]
                            cx.op("dve", lambda e: e.tensor_scalar(acf[:], Gv[:, :, di, 128], -1.0, None, op0=ALU.add), reads=[GB], writes=[acB])
                            cx.op("dve", lambda e: e.tensor_tensor(out=acf[:], in0=acf[:], in1=mrow, op=ALU.mult), reads=[acB, cmB], writes=[acB])
                            cx.op("dve", lambda e: e.tensor_scalar(acf[:], acf[:], 1.0, None, op0=ALU.add), reads=[acB], writes=[acB])
                            cx.op("pool", lambda e: e.memset(Sin[di][:], 0.0), writes=[SinB[di]])
                            order = (list(range(NB, NVB)) + list(range(NB))) if di == 0 else (list(range(NVB - 1, NB - 1, -1)) + list(range(NB - 1, -1, -1)))
                            for v in order:
                                cx.op("pool", lambda e, v=v: e.tensor_scalar(msk[:], Gv[:, v, di, 0:128], mrow[:, v:v + 1], None, op0=ALU.mult),
                                      reads=[GB, cmB], writes=[mskB])
                                cx.op("dve", lambda e, v=v: e.scalar_tensor_tensor(out=Sin[di][:], in0=Sin[di][:], scalar=acf[:, v:v + 1], in1=msk[:],
                                                                                   op0=ALU.mult, op1=ALU.add),
                                      reads=[SinB[di], acB, mskB], writes=[SinB[di]])
                            cx.op("act", lambda e: e.activation(out=SinR[di][:], in_=Sin[di][:], func=AF.Copy), reads=[SinB[di]], writes=[SinRB[di]])
                            cx.dma("sp", lambda q: q.dma_start(out=qg[di][:], in_=qdgT[di, hs, tsl]), writes=[qgB[di]])
                        cx.dma("sp", lambda q: q.dma_start(out=ol[:], in_=olocT[hs, tsl]), writes=[olB])
                        cx.dma("sp", lambda q: q.dma_start(out=gsl[:], in_=gsT[hs, tsl]), writes=[gslB])
                        pt, pB = pbank()
                        cx.pe([(lambda t, di=di: t.matmul(pt[:, :], SinR[di][:], qg[di][:], start=(di == 0), stop=(di == 1))) for di in range(2)],
                              reads=SinRB + qgB, writes=[pB])
                        cx.op("dve", lambda e: e.tensor_tensor(out=ol[:], in0=pt[:, :], in1=ol[:], op=ALU.add), reads=[pB, olB], writes=[olB])
                        cx.op("act", lambda e: e.activation(out=sq[:], in_=ol[:], func=AF.Square), reads=[olB], writes=[sqB])
                        p2, p2B = pbank()
                        cx.pe([lambda t: t.matmul(p2[:, :], ones[:], sq[:], start=True, stop=True)], reads=[cB, sqB], writes=[p2B])
                        cx.op("dve", lambda e: e.tensor_scalar(rs[:], p2[:, :], 1.0 / 128, EPS, op0=ALU.mult, op1=ALU.add), reads=[p2B], writes=[rsB])
                        cx.op("act", lambda e: e.activation(out=rs[:], in_=rs[:], func=AF.Sqrt), reads=[rsB], writes=[rsB])
                        cx.op("dve", lambda e: e.reciprocal(rs[:], rs[:]), reads=[rsB], writes=[rsB])
                        cx.op("dve", lambda e: e.tensor_tensor(out=rs[:], in0=rs[:], in1=ol[:], op=ALU.mult), reads=[rsB, olB], writes=[rsB])
                        cx.op("dve", lambda e: e.tensor_tensor(out=yo[:], in0=rs[:], in1=gsl[:], op=ALU.mult), reads=[rsB, gslB], writes=[yoB])
                        cx.dma("pool", lambda q: q.dma_start(out=mixT[PW + h * 128:PW + (h + 1) * 128, tsl], in_=yo[:]), reads=[yoB])
                cx.barrier()

            chk(6)
            def proj_residual(name, AT_d, W_d, res_src, dst_d, b):
                tsl = slice(b * TB, (b + 1) * TB)
                with contextlib.ExitStack() as st:
                    AT = sb(st, name + "AT", [128, KC, 512], F32R)
                    wbufs = [sb(st, name + "w%d" % i, [128, KC, 256], F32R) for i in range(2)]
                    xr = [sb(st, name + "xr%d" % i, [128, 4, 256]) for i in range(2)]
                    ATB, wB, xrB = Buf(), [Buf(), Buf()], [Buf(), Buf()]
                    cx.dma("sp", lambda q: q.dma_start(out=AT[:], in_=AT_d[:, tsl].rearrange("(kc p) t -> p kc t", p=128)), writes=[ATB])
                    st_ = {}

                    def ep(c0, wcur, ti, pt, pB):
                        a = (c0 // 256) % 2
                        if ti == 0:
                            cx.dma("sp", lambda q: q.dma_start(out=xr[a][:], in_=res_src(b)[:, c0:c0 + 256].rearrange("(t p) c -> p t c", p=128)),
                                   writes=[xrB[a]])
                        cx.op("dve", lambda e: e.tensor_tensor(out=xr[a][:, ti, :], in0=pt[:, 0:256], in1=xr[a][:, ti, :], op=ALU.add),
                              reads=[pB, xrB[a]], writes=[xrB[a]])
                        if ti == 3:
                            cx.dma("pool", lambda q: q.dma_start(out=dst_d[tsl, c0:c0 + 256].rearrange("(t p) c -> p t c", p=128), in_=xr[a][:]),
                                   reads=[xrB[a]])
                    gemm_tm(AT, ATB, 512, W_d, D, ep, wbufs, wB)
                    cx.barrier()

            for b in range(NB):
                tsl = slice(b * TB, (b + 1) * TB)
                proj_residual("s5", mixT, w_out, lambda b: xh[b, 8:520, :], x1_d, b)
                chk(7)
                with contextlib.ExitStack() as st:
                    h1T = sb(st, "h1T", [128, KC, 512], F32R)
                    h1B = Buf()
                    norm_transpose("n2", lambda r0, n: x1_d[b * TB + r0:b * TB + r0 + n, :], [(r, 128, r) for r in range(0, 512, 128)],
                                   gains["g_xat"], h1T, h1B)
                    wbufs = [sb(st, "wq%d" % i, [128, KC, 256], F32R) for i in range(2)]
                    wB = [Buf(), Buf()]
                    qo = [sb(st, "qo%d" % i, [128, 512], F32R) for i in range(2)]
                    qoB = [Buf(), Buf()]
                    ci = [0]

                    def ep_q(cb, pt, pB, t0, tn):
                        a = ci[0] % 2
                        ci[0] += 1
                        if a == 0:
                            cx.op("act", lambda e: e.activation(out=qo[a][:], in_=pt[:, :], func=AF.Copy), reads=[pB], writes=[qoB[a]])
                        else:
                            cx.op("dve", lambda e: e.tensor_copy(qo[a][:], pt[:, :]), reads=[pB], writes=[qoB[a]])
                        cx.dma("pool", lambda q: q.dma_start(out=qT_d[cb * 128:(cb + 1) * 128, tsl], in_=qo[a][:]), reads=[qoB[a]])
                    gemm_fm(h1T, h1B, 512, wq, D, ep_q, wbufs, wB)
                    cx.barrier()
                chk(8)
                with contextlib.ExitStack() as st:
                    qh = sb(st, "qh", [128, XC, 512], F32R)
                    kh = sb(st, "kh", [128, XC, MEM], F32R)
                    vh = sb(st, "vh", [128, 2, XD], F32R)
                    pe_ = [sb(st, "pe%d" % i, [128, MEM]) for i in range(2)]
                    pT = sb(st, "pT", [128, 2, 512], F32R)
                    oh = [sb(st, "oh%d" % i, [128, 512], F32R) for i in range(2)]
                    smx = sb(st, "smx", [128, 16])
                    qhB, khB, vhB, pTB = Buf(), Buf(), Buf(), Buf()
                    peB, ohB = [Buf(), Buf()], [Buf(), Buf()]
                    smB = [Buf() for _ in range(4)]
                    sc = float(XD) ** -0.5
                    for hx in range(XH):
                        es_ = slice(hx * XD, (hx + 1) * XD)
                        cx.dma("sp", lambda q: q.dma_start(out=qh[:], in_=qT_d[es_, tsl].rearrange("(kc p) t -> p kc t", p=128)), writes=[qhB])
                        cx.dma("sp", lambda q: q.dma_start(out=kh[:], in_=kT_d[es_, :].rearrange("(kc p) t -> p kc t", p=128)), writes=[khB])
                        cx.dma("sp", lambda q: q.dma_start(out=vh[:], in_=v_d[:, es_].rearrange("(mt p) c -> p mt c", p=128)), writes=[vhB])
                        for ti in range(4):
                            a = ti % 2
                            pt, pB = pbank()
                            cx.pe([(lambda t, k=k: t.matmul(pt[:, 0:MEM], qh[:, k, ti * 128:(ti + 1) * 128], kh[:, k, :], start=(k == 0), stop=(k == XC - 1)))
                                   for k in range(XC)], reads=[qhB, khB], writes=[pB])
                            mx, nmx, ssum = smx[:, ti * 4:ti * 4 + 1], smx[:, ti * 4 + 1:ti * 4 + 2], smx[:, ti * 4 + 2:ti * 4 + 3]
                            cx.op("dve", lambda e: e.tensor_reduce(out=mx, in_=pt[:, 0:MEM], axis=AX.X, op=ALU.max), reads=[pB], writes=[smB[ti]])
                            cx.op("dve", lambda e: e.tensor_scalar(nmx, mx, -sc, None, op0=ALU.mult), reads=[smB[ti]], writes=[smB[ti]])
                            cx.op("act", lambda e: e.activation(out=pe_[a][:], in_=pt[:, 0:MEM], func=AF.Exp, bias=nmx, scale=sc, accum_out=ssum),
                                  reads=[pB, smB[ti]], writes=[peB[a], smB[ti]])
                            cx.op("dve", lambda e: e.reciprocal(ssum, ssum), reads=[smB[ti]], writes=[smB[ti]])
                            cx.op("dve", lambda e: e.tensor_scalar(pe_[a][:], pe_[a][:], ssum, None, op0=ALU.mult), reads=[peB[a], smB[ti]], writes=[peB[a]])
                            p2, p2B = pbank()
                            cx.pe([(lambda t, m=m: t.transpose(out=p2[:, m * 128:(m + 1) * 128], in_=pe_[a][:, m * 128:(m + 1) * 128], identity=ident[:]))
                                   for m in range(2)], reads=[peB[a], cB], writes=[p2B])
                            cx.op("act", lambda e: e.activation(out=pT[:, :, ti * 128:(ti + 1) * 128], in_=p2[:, 0:256].rearrange("p (m t) -> p m t", m=2),
                                                                func=AF.Copy), reads=[p2B], writes=[pTB])
                        for eb in range(XC):
                            a = eb % 2
                            pt, pB = pbank()
                            cx.pe([(lambda t, m=m: t.matmul(pt[:, :], vh[:, m, eb * 128:(eb + 1) * 128], pT[:, m, :], start=(m == 0), stop=(m == 1)))
                                   for m in range(2)], reads=[vhB, pTB], writes=[pB])
                            cx.op("dve" if a else "act", (lambda e: e.tensor_copy(oh[a][:], pt[:, :])) if a else
                                  (lambda e: e.activation(out=oh[a][:], in_=pt[:, :], func=AF.Copy)), reads=[pB], writes=[ohB[a]])
                            r0 = hx * XD + eb * 128
                            cx.dma("pool", lambda q: q.dma_start(out=oT_d[r0:r0 + 128, tsl], in_=oh[a][:]), reads=[ohB[a]])
                    cx.barrier()
                chk(9)
                proj_residual("s6", oT_d, wo, lambda b: x1_d[b * TB:(b + 1) * TB, :], x2_d, b)
                chk(10)
                with contextlib.ExitStack() as st:
                    h3T = sb(st, "h3T", [128, KC, 512])
                    h3B = Buf()
                    wr = sb(st, "wr", [128, KC, 36])
                    sl = sb(st, "sl", [128, 128])
                    ebase = sb(st, "ebase", [128, 32])
                    rB = Buf()
                    cx.dma("sp", lambda q: q.dma_start(out=wr[:], in_=wr_d.rearrange("(kc p) c -> p kc c", p=128)), writes=[rB])
                    cx.dma("sp", lambda q: q.dma_start(out=sl[:], in_=sl_d), writes=[rB])
                    cx.dma("sp", lambda q: q.dma_start(out=ebase[:], in_=ebase_d[0].partition_broadcast(128)), writes=[rB])
                    RT = [{n_: sb(st, "%s%d" % (n_, s_), shp) for n_, shp in (("lg", [128, 36]), ("oh4", [128, 4]), ("t32", [128, 32]), ("sel", [128, 8]),
                                                                              ("m8", [128, 8]), ("o1", [128, 8]), ("o2", [128, 8]), ("M1", [128, 32]),
                                                                              ("M2", [128, 32]), ("pos", [128, 32]), ("sc", [128, 16]))} for s_ in range(2)]
                    idf = [sb(st, "idf%d" % s_, [128, 2]) for s_ in range(2)]
                    RB = [Buf(), Buf()]

                    for ti in range(4):
                        gt = b * 4 + ti
                        norm_transpose("n3", lambda r0, n: x2_d[b * TB + r0:b * TB + r0 + n, :], [(ti * 128, 128, ti * 128)], gains["g_moe"], h3T, h3B)
                        R = RT[ti % 2]
                        rb = RB[ti % 2]
                        pt, pB = pbank()
                        cx.pe([(lambda t, k=k: t.matmul(pt[:, 0:36], h3T[:, k, ti * 128:(ti + 1) * 128], wr[:, k, :], start=(k == 0), stop=(k == KC - 1)))
                               for k in range(KC)], reads=[h3B, rB], writes=[pB])
                        lg, oh4, t32, sel, m8, o1, o2, M1, M2, pos, scr = (R[k] for k in ("lg", "oh4", "t32", "sel", "m8", "o1", "o2", "M1", "M2", "pos", "sc"))
                        V = lambda f: cx.op("dve", f, reads=[rb, rB, MallB], writes=[rb])
                        cx.op("dve", lambda e: e.tensor_copy(lg[:], pt[:, 0:36]), reads=[pB], writes=[rb])
                        gmx, gsum, gw, d21, w1c, w2c = (scr[:, j:j + 1] for j in range(6))
                        V(lambda e: e.tensor_reduce(out=gmx, in_=lg[:, 0:4], axis=AX.X, op=ALU.max))
                        V(lambda e: e.tensor_scalar(oh4[:], lg[:, 0:4], gmx, None, op0=ALU.is_equal))
                        V(lambda e: e.tensor_scalar(scr[:, 6:7], gmx, -1.0, None, op0=ALU.mult))
                        cx.op("act", lambda e: e.activation(out=scr[:, 8:12], in_=lg[:, 0:4], func=AF.Exp, bias=scr[:, 6:7], scale=1.0, accum_out=gsum),
                              reads=[rb], writes=[rb])
                        V(lambda e: e.reciprocal(gw, gsum))
                        V(lambda e: e.tensor_tensor(out=t32[:].rearrange("p (g j) -> p g j", g=4), in0=lg[:, 4:36].rearrange("p (g j) -> p g j", g=4),
                                                    in1=oh4[:].unsqueeze(2).to_broadcast([128, 4, 8]), op=ALU.mult))
                        V(lambda e: e.tensor_reduce(out=sel[:], in_=t32[:].rearrange("p (g j) -> p j g", g=4), axis=AX.X, op=ALU.add))
                        V(lambda e: e.max(out=m8[:], in_=sel[:]))
                        V(lambda e: e.tensor_scalar(o1[:], sel[:], m8[:, 0:1], None, op0=ALU.is_equal))
                        V(lambda e: e.tensor_scalar(o2[:], sel[:], m8[:, 1:2], None, op0=ALU.is_equal))
                        V(lambda e: e.tensor_tensor(out=d21, in0=m8[:, 1:2], in1=m8[:, 0:1], op=ALU.subtract))
                        cx.op("act", lambda e: e.activation(out=d21, in_=d21, func=AF.Exp), reads=[rb], writes=[rb])
                        V(lambda e: e.tensor_scalar(d21, d21, 1.0, None, op0=ALU.add))
                        V(lambda e: e.reciprocal(w1c, d21))
                        V(lambda e: e.tensor_scalar(w2c, w1c, -1.0, 1.0, op0=ALU.mult, op1=ALU.add))
                        cx.op("dve", lambda e: e.tensor_scalar(cw_all[:, gt * 2:gt * 2 + 1], w1c, gw, None, op0=ALU.mult), reads=[rb], writes=[cwB])
                        cx.op("dve", lambda e: e.tensor_scalar(cw_all[:, gt * 2 + 1:gt * 2 + 2], w2c, gw, None, op0=ALU.mult), reads=[rb], writes=[cwB])
                        for Mx, ox in ((M1, o1), (M2, o2)):
                            V(lambda e, Mx=Mx, ox=ox: e.tensor_tensor(out=Mx[:].rearrange("p (g j) -> p g j", g=4),
                                                                      in0=oh4[:].unsqueeze(2).to_broadcast([128, 4, 8]),
                                                                      in1=ox[:].unsqueeze(1).to_broadcast([128, 4, 8]), op=ALU.mult))
                        cx.op("dve", lambda e: e.tensor_tensor(out=Mall[:, gt, :], in0=M1[:], in1=M2[:], op=ALU.add), reads=[rb], writes=[MallB])
                        pp, ppB = pbank()
                        fns = [(lambda t, j=j: t.matmul(pp[:, 0:32], ones[:].bitcast(F32), Mall[:, j, :], start=(j == 0), stop=False)) for j in range(gt)]
                        fns.append(lambda t: t.matmul(pp[:, 0:32], sl[:], Mall[:, gt, :], start=(gt == 0), stop=True))
                        cx.pe(fns, reads=[MallB, cB, rB], writes=[ppB])
                        cx.op("dve", lambda e: e.tensor_tensor(out=pos[:], in0=pp[:, 0:32], in1=ebase[:], op=ALU.add), reads=[ppB, rB], writes=[rb])
                        for j, Mx in enumerate((M1, M2)):
                            V(lambda e, Mx=Mx: e.tensor_tensor(out=t32[:], in0=pos[:], in1=Mx[:], op=ALU.mult))
                            V(lambda e, j=j: e.tensor_reduce(out=idf[ti % 2][:, j:j + 1], in_=t32[:], axis=AX.X, op=ALU.add))
                        cx.op("dve", lambda e: e.tensor_copy(idx_all[:, gt * 2:gt * 2 + 2], idf[ti % 2][:]), reads=[rb], writes=[idxB])
                    def scat(i, xs_, xsB_, n):
                        gt = b * 4 + i
                        for j in range(2):
                            cx.dma("pool", lambda q, j=j: q.indirect_dma_start(out=Xs_d[:, :], out_offset=bass.IndirectOffsetOnAxis(ap=idx_all[:, gt * 2 + j:gt * 2 + j + 1], axis=0),
                                                                              in_=xs_[:, :], in_offset=None), reads=[xsB_, idxB])
                    norm_transpose("n4", lambda r0, n: x2_d[b * TB + r0:b * TB + r0 + n, :], [(r, 128, r) for r in range(0, 512, 128)], gains["g_moe"],
                                   None, None, keep_rows=scat)
                    cx.barrier()

            chk(11)
            with contextlib.ExitStack() as st:
                Xe = [sb(st, "Xe%d" % i, [128, D]) for i in range(2)]
                XeT = sb(st, "XeT", [128, KC, 128], F32R)
                wbufs = [sb(st, "wE%d" % i, [128, KC, 256], F32R) for i in range(3)]
                hm = sb(st, "hm", [128, DE])
                a_s = sb(st, "a_s", [128, DE])
                hmT = sb(st, "hmT", [128, DC, 128], F32R)
                ye = [sb(st, "ye%d" % i, [128, D]) for i in range(2)]
                XeB, XeTB, hmB, asB, hmTB = [Buf(), Buf()], Buf(), Buf(), Buf(), Buf()
                wB = [Buf(), Buf(), Buf()]
                yeB = [Buf(), Buf()]
                wtag = [0]
                for e_ in range(NE):
                    a = e_ % 2
                    cx.dma("sp", lambda q: q.dma_start(out=Xe[a][:], in_=Xs_d[e_ * 128:(e_ + 1) * 128, :]), writes=[XeB[a]])
                    for k0 in range(0, KC, 4):
                        pt, pB = pbank()
                        cx.pe([(lambda t, c=c: t.transpose(out=pt[:, c * 128:(c + 1) * 128], in_=Xe[a][:, (k0 + c) * 128:(k0 + c + 1) * 128], identity=ident[:]))
                               for c in range(4)], reads=[XeB[a], cB], writes=[pB])
                        src = pt[:, :].rearrange("p (c n) -> p c n", c=4)
                        if (k0 // 4) % 2:
                            cx.op("dve", lambda e: e.tensor_copy(XeT[:, k0:k0 + 4, :], src), reads=[pB], writes=[XeTB])
                        else:
                            cx.op("act", lambda e: e.activation(out=XeT[:, k0:k0 + 4, :], in_=src, func=AF.Copy), reads=[pB], writes=[XeTB])

                    def ep_a(c0, wcur, ti, pt, pB):
                        cx.op("act", lambda e: e.activation(out=a_s[:, c0:c0 + wcur], in_=pt[:, 0:wcur], func=AF.Silu), reads=[pB], writes=[asB])

                    def ep_c(c0, wcur, ti, pt, pB):
                        cx.op("dve", lambda e: e.tensor_tensor(out=hm[:, c0:c0 + wcur], in0=pt[:, 0:wcur], in1=a_s[:, c0:c0 + wcur], op=ALU.mult),
                              reads=[pB, asB], writes=[hmB])
                    KQ = 8
                    CW = (KC * 256) // KQ
                    for Wd_, ep_ in ((w1, ep_a), (w3, ep_c)):
                        for ch in range(0, DE, CW):
                            subs = [(c1, min(512, CW - c1)) + pbank() for c1 in range(0, CW, 512)]
                            for kq in range(0, KC, KQ):
                                wa = wtag[0] % 3
                                wtag[0] += 1
                                wv1 = wbufs[wa][:].rearrange("p k c -> p (k c)")[:, 0:KQ * CW].rearrange("p (k c) -> p k c", k=KQ)
                                load_w(wv1, Wd_[e_][kq * 128:(kq + KQ) * 128, ch:ch + CW], wB[wa])
                                for (c1, cw, pt, pB) in subs:
                                    cx.pe([(lambda t, k=k: t.matmul(pt[:, 0:cw], XeT[:, kq + k, :], wv1[:, k, c1:c1 + cw],
                                                                    start=(kq == 0 and k == 0), stop=(kq == KC - KQ and k == KQ - 1))) for k in range(KQ)],
                                          reads=[XeTB, wB[wa]], writes=[pB])
                            for (c1, cw, pt, pB) in subs:
                                ep_(ch + c1, cw, 0, pt, pB)
                    for k0 in range(0, DC, 4):
                        kk = min(4, DC - k0)
                        pt, pB = pbank()
                        cx.pe([(lambda t, c=c: t.transpose(out=pt[:, c * 128:(c + 1) * 128], in_=hm[:, (k0 + c) * 128:(k0 + c + 1) * 128], identity=ident[:]))
                               for c in range(kk)], reads=[hmB, cB], writes=[pB])
                        cx.op("act", lambda e: e.activation(out=hmT[:, k0:k0 + kk, :], in_=pt[:, 0:kk * 128].rearrange("p (c n) -> p c n", c=kk), func=AF.Copy),
                              reads=[pB], writes=[hmTB])
                    WB2 = (KC * 256) // DC
                    WB2 = min(WB2, D)
                    for c0 in range(0, D, WB2):
                        wa = wtag[0] % 3
                        wtag[0] += 1
                        wv_ = wbufs[wa][:].rearrange("p k c -> p (k c)")[:, 0:DC * WB2].rearrange("p (k c) -> p k c", k=DC)
                        load_w(wv_, w2[e_][:, c0:c0 + WB2], wB[wa])
                        for c1 in range(0, WB2, 512):
                            cw = min(512, WB2 - c1)
                            pt, pB = pbank()
                            cx.pe([(lambda t, k=k: t.matmul(pt[:, 0:cw], hmT[:, k, :], wv_[:, k, c1:c1 + cw], start=(k == 0), stop=(k == DC - 1))) for k in range(DC)],
                                  reads=[hmTB, wB[wa]], writes=[pB])
                            dst = ye[a][:, c0 + c1:c0 + c1 + cw]
                            if (c1 // 512) % 2:
                                cx.op("dve", lambda e: e.tensor_copy(dst, pt[:, 0:cw]), reads=[pB], writes=[yeB[a]])
                            else:
                                cx.op("act", lambda e: e.activation(out=dst, in_=pt[:, 0:cw], func=AF.Copy), reads=[pB], writes=[yeB[a]])
                    cx.dma("pool", lambda q: q.dma_start(out=Y_d[e_ * 128:(e_ + 1) * 128, :], in_=ye[a][:]), reads=[yeB[a]])
                cx.barrier()

            chk(12)
            with contextlib.ExitStack() as st:
                gbc = sb(st, "fgbc", [128, D])
                y1 = [sb(st, "y1_%d" % i, [128, D]) for i in range(2)]
                y2 = [sb(st, "y2_%d" % i, [128, D]) for i in range(2)]
                xx = [sb(st, "xx_%d" % i, [128, D]) for i in range(2)]
                junk = sb(st, "fjunk", [128, D])
                ss = sb(st, "fss", [128, 8])
                gB, jB = Buf(), Buf()
                y1B, y2B, xxB = [Buf(), Buf()], [Buf(), Buf()], [Buf(), Buf()]
                ssB = [Buf() for _ in range(8)]
                cx.dma("sp", lambda q: q.dma_start(out=gbc[:], in_=gains["g_fin"][0].partition_broadcast(128)), writes=[gB])
                for gt in range(NB * 4):
                    a = gt % 2
                    cx.dma("sp", lambda q: q.dma_start(out=xx[a][:], in_=x2_d[gt * 128:(gt + 1) * 128, :]), writes=[xxB[a]])
                    for j, (yy, yyB) in enumerate(((y1[a], y1B[a]), (y2[a], y2B[a]))):
                        cx.dma("pool", lambda q, yy=yy, j=j: q.indirect_dma_start(out=yy[:, :], out_offset=None, in_=Y_d[:, :],
                                                                                 in_offset=bass.IndirectOffsetOnAxis(ap=idx_all[:, gt * 2 + j:gt * 2 + j + 1], axis=0)),
                               reads=[idxB], writes=[yyB])
                        cx.op("dve", lambda e, yy=yy, j=j: e.scalar_tensor_tensor(out=xx[a][:], in0=yy[:], scalar=cw_all[:, gt * 2 + j:gt * 2 + j + 1], in1=xx[a][:],
                                                                                 op0=ALU.mult, op1=ALU.add), reads=[yyB, cwB, xxB[a]], writes=[xxB[a]])
                    s1 = ss[:, gt % 8:gt % 8 + 1]
                    sB = ssB[gt % 8]
                    cx.op("act", lambda e: e.activation(out=junk[:], in_=xx[a][:], func=AF.Square, accum_out=s1), reads=[xxB[a]], writes=[jB, sB])
                    rstd_from_ss(s1, 128, sB, 1.0 / D)
                    cx.op("dve", lambda e: e.scalar_tensor_tensor(out=xx[a][:], in0=xx[a][:], scalar=s1, in1=gbc[:], op0=ALU.mult, op1=ALU.mult),
                          reads=[xxB[a], sB, gB], writes=[xxB[a]])
                    cx.dma("sp", lambda q: q.dma_start(out=out_d[gt * 128:(gt + 1) * 128, :], in_=xx[a][:]), reads=[xxB[a]])
                cx.barrier()
        _body()
        cx.dead = False
        cx.barrier()
    return nc


def host_inputs(cfg, inp):
    g = dims(cfg)
    D, NB, NC, TOK, PW, PG, HK, NH, NVB = g["D"], g["NB"], g["NC"], g["TOK"], g["PW"], g["PG"], g["HK"], g["NH"], g["NVB"]
    f = np.float32
    x = np.asarray(inp["x"], f)[0]
    S = x.shape[0]
    xp = np.zeros((S + 16, D), f)
    xp[8:8 + S] = x
    common = dict(
        mem=np.ascontiguousarray(np.asarray(inp["mem"], f)[0]),
        g_mix=np.asarray(inp["norm_mix"], f).reshape(1, D), g_xat=np.asarray(inp["norm_xattn"], f).reshape(1, D),
        g_mem=np.asarray(inp["norm_mem"], f).reshape(1, D), g_moe=np.asarray(inp["norm_moe"], f).reshape(1, D),
        g_fin=np.asarray(inp["norm_final"], f).reshape(1, D),
        w_in=np.asarray(inp["w_in"], f)[0], pool_w=np.asarray(inp["pool_w"], f)[0],
        psc=np.ascontiguousarray(np.asarray(inp["pool_scale"], f)[0].reshape(PW // 128, 128).T),
        lbp=np.ascontiguousarray(np.stack([np.asarray(inp["lb_fwd"], f), np.asarray(inp["lb_bwd"], f)], 0).reshape(2, 2, NH, 128).transpose(3, 0, 1, 2)),
        hgn=np.ascontiguousarray(np.asarray(inp["hgrn_norm"], f)[0].reshape(NH, 128).T),
        w_out=np.asarray(inp["w_out"], f)[0], wq=np.asarray(inp["w_q"], f)[0], wk=np.asarray(inp["w_k"], f)[0],
        wv=np.asarray(inp["w_v"], f)[0], wo=np.asarray(inp["w_o"], f)[0],
        wr=np.ascontiguousarray(np.concatenate([np.asarray(inp["w_router_group"], f)[0],
                                                np.asarray(inp["w_router_expert"], f)[0].transpose(1, 0, 2).reshape(D, 32)], 1)),
        w1=np.asarray(inp["w1"], f)[0], w3=np.asarray(inp["w3"], f)[0], w2=np.asarray(inp["w2"], f)[0],
        ident=np.eye(128, dtype=f), ones=np.ones((128, 128), f),
        sl=np.triu(np.ones((128, 128), f), 1),
        ebase=(np.arange(32, dtype=f) * 128).reshape(1, 32),
    )
    i = np.arange(128)
    same = (i[:, None] // 64) == (i[None, :] // 64)
    common["maskf"] = np.tile((same & (i[:, None] <= i[None, :])).astype(f), (1, 4))
    common["maskb"] = np.tile((same & (i[:, None] >= i[None, :])).astype(f), (1, 4))
    common["rmask"] = np.tile(((np.arange(512) % 64) != 0).astype(f)[None, :], (128, 1))
    t = np.arange(S)
    inv = np.zeros((4, S), f)
    for gi, w in enumerate((2, 4, 8, 16)):
        lo = np.clip(t - w // 2, 0, S - 1)
        hi = np.clip(t + w // 2 - 1, 0, S - 1)
        inv[gi] = 1.0 / (hi - lo + 1)
    maps = []
    for c in range(NC):
        m = dict(common)
        own = [c * NB + b for b in range(NB)]
        slots = own + [v for v in range(NVB) if v not in own]
        m["xh"] = np.stack([xp[v * TB: v * TB + 528] for v in slots], 0)
        m["invcnt"] = np.stack([inv[:, v * TB:(v + 1) * TB].reshape(-1) for v in own], 0)
        cm = np.zeros((NB, 2, NVB), f)
        for b in range(NB):
            for j, v in enumerate(slots):
                cm[b, 0, j] = 1.0 if v < own[b] else 0.0
                cm[b, 1, j] = 1.0 if v > own[b] else 0.0
        m["cmask"] = cm.reshape(NB, 2 * NVB)
        maps.append(m)
    return maps


FULL = dict(D=4096, NB=2, NC=8)
_NC_CACHE = {}


def run(cfg, inp):
    key = tuple(sorted(cfg.items()))
    if key not in _NC_CACHE:
        _NC_CACHE[key] = build(cfg)
    nc = _NC_CACHE[key]
    maps = host_inputs(cfg, inp)
    res = run_bass_kernel_spmd(nc, maps, core_ids=list(range(cfg["NC"])))
    out = np.concatenate([r["out"] for r in res.results], axis=0)
    return out[None].astype(np.float32)


def kernel(**inputs):
    return run(FULL, inputs)
```
